# Optimizing a Trainium2 kernel written in Bass

```python
import math
import jax
import jax.numpy as jnp
from jax import lax
import numpy as np

D_MODEL = 1024
BATCH = 4
SEQ = 4096
DEPTH = 2

HEAD_DIM = 64
D_MIX = D_MODEL
N_HEADS_MIX = D_MIX // HEAD_DIM
A_HEADS = N_HEADS_MIX // 4
A_WIDTH = A_HEADS * HEAD_DIM
DILATED_PATTERNS = ((128, 1), (512, 4), (2048, 16))
C_HEADS = N_HEADS_MIX // 4
C_WIDTH = C_HEADS * HEAD_DIM
MOBA_BLOCK = 256
MOBA_TOPK = 3
MOBA_Q_CHUNK = 64
B_HEADS = N_HEADS_MIX - A_HEADS - C_HEADS
SSM_HEAD_DIM = HEAD_DIM
D_SSM = B_HEADS * SSM_HEAD_DIM
SSM_STATE = 128
SSM_GROUPS = 2
SSM_CONV = 4
SSM_CHUNK = 128
CONV_DIM = D_SSM + 2 * SSM_GROUPS * SSM_STATE
D_FF = 4 * D_MODEL
D_IN = 3 * A_WIDTH + 3 * C_WIDTH + D_SSM + CONV_DIM + B_HEADS
H_ATTN = A_HEADS + C_HEADS
EPS = 1e-6
NEG_INF = -1e30

kernel_name = "hybrid_dilated_ssd_moba_block"


def rms_norm(x, w):
    xf = x.astype(jnp.float32)
    y = xf * lax.rsqrt(jnp.mean(xf * xf, axis=-1, keepdims=True) + EPS)
    return (y * w.astype(jnp.float32)).astype(x.dtype)


def alibi_slopes():
    return 2.0 ** (-8.0 * jnp.arange(1, H_ATTN + 1, dtype=jnp.float32) / H_ATTN)


def dilated_band_attention(q, k, v, slopes, window, dilation):
    bsz, seq, nh, hd = q.shape
    n_off = window // dilation
    blk = n_off
    length = seq // dilation
    n_blk = -(-length // blk)
    lp = n_blk * blk

    def to_sub(t):
        return t.reshape(bsz, length, dilation, nh, hd).transpose(0, 2, 1, 3, 4)

    qs = jnp.pad(to_sub(q), ((0, 0), (0, 0), (0, lp - length), (0, 0), (0, 0)))
    qs = qs.reshape(bsz, dilation, n_blk, blk, nh, hd)
    pad_kv = ((0, 0), (0, 0), (blk, lp - length), (0, 0), (0, 0))

    def windows(t):
        tb = jnp.pad(to_sub(t), pad_kv).reshape(bsz, dilation, n_blk + 1, blk, nh, hd)
        return jnp.concatenate([tb[:, :, :-1], tb[:, :, 1:]], axis=3)

    kw, vw = windows(k), windows(v)
    logits = jnp.einsum('brmahe,brmche->brhmac', qs, kw) * (hd ** -0.5)
    a_idx = jnp.arange(blk)[:, None]
    c_idx = jnp.arange(2 * blk)[None, :]
    offset = a_idx + blk - c_idx
    key_idx = jnp.arange(n_blk)[:, None, None] * blk - blk + c_idx[None]
    valid = (offset >= 0) & (offset <= n_off) & (key_idx >= 0)
    bias = -slopes[:, None, None, None] * (dilation * offset).astype(jnp.float32)
    logits = jnp.where(valid, logits + bias, NEG_INF)
    lse = jax.nn.logsumexp(logits, axis=-1)
    probs = jnp.exp(logits - lse[..., None])
    out = jnp.einsum('brhmac,brmche->brmahe', probs, vw)
    out = out.reshape(bsz, dilation, lp, nh, hd)[:, :, :length]
    out = out.transpose(0, 2, 1, 3, 4).reshape(bsz, seq, nh, hd)
    lse = lse.transpose(0, 1, 3, 4, 2).reshape(bsz, dilation, lp, nh)[:, :, :length]
    lse = lse.transpose(0, 2, 1, 3).reshape(bsz, seq, nh)
    return out, lse


def dilated_mixture(q, k, v, slopes):
    outs, lses = [], []
    for window, dilation in DILATED_PATTERNS:
        o, l = dilated_band_attention(q, k, v, slopes, window, dilation)
        outs.append(o)
        lses.append(l)
    w = jax.nn.softmax(jnp.stack(lses, axis=0), axis=0)
    return jnp.einsum('pbsh,pbshe->bshe', w, jnp.stack(outs, axis=0))


def causal_depthwise_conv(x, w, b):
    out = lax.conv_general_dilated(
        x, w[:, None, :].astype(x.dtype), window_strides=(1,),
        padding=((SSM_CONV - 1, 0),), dimension_numbers=('NWC', 'WIO', 'NWC'),
        feature_group_count=x.shape[-1])
    return out + b.astype(x.dtype)


def segsum_exp(a_cs):
    l = a_cs.shape[-1]
    mask = jnp.tril(jnp.ones((l, l), dtype=bool))
    diff = a_cs[..., :, None] - a_cs[..., None, :]
    return jnp.where(mask, jnp.exp(jnp.where(mask, diff, 0.0)), 0.0)


def ssd_scan(x, dt, a, b_mat, c_mat):
    bsz, seq, nh, hp = x.shape
    nc, l = seq // SSM_CHUNK, SSM_CHUNK
    xd = (x * dt[..., None]).reshape(bsz, nc, l, nh, hp)
    da = (dt * a).reshape(bsz, nc, l, nh).transpose(0, 1, 3, 2)
    bc = b_mat.reshape(bsz, nc, l, nh, -1)
    cc = c_mat.reshape(bsz, nc, l, nh, -1)
    a_cs = jnp.cumsum(da, axis=-1)
    scores = jnp.einsum('bclhn,bcshn->bchls', cc, bc) * segsum_exp(a_cs)
    y_diag = jnp.einsum('bchls,bcshp->bclhp', scores, xd)
    decay_states = jnp.exp(a_cs[..., -1:] - a_cs)
    chunk_states = jnp.einsum('bclhn,bchl,bclhp->bchpn', bc, decay_states, xd)
    chunk_decay = jnp.exp(a_cs[..., -1])

    def step(state, inp):
        cs, dec = inp
        return state * dec[..., None, None] + cs, state

    init = jnp.zeros((bsz, nh, hp, bc.shape[-1]), jnp.float32)
    _, states_in = lax.scan(step, init, (chunk_states.transpose(1, 0, 2, 3, 4),
                                         chunk_decay.transpose(1, 0, 2)))
    states_in = states_in.transpose(1, 0, 2, 3, 4)
    y_off = jnp.einsum('bclhn,bchpn,bchl->bclhp', cc, states_in, jnp.exp(a_cs))
    return (y_diag + y_off).reshape(bsz, seq, nh, hp)


def mamba2_mixer(z, xbc, dt_raw, conv_w, conv_b, dt_bias, a_log, d_skip, norm_w):
    bsz, seq, _ = z.shape
    xbc = jax.nn.silu(causal_depthwise_conv(xbc, conv_w, conv_b)).astype(jnp.float32)
    xs = xbc[..., :D_SSM].reshape(bsz, seq, B_HEADS, SSM_HEAD_DIM)
    rep = B_HEADS // SSM_GROUPS
    bm = jnp.repeat(xbc[..., D_SSM:D_SSM + SSM_GROUPS * SSM_STATE].reshape(bsz, seq, SSM_GROUPS, SSM_STATE), rep, axis=2)
    cm = jnp.repeat(xbc[..., D_SSM + SSM_GROUPS * SSM_STATE:].reshape(bsz, seq, SSM_GROUPS, SSM_STATE), rep, axis=2)
    dt = jax.nn.softplus(dt_raw.astype(jnp.float32) + dt_bias.astype(jnp.float32))
    a = -jnp.exp(a_log.astype(jnp.float32))
    y = ssd_scan(xs, dt, a, bm, cm) + d_skip.astype(jnp.float32)[:, None] * xs
    y = y.reshape(bsz, seq, D_SSM) * jax.nn.silu(z.astype(jnp.float32))
    y = rms_norm(y.reshape(bsz, seq, SSM_GROUPS, D_SSM // SSM_GROUPS),
                 norm_w.reshape(SSM_GROUPS, D_SSM // SSM_GROUPS))
    return y.reshape(bsz, seq, D_SSM)


def moba_attention(q, k, v, slopes):
    bsz, seq, nh, hd = q.shape
    n_blk = -(-seq // MOBA_BLOCK)
    sp = n_blk * MOBA_BLOCK
    pad = ((0, 0), (0, sp - seq), (0, 0), (0, 0))
    q, k, v = jnp.pad(q, pad), jnp.pad(k, pad), jnp.pad(v, pad)
    kb = k.reshape(bsz, n_blk, MOBA_BLOCK, nh, hd)
    vb = v.reshape(bsz, n_blk, MOBA_BLOCK, nh, hd)
    k_mean = kb.mean(axis=2)
    kbh = kb.transpose(0, 3, 1, 2, 4)
    vbh = vb.transpose(0, 3, 1, 2, 4)
    n_sel = min(MOBA_TOPK, n_blk - 1)
    chunks_per_blk = MOBA_BLOCK // MOBA_Q_CHUNK
    n_chunks = sp // MOBA_Q_CHUNK
    scale = hd ** -0.5
    q_chunks = q.reshape(bsz, n_chunks, MOBA_Q_CHUNK, nh, hd).transpose(1, 0, 2, 3, 4)
    in_blk = jnp.arange(MOBA_BLOCK)
    b_idx = jnp.arange(bsz)[:, None, None, None]
    h_idx = jnp.arange(nh)[None, None, :, None]

    def chunk_fn(args):
        qi, ci = args
        blk = ci // chunks_per_blk
        pos_q = ci * MOBA_Q_CHUNK + jnp.arange(MOBA_Q_CHUNK)
        k_own = lax.dynamic_index_in_dim(kb, blk, axis=1, keepdims=False)
        v_own = lax.dynamic_index_in_dim(vb, blk, axis=1, keepdims=False)
        dist_own = pos_q[:, None] - (blk * MOBA_BLOCK + in_blk)[None, :]
        logit_own = (jnp.einsum('bqhe,bkhe->bhqk', qi, k_own) * scale
                     - slopes[:, None, None] * dist_own.astype(jnp.float32))
        logit_own = jnp.where(dist_own >= 0, logit_own, NEG_INF)
        if n_sel == 0:
            p_own = jax.nn.softmax(logit_own, axis=-1)
            return jnp.einsum('bhqk,bkhe->bqhe', p_own, v_own)
        gate = jnp.einsum('bqhe,bnhe->bqhn', qi, k_mean)
        gate = jnp.where(jnp.arange(n_blk) < blk, gate, NEG_INF)
        _, idx = lax.top_k(gate, n_sel)
        k_sel = kbh[b_idx, h_idx, idx]
        v_sel = vbh[b_idx, h_idx, idx]
        dist_sel = pos_q[None, :, None, None, None] - (idx[..., None] * MOBA_BLOCK + in_blk)
        logit_sel = (jnp.einsum('bqhe,bqhsje->bqhsj', qi, k_sel) * scale
                     - slopes[None, None, :, None, None] * dist_sel.astype(jnp.float32))
        sel_valid = jnp.arange(n_sel) < blk
        logit_sel = jnp.where(sel_valid[:, None], logit_sel, NEG_INF)
        logit_sel = logit_sel.reshape(bsz, MOBA_Q_CHUNK, nh, n_sel * MOBA_BLOCK).transpose(0, 2, 1, 3)
        probs = jax.nn.softmax(jnp.concatenate([logit_own, logit_sel], axis=-1), axis=-1)
        p_own = probs[..., :MOBA_BLOCK]
        p_sel = probs[..., MOBA_BLOCK:].transpose(0, 2, 1, 3).reshape(bsz, MOBA_Q_CHUNK, nh, n_sel, MOBA_BLOCK)
        return (jnp.einsum('bhqk,bkhe->bqhe', p_own, v_own)
                + jnp.einsum('bqhsj,bqhsje->bqhe', p_sel, v_sel))

    out = lax.map(chunk_fn, (q_chunks, jnp.arange(n_chunks)))
    return out.transpose(1, 0, 2, 3, 4).reshape(bsz, sp, nh, hd)[:, :seq]


def hybrid_layer(x, norm1_w, w_in, a_q_norm, a_k_norm, c_q_norm, c_k_norm,
                 conv_w, conv_b, dt_bias, a_log, d_skip, ssm_norm_w, w_out,
                 norm2_w, w_mlp_in, w_mlp_out, slopes):
    bsz, seq, _ = x.shape
    h = rms_norm(x, norm1_w)
    proj = h @ w_in
    sizes = [A_WIDTH] * 3 + [C_WIDTH] * 3 + [D_SSM, CONV_DIM]
    points, acc = [], 0
    for s in sizes:
        acc += s
        points.append(acc)
    qa, ka, va, qc, kc, vc, z, xbc, dt_raw = jnp.split(proj, points, axis=-1)

    def heads(t, n):
        return t.reshape(bsz, seq, n, HEAD_DIM)

    f32 = jnp.float32
    ya = dilated_mixture(rms_norm(heads(qa, A_HEADS), a_q_norm).astype(f32),
                         rms_norm(heads(ka, A_HEADS), a_k_norm).astype(f32),
                         heads(va, A_HEADS).astype(f32), slopes[0::2])
    yb = mamba2_mixer(z, xbc, dt_raw, conv_w, conv_b, dt_bias, a_log, d_skip, ssm_norm_w)
    yc = moba_attention(rms_norm(heads(qc, C_HEADS), c_q_norm).astype(f32),
                        rms_norm(heads(kc, C_HEADS), c_k_norm).astype(f32),
                        heads(vc, C_HEADS).astype(f32), slopes[1::2])
    y = jnp.concatenate([ya.reshape(bsz, seq, A_WIDTH), yb, yc.reshape(bsz, seq, C_WIDTH)],
                        axis=-1).astype(x.dtype)
    x = x + y @ w_out
    h = rms_norm(x, norm2_w)
    return x + jnp.square(jax.nn.relu(h @ w_mlp_in)) @ w_mlp_out


def setup_inputs(seed: int = 0) -> dict:
    key = jax.random.key(seed)
    ks = jax.random.split(key, 17)
    f32 = jnp.float32

    def nrm(k, shape, scale):
        return jax.random.normal(k, shape, f32) * scale

    def gain(k, shape, noise=0.02):
        return 1.0 + noise * jax.random.normal(k, shape, f32)

    dt = jnp.exp(jax.random.uniform(ks[9], (DEPTH, B_HEADS), f32, math.log(1e-3), math.log(1e-1)))
    return {
        "x": jax.random.normal(ks[0], (BATCH, SEQ, D_MODEL), f32),
        "norm1_w": gain(ks[1], (DEPTH, D_MODEL)),
        "w_in": nrm(ks[2], (DEPTH, D_MODEL, D_IN), D_MODEL ** -0.5),
        "a_q_norm": gain(ks[3], (DEPTH, HEAD_DIM)),
        "a_k_norm": gain(ks[4], (DEPTH, HEAD_DIM)),
        "c_q_norm": gain(ks[5], (DEPTH, HEAD_DIM)),
        "c_k_norm": gain(ks[6], (DEPTH, HEAD_DIM)),
        "conv_w": nrm(ks[7], (DEPTH, SSM_CONV, CONV_DIM), SSM_CONV ** -0.5),
        "conv_b": nrm(ks[8], (DEPTH, CONV_DIM), 0.02),
        "dt_bias": dt + jnp.log(-jnp.expm1(-dt)),
        "a_log": jnp.log(jax.random.uniform(ks[10], (DEPTH, B_HEADS), f32, 1.0, 16.0)),
        "d_skip": gain(ks[11], (DEPTH, B_HEADS), 0.1),
        "ssm_norm_w": gain(ks[12], (DEPTH, D_SSM)),
        "w_out": nrm(ks[13], (DEPTH, D_MIX, D_MODEL), D_MIX ** -0.5),
        "norm2_w": gain(ks[14], (DEPTH, D_MODEL)),
        "w_mlp_in": nrm(ks[15], (DEPTH, D_MODEL, D_FF), D_MODEL ** -0.5),
        "w_mlp_out": nrm(ks[16], (DEPTH, D_FF, D_MODEL), D_FF ** -0.5),
    }


def reference(x, norm1_w, w_in, a_q_norm, a_k_norm, c_q_norm, c_k_norm, conv_w, conv_b,
              dt_bias, a_log, d_skip, ssm_norm_w, w_out, norm2_w, w_mlp_in, w_mlp_out):
    slopes = alibi_slopes()
    for i in range(DEPTH):
        x = hybrid_layer(x, norm1_w[i], w_in[i], a_q_norm[i], a_k_norm[i], c_q_norm[i],
                         c_k_norm[i], conv_w[i], conv_b[i], dt_bias[i], a_log[i], d_skip[i],
                         ssm_norm_w[i], w_out[i], norm2_w[i], w_mlp_in[i], w_mlp_out[i], slopes)
    return x
```

```python
import math
from contextlib import ExitStack

import numpy as np
import ml_dtypes

import concourse.bass as bass
import concourse.mybir as mybir
from concourse.bass_utils import run_bass_kernel_spmd

F32 = mybir.dt.float32
BF16 = mybir.dt.bfloat16
ALU = mybir.AluOpType
AF = mybir.ActivationFunctionType
AX = mybir.AxisListType

ENGS = ("pe", "act", "dve", "pool", "sp")
S = 4096
NT = 32
DM = 1024
DIN = 3080
DFF = 4096
EPS = 1e-6
NEG = -1e30
SLOPES = [2.0 ** (-8.0 * i / 8) for i in range(1, 9)]
SL_A = SLOPES[0::2]
SL_C = SLOPES[1::2]


class Slot:
    __slots__ = ("sem", "cnt")

    def __init__(self, sem):
        self.sem = sem
        self.cnt = 0


class Res:
    __slots__ = ("name", "w", "r", "slot", "excl")

    def __init__(self, name, slot=None):
        self.name = name
        self.w = None
        self.r = []
        self.slot = slot
        self.excl = False


class Op:
    __slots__ = ("eng", "fn", "deps", "need_inc", "inc_idx", "kind")

    def __init__(self, eng, fn, deps, kind="c"):
        self.eng = eng
        self.fn = fn
        self.deps = deps
        self.need_inc = False
        self.inc_idx = 0
        self.kind = kind


class Prog:
    def __init__(self, nc, es, n_dma=84):
        self.nc = nc
        self.q = {e: [] for e in ENGS}
        self.esem = {e: es.enter_context(nc.semaphore("prog_" + e)) for e in ENGS if e != "sp"}
        self.slots = [Slot(es.enter_context(nc.semaphore("dq%d" % i))) for i in range(n_dma)]
        self.free = list(self.slots)
        self.phase = []

    def res(self, name, dma=False):
        slot = None
        if dma:
            slot = self.free.pop()
            self.phase.append(slot)
        return Res(name, slot)

    def _collect(self, reads, writes):
        deps = []
        for r in reads:
            if r.w is not None:
                deps.append(r.w)
        for w in writes:
            if w.w is not None:
                deps.append(w.w)
            deps.extend(w.r)
        for d in deps:
            if isinstance(d, Op):
                d.need_inc = True
        return deps

    def op(self, eng, fn, reads=(), writes=()):
        ex = [r for r in reads if r.excl]
        if ex:
            reads = [r for r in reads if not r.excl]
            writes = list(writes) + ex
        o = Op(eng, fn, self._collect(reads, writes))
        self.q[eng].append(o)
        for r in reads:
            r.r.append(o)
        for w in writes:
            w.w = o
            w.r = []
        return o

    def dma(self, queue, out, in_, reads=(), writes=(), owner=None, **kw):
        slot = owner.slot
        deps = self._collect(reads, writes)
        slot.cnt += 16
        tok = ("d", slot, slot.cnt)

        def fn(e, out=out, in_=in_, sem=slot.sem):
            return e.dma_start(out=out, in_=in_, **kw).then_inc(sem, 16)

        self.q[queue].append(Op(queue, fn, deps, kind="dma"))
        for r in reads:
            r.r.append(tok)
        for w in writes:
            w.w = tok
            w.r = []
        return tok

    def barrier(self):
        deps = []
        for e in ENGS:
            for o in reversed(self.q[e]):
                if o.kind == "c":
                    deps.append(o)
                    o.need_inc = True
                    break
        for s in self.slots:
            if s.cnt > 0:
                deps.append(("d", s, s.cnt))
        for e in ENGS:
            self.q[e].append(Op(e, None, list(deps), kind="wait"))

    def end_phase(self):
        self.barrier()
        self.free.extend(self.phase)
        self.phase = []

    def emit(self):
        nc = self.nc
        for e in ENGS:
            c = 0
            for o in self.q[e]:
                if o.kind == "c" and o.need_inc:
                    c += 1
                    o.inc_idx = c
        stats = {}

        def replay(ename, eng):
            observed = {}
            nwait = 0
            for o in self.q[ename]:
                need = {}
                for d in o.deps:
                    if isinstance(d, Op):
                        if d.eng == ename and ename == "pe":
                            continue
                        key = d.eng
                        sem = self.esem[d.eng]
                        val = d.inc_idx
                    else:
                        _, s, val = d
                        key = id(s)
                        sem = s.sem
                    if observed.get(key, 0) >= val:
                        continue
                    if key not in need or need[key][1] < val:
                        need[key] = (sem, val)
                for key, (sem, val) in need.items():
                    eng.wait_ge(sem, val)
                    observed[key] = val
                    nwait += 1
                if o.fn is not None:
                    ins = o.fn(eng)
                    if o.kind == "c" and o.need_inc:
                        ins.then_inc(self.esem[ename], 1)
            stats[ename] = (len(self.q[ename]), nwait)

        with nc.Block() as block:
            @block.tensor
            def _(e):
                replay("pe", e)

            @block.scalar
            def _(e):
                replay("act", e)

            @block.vector
            def _(e):
                replay("dve", e)

            @block.gpsimd
            def _(e):
                replay("pool", e)

            @block.sync
            def _(e):
                replay("sp", e)
        return stats


DT_SIZE = {F32: 4, BF16: 2}


class Arena:
    def __init__(self, nc):
        nbytes = (nc.sbuf_bytes_remaining - 2048) // 64 * 64
        self.words = nbytes // 4
        self.t = nc.alloc_sbuf_tensor("arena", [128, self.words], F32)
        self.off = 0
        self.peak = 0

    def alloc(self, cols, dtype=F32):
        words = (cols * DT_SIZE[dtype] + 3) // 4
        words = (words + 7) // 8 * 8
        assert self.off + words <= self.words, ("SBUF arena overflow", self.off * 4, words * 4, self.words * 4)
        ap = self.t[:, self.off:self.off + words]
        self.off += words
        self.peak = max(self.peak, self.off)
        if dtype != F32:
            ap = ap.bitcast(dtype)
        return ap[:, 0:cols]


def bcast_rows(ap1d, reps=1):
    n = ap1d.shape[0]
    if reps == 1:
        return bass.AP(ap1d.tensor, ap1d.offset, [[0, 128], [1, n]])
    return bass.AP(ap1d.tensor, ap1d.offset, [[0, 128], [0, reps], [1, n]])


def host_consts():
    c = {}
    c["ident"] = np.eye(128, dtype=np.float32)
    kl = np.arange(128)[:, None]
    def mk(nof, slopes, mult_fn):
        col = np.arange(nof * 128)[None, :]
        delta = (col - kl).astype(np.float64)
        out = np.zeros((4, 128, nof * 128), np.float32)
        for h, sl in enumerate(slopes):
            m = mult_fn(delta) * np.exp(-sl * np.maximum(delta, 0.0))
            out[h] = np.where(delta >= 0, m, 0.0).astype(np.float32)
        return out
    def multA(d):
        return ((d <= 128).astype(np.float64) + ((d % 4 == 0) & (d <= 512)) + ((d % 16 == 0) & (d <= 2048)))
    c["maskA"] = mk(17, SL_A, multA)
    c["maskC"] = mk(32, SL_C, lambda d: np.ones_like(d))
    k = np.arange(128)[:, None]
    j = np.arange(128)[None, :]
    c["tri"] = (k <= j).astype(np.float32)
    c["upp"] = (k > j).astype(np.float32)
    c["caus"] = (j >= k).astype(np.float32)
    pm = np.zeros((128, 32), np.float32)
    pm[:, 16:] = NEG
    c["pastm"] = pm
    return c


CONST_SHAPES = {"ident": [128, 128], "maskA": [4, 128, 17 * 128], "maskC": [4, 128, 32 * 128],
                "tri": [128, 128], "upp": [128, 128], "caus": [128, 128], "pastm": [128, 32]}

PARAM_SHAPES = {
    "norm1_w": [2, 1024], "w_in": [2, 1024, 3080], "a_q_norm": [2, 64], "a_k_norm": [2, 64],
    "c_q_norm": [2, 64], "c_k_norm": [2, 64], "conv_w": [2, 4, 1024], "conv_b": [2, 1024],
    "dt_bias": [2, 8], "a_log": [2, 8], "d_skip": [2, 8], "ssm_norm_w": [2, 512],
    "w_out": [2, 1024, 1024], "norm2_w": [2, 1024], "w_mlp_in": [2, 1024, 4096],
    "w_mlp_out": [2, 4096, 1024],
}

SCRATCH = {
    "qkT": ([8, 128, S], BF16), "v_a": ([S, 256], BF16), "v_c": ([S, 256], BF16),
    "sel": ([S, 64], F32), "z": ([S, 512], F32), "xbcT": ([1024, S], F32), "dtr": ([S, 8], F32),
    "Y": ([S, 1024], BF16), "x1": ([S, 1024], F32), "h2T": ([1024, S], BF16), "xcur": ([S, 1024], F32),
}


class Ctx:
    pass


def phase_A(C, l, x_src):
    nc, P, D = C.nc, C.P, C.D
    A = C.arena
    A.off = C.base_off
    bank, rb = C.bank, C.rb
    win = A.alloc(8 * DIN, BF16).rearrange("p (k n) -> p k n", k=8)
    r_win = P.res("win", dma=True)
    for k in range(8):
        P.dma("pool", win[:, k, :], D["w_in"][l, k * 128:(k + 1) * 128, :], writes=[r_win], owner=r_win)
    n1w = A.alloc(1024)
    qkwA = A.alloc(512)
    qwC = A.alloc(256)
    kwC = A.alloc(256)
    r_small = P.res("smallA", dma=True)
    P.dma("sp", n1w, bcast_rows(D["norm1_w"][l]), writes=[r_small], owner=r_small)
    P.dma("sp", qkwA[:, 0:256].rearrange("p (r n) -> p r n", r=4), bcast_rows(D["a_q_norm"][l], 4), writes=[r_small], owner=r_small)
    P.dma("sp", qkwA[:, 256:512].rearrange("p (r n) -> p r n", r=4), bcast_rows(D["a_k_norm"][l], 4), writes=[r_small], owner=r_small)
    P.dma("sp", qwC.rearrange("p (r n) -> p r n", r=4), bcast_rows(D["c_q_norm"][l], 4), writes=[r_small], owner=r_small)
    P.dma("sp", kwC.rearrange("p (r n) -> p r n", r=4), bcast_rows(D["c_k_norm"][l], 4), writes=[r_small], owner=r_small)

    NXB = 3
    xs = [A.alloc(1024) for _ in range(NXB)]
    r_xs = [P.res("xs%d" % i, dma=True) for i in range(NXB)]
    junk = A.alloc(1024)
    r_junk = P.res("junk")
    st = [A.alloc(8) for _ in range(2)]
    r_st = [P.res("st%d" % i) for i in range(2)]
    hb = [A.alloc(1024, BF16) for _ in range(2)]
    r_hb = [P.res("hb%d" % i) for i in range(2)]
    hT = [A.alloc(8 * 512, BF16).rearrange("p (k n) -> p k n", k=8) for _ in range(2)]
    r_hT = [P.res("hT%d" % i) for i in range(2)]
    sq = [A.alloc(512) for _ in range(2)]
    r_sq = [P.res("sq%d" % i) for i in range(2)]
    hst = [A.alloc(48) for _ in range(2)]
    r_hst = [P.res("hst%d" % i) for i in range(2)]
    qn = [A.alloc(512) for _ in range(2)]
    r_qn = [P.res("qn%d" % i) for i in range(2)]
    qkbA = [A.alloc(512, BF16) for _ in range(2)]
    r_qkbA = [P.res("qkbA%d" % i) for i in range(2)]
    qkC = [A.alloc(512) for _ in range(2)]
    r_qkC = [P.res("qkC%d" % i) for i in range(2)]
    stA = [A.alloc(4 * 512, BF16).rearrange("p (j n) -> p j n", j=4) for _ in range(2)]
    r_stA = [P.res("stA%d" % i, dma=True) for i in range(2)]
    stC = [A.alloc(4 * 512, BF16).rearrange("p (j n) -> p j n", j=4) for _ in range(2)]
    r_stC = [P.res("stC%d" % i, dma=True) for i in range(2)]
    stv = [A.alloc(512, BF16) for _ in range(2)]
    r_stv = [P.res("stv%d" % i, dma=True) for i in range(2)]
    stz = [A.alloc(512) for _ in range(2)]
    r_stz = [P.res("stz%d" % i, dma=True) for i in range(2)]
    stdt = [A.alloc(8) for _ in range(2)]
    r_stdt = [P.res("stdt%d" % i, dma=True) for i in range(2)]
    stx = [A.alloc(512) for _ in range(2)]
    r_stx = [P.res("stx%d" % i, dma=True) for i in range(2)]
    qTf = [A.alloc(256).rearrange("p (j n) -> p j n", j=2) for _ in range(2)]
    r_qTf = [P.res("qTf%d" % i) for i in range(2)]
    ksum = [A.alloc(2) for _ in range(2)]
    r_ksum = [P.res("ksum%d" % i) for i in range(2)]
    kmT = A.alloc(32).rearrange("p (j n) -> p j n", j=2)
    r_kmT = P.res("kmT")
    gm = [A.alloc(64) for _ in range(2)]
    r_gm = [P.res("gm%d" % i) for i in range(2)]
    mx8 = [A.alloc(32) for _ in range(2)]
    r_mx8 = [P.res("mx8%d" % i) for i in range(2)]
    selt = [A.alloc(64) for _ in range(2)]
    r_selt = [P.res("selt%d" % i, dma=True) for i in range(2)]

    P.op("dve", lambda e: e.memset(kmT, 0.0), writes=[r_kmT])

    psT = bank[0][:].bitcast(BF16)
    ps6 = bank[6][:].bitcast(BF16)
    x_view = x_src.rearrange("(t p) d -> t p d", p=128)

    def load_x(t):
        b = t % NXB
        P.dma("sp", xs[b], x_view[t], writes=[r_xs[b]], owner=r_xs[b])

    load_x(0)
    load_x(1)
    for g in range(8):
        hTg, r_hTg = hT[g % 2], r_hT[g % 2]
        for i in range(4):
            t = 4 * g + i
            if t + 2 < NT:
                load_x(t + 2)
            b = t % NXB
            p2 = t % 2
            P.op("act", lambda e, b=b, p2=p2: e.activation(out=junk, in_=xs[b], func=AF.Square, accum_out=st[p2][:, 0:1]),
                 reads=[r_xs[b]], writes=[r_junk, r_st[p2]])
            P.op("act", lambda e, p2=p2: e.activation(out=st[p2][:, 1:2], in_=st[p2][:, 0:1], func=AF.Sqrt, scale=1.0 / DM, bias=C.eps_t[:, 0:1]),
                 reads=[r_st[p2]], writes=[r_st[p2]])
            P.op("dve", lambda e, p2=p2: e.reciprocal(out=st[p2][:, 2:3], in_=st[p2][:, 1:2]), reads=[r_st[p2]], writes=[r_st[p2]])
            P.op("dve", lambda e, b=b, p2=p2: e.scalar_tensor_tensor(out=hb[p2], in0=xs[b], scalar=st[p2][:, 2:3], in1=n1w, op0=ALU.mult, op1=ALU.mult),
                 reads=[r_xs[b], r_st[p2], r_small], writes=[r_hb[p2]])
            for k in range(8):
                P.op("pe", lambda e, k=k, p2=p2: e.transpose(out=psT[:, k * 128:(k + 1) * 128], in_=hb[p2][:, k * 128:(k + 1) * 128], identity=C.ident_b),
                     reads=[r_hb[p2]], writes=[rb[0]])
            P.op("act", lambda e, i=i, hTg=hTg: e.copy(out=hTg[:, :, i * 128:(i + 1) * 128], in_=psT.rearrange("p (k n) -> p k n", k=8)),
                 reads=[rb[0]], writes=[r_hTg])
        for i in range(4):
            t = 4 * g + i
            p2 = t % 2
            blk = t // 2
            tsl = slice(i * 128, (i + 1) * 128)
            for c in range(4):
                bk = 1 + (c % 2)
                for k in range(8):
                    P.op("pe", lambda e, k=k, c=c, bk=bk, hTg=hTg, tsl=tsl: e.matmul(bank[bk][:], lhsT=hTg[:, k, tsl], rhs=win[:, k, c * 512:(c + 1) * 512], start=(k == 0), stop=(k == 7)),
                         reads=[r_hTg, r_win], writes=[rb[bk]])
                pb = bank[bk][:]
                if c == 0:
                    P.op("act", lambda e, pb=pb, p2=p2: e.activation(out=sq[p2], in_=pb, func=AF.Square), reads=[rb[bk]], writes=[r_sq[p2]])
                    P.op("dve", lambda e, p2=p2: e.tensor_reduce(out=hst[p2][:, 0:8], in_=sq[p2].rearrange("p (h n) -> p h n", h=8), axis=AX.X, op=ALU.add),
                         reads=[r_sq[p2]], writes=[r_hst[p2]])
                    P.op("act", lambda e, p2=p2: e.activation(out=hst[p2][:, 16:24], in_=hst[p2][:, 0:8], func=AF.Sqrt, scale=1.0 / 64, bias=C.eps_t[:, 0:1]),
                         reads=[r_hst[p2]], writes=[r_hst[p2]])
                    P.op("dve", lambda e, p2=p2: e.reciprocal(out=hst[p2][:, 32:40], in_=hst[p2][:, 16:24]), reads=[r_hst[p2]], writes=[r_hst[p2]])
                    P.op("dve", lambda e, pb=pb, p2=p2: e.tensor_tensor(out=qn[p2].rearrange("p (h n) -> p h n", h=8), in0=pb.rearrange("p (h n) -> p h n", h=8),
                                                                   in1=hst[p2][:, 32:40].unsqueeze(2).to_broadcast([128, 8, 64]), op=ALU.mult),
                         reads=[rb[bk], r_hst[p2]], writes=[r_qn[p2]])
                    P.op("pool", lambda e, p2=p2: e.tensor_tensor(out=qkbA[p2], in0=qn[p2], in1=qkwA, op=ALU.mult),
                         reads=[r_qn[p2], r_small], writes=[r_qkbA[p2]])
                    for j in range(4):
                        P.op("pe", lambda e, j=j, p2=p2: e.transpose(out=ps6[:, j * 128:(j + 1) * 128], in_=qkbA[p2][:, j * 128:(j + 1) * 128], identity=C.ident_b),
                             reads=[r_qkbA[p2]], writes=[rb[6]])
                    P.op("act", lambda e, g=g, tsl=tsl: e.copy(out=stA[g % 2][:, :, tsl], in_=ps6[:, 0:512].rearrange("p (j n) -> p j n", j=4)),
                         reads=[rb[6]], writes=[r_stA[g % 2]])
                elif c == 1:
                    P.op("act", lambda e, pb=pb, p2=p2: e.copy(out=stv[p2][:, 0:256], in_=pb[:, 0:256]), reads=[rb[bk]], writes=[r_stv[p2]])
                    P.op("act", lambda e, pb=pb, p2=p2: e.activation(out=sq[p2][:, 0:256], in_=pb[:, 256:512], func=AF.Square), reads=[rb[bk]], writes=[r_sq[p2]])
                    P.op("dve", lambda e, p2=p2: e.tensor_reduce(out=hst[p2][:, 8:12], in_=sq[p2][:, 0:256].rearrange("p (h n) -> p h n", h=4), axis=AX.X, op=ALU.add),
                         reads=[r_sq[p2]], writes=[r_hst[p2]])
                    P.op("act", lambda e, p2=p2: e.activation(out=hst[p2][:, 24:28], in_=hst[p2][:, 8:12], func=AF.Sqrt, scale=1.0 / 64, bias=C.eps_t[:, 0:1]),
                         reads=[r_hst[p2]], writes=[r_hst[p2]])
                    P.op("dve", lambda e, p2=p2: e.reciprocal(out=hst[p2][:, 40:44], in_=hst[p2][:, 24:28]), reads=[r_hst[p2]], writes=[r_hst[p2]])
                    P.op("dve", lambda e, pb=pb, p2=p2: e.tensor_tensor(out=qn[p2][:, 0:256].rearrange("p (h n) -> p h n", h=4), in0=pb[:, 256:512].rearrange("p (h n) -> p h n", h=4),
                                                                   in1=hst[p2][:, 40:44].unsqueeze(2).to_broadcast([128, 4, 64]), op=ALU.mult),
                         reads=[rb[bk], r_hst[p2]], writes=[r_qn[p2]])
                    P.op("pool", lambda e, p2=p2: e.tensor_tensor(out=qkC[p2][:, 0:256], in0=qn[p2][:, 0:256], in1=qwC, op=ALU.mult),
                         reads=[r_qn[p2], r_small], writes=[r_qkC[p2]])
                elif c == 2:
                    P.op("act", lambda e, pb=pb, p2=p2: e.copy(out=stv[p2][:, 256:512], in_=pb[:, 256:512]), reads=[rb[bk]], writes=[r_stv[p2]])
                    P.op("act", lambda e, pb=pb, p2=p2: e.activation(out=sq[p2][:, 256:512], in_=pb[:, 0:256], func=AF.Square), reads=[rb[bk]], writes=[r_sq[p2]])
                    P.op("dve", lambda e, p2=p2: e.tensor_reduce(out=hst[p2][:, 12:16], in_=sq[p2][:, 256:512].rearrange("p (h n) -> p h n", h=4), axis=AX.X, op=ALU.add),
                         reads=[r_sq[p2]], writes=[r_hst[p2]])
                    P.op("act", lambda e, p2=p2: e.activation(out=hst[p2][:, 28:32], in_=hst[p2][:, 12:16], func=AF.Sqrt, scale=1.0 / 64, bias=C.eps_t[:, 0:1]),
                         reads=[r_hst[p2]], writes=[r_hst[p2]])
                    P.op("dve", lambda e, p2=p2: e.reciprocal(out=hst[p2][:, 44:48], in_=hst[p2][:, 28:32]), reads=[r_hst[p2]], writes=[r_hst[p2]])
                    P.op("dve", lambda e, pb=pb, p2=p2: e.tensor_tensor(out=qn[p2][:, 256:512].rearrange("p (h n) -> p h n", h=4), in0=pb[:, 0:256].rearrange("p (h n) -> p h n", h=4),
                                                                   in1=hst[p2][:, 44:48].unsqueeze(2).to_broadcast([128, 4, 64]), op=ALU.mult),
                         reads=[rb[bk], r_hst[p2]], writes=[r_qn[p2]])
                    P.op("pool", lambda e, p2=p2: e.tensor_tensor(out=qkC[p2][:, 256:512], in0=qn[p2][:, 256:512], in1=kwC, op=ALU.mult),
                         reads=[r_qn[p2], r_small], writes=[r_qkC[p2]])
                    P.dma("sp", D["v_a"][t * 128:(t + 1) * 128, :], stv[p2][:, 0:256], reads=[r_stv[p2]], owner=r_stv[p2])
                    P.dma("sp", D["v_c"][t * 128:(t + 1) * 128, :], stv[p2][:, 256:512], reads=[r_stv[p2]], owner=r_stv[p2])
                    for j in range(4):
                        P.op("pe", lambda e, j=j, p2=p2: e.transpose(out=bank[7][:, j * 128:(j + 1) * 128], in_=qkC[p2][:, j * 128:(j + 1) * 128], identity=C.ident_f),
                             reads=[r_qkC[p2]], writes=[rb[7]])
                    P.op("act", lambda e, g=g, tsl=tsl: e.copy(out=stC[g % 2][:, :, tsl], in_=bank[7][:].rearrange("p (j n) -> p j n", j=4)),
                         reads=[rb[7]], writes=[r_stC[g % 2]])
                    P.op("dve", lambda e, p2=p2: e.tensor_copy(out=qTf[p2], in_=bank[7][:, 0:256].rearrange("p (j n) -> p j n", j=2)),
                         reads=[rb[7]], writes=[r_qTf[p2]])
                    P.op("dve", lambda e, p2=p2: e.tensor_reduce(out=ksum[p2], in_=bank[7][:, 256:512].rearrange("p (j n) -> p j n", j=2), axis=AX.X, op=ALU.add),
                         reads=[rb[7]], writes=[r_ksum[p2]])
                    for h in range(4):
                        hp, pr = h % 2, h // 2
                        P.op("pe", lambda e, h=h, hp=hp, pr=pr, p2=p2: e.matmul(bank[3][:, 32 + h * 16:48 + h * 16], lhsT=qTf[p2][hp * 64:(hp + 1) * 64, pr, :],
                                                                             rhs=kmT[hp * 64:(hp + 1) * 64, pr, :], start=True, stop=True),
                             reads=[r_qTf[p2], r_kmT], writes=[rb[3]])
                    P.op("dve", lambda e, p2=p2, blk=blk: e.tensor_tensor(out=gm[p2].rearrange("p (h n) -> p h n", h=4), in0=bank[3][:, 32:96].rearrange("p (h n) -> p h n", h=4),
                                                                     in1=C.pastm[:, 16 - blk:32 - blk].unsqueeze(1).to_broadcast([128, 4, 16]), op=ALU.add),
                         reads=[rb[3]], writes=[r_gm[p2]])
                    for h in range(4):
                        P.op("dve", lambda e, h=h, p2=p2: e.max(out=mx8[p2][:, h * 8:(h + 1) * 8], in_=gm[p2][:, h * 16:(h + 1) * 16]),
                             reads=[r_gm[p2]], writes=[r_mx8[p2]])
                    P.op("dve", lambda e, p2=p2: e.tensor_tensor(out=selt[p2].rearrange("p (h n) -> p h n", h=4), in0=gm[p2].rearrange("p (h n) -> p h n", h=4),
                                                            in1=mx8[p2].rearrange("p (h n) -> p h n", h=4)[:, :, 2:3].to_broadcast([128, 4, 16]), op=ALU.is_ge),
                         reads=[r_gm[p2], r_mx8[p2]], writes=[r_selt[p2]])
                    P.op("dve", lambda e, p2=p2, blk=blk: e.memset(selt[p2].rearrange("p (h n) -> p h n", h=4)[:, :, blk:blk + 1], 1.0),
                         reads=[], writes=[r_selt[p2]])
                    P.dma("sp", D["sel"][t * 128:(t + 1) * 128, :], selt[p2], reads=[r_selt[p2]], owner=r_selt[p2])
                    P.op("dve", lambda e, p2=p2, blk=blk: e.scalar_tensor_tensor(out=kmT[:, :, blk], in0=ksum[p2], scalar=1.0 / 256, in1=kmT[:, :, blk], op0=ALU.mult, op1=ALU.add),
                         reads=[r_ksum[p2], r_kmT], writes=[r_kmT])
                else:
                    P.op("act", lambda e, pb=pb, p2=p2: e.copy(out=stz[p2], in_=pb), reads=[rb[bk]], writes=[r_stz[p2]])
                    P.dma("sp", D["z"][t * 128:(t + 1) * 128, :], stz[p2], reads=[r_stz[p2]], owner=r_stz[p2])
            for k in range(8):
                P.op("pe", lambda e, k=k, hTg=hTg, tsl=tsl: e.matmul(bank[3][:, 0:8], lhsT=hTg[:, k, tsl], rhs=win[:, k, 3072:3080], start=(k == 0), stop=(k == 7)),
                     reads=[r_hTg, r_win], writes=[rb[3]])
            P.op("act", lambda e, p2=p2: e.copy(out=stdt[p2], in_=bank[3][:, 0:8]), reads=[rb[3]], writes=[r_stdt[p2]])
            P.dma("sp", D["dtr"][t * 128:(t + 1) * 128, :], stdt[p2], reads=[r_stdt[p2]], owner=r_stdt[p2])
        for j in range(4):
            P.dma("sp", D["qkT"][j][:, g * 512:(g + 1) * 512], stA[g % 2][:, j, :], reads=[r_stA[g % 2]], owner=r_stA[g % 2])
            P.dma("sp", D["qkT"][4 + j][:, g * 512:(g + 1) * 512], stC[g % 2][:, j, :], reads=[r_stC[g % 2]], owner=r_stC[g % 2])
        for m in range(8):
            bk = 4 + (m % 2)
            for k in range(8):
                P.op("pe", lambda e, k=k, m=m, bk=bk, hTg=hTg: e.matmul(bank[bk][:], lhsT=win[:, k, 2048 + m * 128:2048 + (m + 1) * 128], rhs=hTg[:, k, :], start=(k == 0), stop=(k == 7)),
                     reads=[r_hTg, r_win], writes=[rb[bk]])
            P.op("act", lambda e, bk=bk, m=m: e.copy(out=stx[m % 2], in_=bank[bk][:]), reads=[rb[bk]], writes=[r_stx[m % 2]])
            P.dma("sp", D["xbcT"][m * 128:(m + 1) * 128, g * 512:(g + 1) * 512], stx[m % 2], reads=[r_stx[m % 2]], owner=r_stx[m % 2])
    P.end_phase()


def phase_attn(C, l, kind):
    nc, P, D = C.nc, C.P, C.D
    A = C.arena
    A.off = C.base_off
    bank, rb = C.bank, C.rb
    isA = kind == "A"
    nof = 17 if isA else 32
    qi0, ki0 = (0, 2) if isA else (4, 6)
    ycol = 0 if isA else 768
    qT = [A.alloc(S, BF16) for _ in range(2)]
    kT = [A.alloc(S, BF16) for _ in range(2)]
    r_qk = P.res("qk", dma=True)
    for p in range(2):
        P.dma("sp", qT[p], D["qkT"][qi0 + p], writes=[r_qk], owner=r_qk)
        P.dma("sp", kT[p], D["qkT"][ki0 + p], writes=[r_qk], owner=r_qk)
    V = A.alloc(NT * 4 * 65, BF16).rearrange("p (t h e) -> p t h e", t=NT, h=4)
    r_V = P.res("V", dma=True)
    vsrc = D["v_a" if isA else "v_c"].rearrange("(t p) (h e) -> p t h e", p=128, h=4)
    for t in range(NT):
        P.dma("sp", V[:, t, :, 0:64], vsrc[:, t], writes=[r_V], owner=r_V)
    P.op("pool", lambda e: e.memset(V[:, :, :, 64:65], 1.0), writes=[r_V])
    mask = A.alloc(4 * nof * 128).rearrange("p (h n) -> p h n", h=4)
    r_mask = P.res("mask", dma=True)
    msrc = D["c_maskA" if isA else "c_maskC"]
    for h in range(4):
        P.dma("sp", mask[:, h, :], msrc[h], writes=[r_mask], owner=r_mask)
    if not isA:
        selall = A.alloc(NT * 64).rearrange("p (t c) -> p t c", t=NT)
        r_sel = P.res("selall", dma=True)
        ssrc = D["sel"].rearrange("(t p) c -> p t c", p=128)
        for q4 in range(4):
            P.dma("sp", selall[:, q4 * 8:(q4 + 1) * 8, :], ssrc[:, q4 * 8:(q4 + 1) * 8, :], writes=[r_sel], owner=r_sel)
        acc = [A.alloc(4 * 65).rearrange("p (i e) -> p i e", i=4) for _ in range(2)]
        r_acc = [P.res("acc%d" % i) for i in range(2)]
        tmp = [A.alloc(4 * 65).rearrange("p (i e) -> p i e", i=4) for _ in range(3)]
        r_tmp = [P.res("tmp%d" % i) for i in range(3)]
    ex = [A.alloc(512) for _ in range(2)]
    r_ex = [P.res("ex%d" % i) for i in range(2)]
    pT = [A.alloc(512, BF16) for _ in range(2)]
    r_pT = [P.res("pT%d" % i) for i in range(2)]
    rc = [A.alloc(4) for _ in range(2)]
    r_rc = [P.res("rc%d" % i) for i in range(2)]
    yst = [A.alloc(4 * 256, BF16).rearrange("p (i c) -> p i c", i=4) for _ in range(2)]
    r_yst = [P.res("yst%d" % i, dma=True) for i in range(2)]

    cnt = {"step": 0, "ob": 0, "tmp": 0}

    def emit_qk(h, j, i0, i1):
        sb = cnt["step"] % 2
        cnt["step"] += 1
        pr, hp = h // 2, h % 2
        psl = slice(hp * 64, hp * 64 + 64)
        N = (i1 - i0 + 1) * 128
        P.op("pe", lambda e: e.matmul(bank[sb][:, 0:N], lhsT=kT[pr][psl, j * 128:(j + 1) * 128], rhs=qT[pr][psl, i0 * 128:(i1 + 1) * 128], start=True, stop=True),
             reads=[r_qk], writes=[rb[sb]])
        P.op("act", lambda e: e.activation(out=ex[sb][:, 0:N], in_=bank[sb][:, 0:N], func=AF.Exp, scale=0.125), reads=[rb[sb]], writes=[r_ex[sb]])
        P.op("dve", lambda e: e.tensor_tensor(out=pT[sb][:, 0:N], in0=ex[sb][:, 0:N], in1=mask[:, h, (i0 - j) * 128:(i1 - j + 1) * 128], op=ALU.mult),
             reads=[r_ex[sb], r_mask], writes=[r_pT[sb]])
        return sb

    def emit_pv(sb, h, j, i0, i1, g, ob, started):
        for i in range(i0, i1 + 1):
            st = not started[0]
            started[0] = True
            c0 = (i - 4 * g) * 65
            P.op("pe", lambda e, i=i, st=st, c0=c0: e.matmul(bank[ob][:, c0:c0 + 65], lhsT=pT[sb][:, (i - i0) * 128:(i - i0 + 1) * 128], rhs=V[:, j, h, :],
                                                           start=st, stop=(j == i), skip_group_check=True),
                 reads=[r_pT[sb], r_V], writes=[rb[ob]])

    def run_steps(steps, h, g, ob):
        started = [False]
        prev = None
        for (j, i0, i1) in steps:
            sb = emit_qk(h, j, i0, i1)
            if prev is not None:
                emit_pv(*prev, g, ob, started)
            prev = (sb, h, j, i0, i1)
        emit_pv(*prev, g, ob, started)

    for g in range(8):
        y2 = g % 2
        for h in range(4):
            if isA:
                ob = 2 + cnt["ob"] % 2
                cnt["ob"] += 1
                steps = []
                for j in range(max(0, 4 * g - 16), 4 * g + 4):
                    i0, i1 = max(4 * g, j), min(4 * g + 3, j + 16)
                    if i0 <= i1:
                        steps.append((j, i0, i1))
                run_steps(steps, h, g, ob)
                src = bank[ob][:, 0:260].rearrange("p (i e) -> p i e", i=4)
                r2 = cnt["ob"] % 2
                P.op("dve", lambda e, src=src, r2=r2: e.reciprocal(out=rc[r2].unsqueeze(2), in_=src[:, :, 64:65]), reads=[rb[ob]], writes=[r_rc[r2]])
                P.op("dve", lambda e, src=src, r2=r2, h=h, y2=y2: e.tensor_tensor(out=yst[y2][:, :, h * 64:(h + 1) * 64], in0=src[:, :, 0:64],
                                                                              in1=rc[r2].unsqueeze(2).to_broadcast([128, 4, 64]), op=ALU.mult),
                     reads=[rb[ob], r_rc[r2]], writes=[r_yst[y2]])
            else:
                a2 = h % 2
                for n in range(0, 2 * g + 2):
                    ob = 2 + cnt["ob"] % 3
                    cnt["ob"] += 1
                    steps = []
                    for j in (2 * n, 2 * n + 1):
                        i0, i1 = max(4 * g, j), 4 * g + 3
                        if i0 <= i1:
                            steps.append((j, i0, i1))
                    run_steps(steps, h, g, ob)
                    ia = max(4 * g, 2 * n) - 4 * g
                    ni = 4 - ia
                    src = bank[ob][:, 0:260].rearrange("p (i e) -> p i e", i=4)[:, ia:4, :]
                    selv = selall[:, 4 * g + ia:4 * g + 4, h * 16 + n:h * 16 + n + 1].to_broadcast([128, ni, 65])
                    if n == 0:
                        P.op("dve", lambda e, src=src, selv=selv, a2=a2: e.tensor_tensor(out=acc[a2], in0=src, in1=selv, op=ALU.mult),
                             reads=[rb[ob], r_sel], writes=[r_acc[a2]])
                    else:
                        t3 = cnt["tmp"] % 3
                        cnt["tmp"] += 1
                        P.op("dve", lambda e, src=src, selv=selv, t3=t3, ia=ia: e.tensor_tensor(out=tmp[t3][:, ia:4, :], in0=src, in1=selv, op=ALU.mult),
                             reads=[rb[ob], r_sel], writes=[r_tmp[t3]])
                        P.op("pool", lambda e, t3=t3, a2=a2, ia=ia: e.tensor_tensor(out=acc[a2][:, ia:4, :], in0=acc[a2][:, ia:4, :], in1=tmp[t3][:, ia:4, :], op=ALU.add),
                             reads=[r_tmp[t3], r_acc[a2]], writes=[r_acc[a2]])
                r2 = h % 2
                P.op("dve", lambda e, a2=a2, r2=r2: e.reciprocal(out=rc[r2].unsqueeze(2), in_=acc[a2][:, :, 64:65]), reads=[r_acc[a2]], writes=[r_rc[r2]])
                P.op("dve", lambda e, a2=a2, r2=r2, h=h, y2=y2: e.tensor_tensor(out=yst[y2][:, :, h * 64:(h + 1) * 64], in0=acc[a2][:, :, 0:64],
                                                                              in1=rc[r2].unsqueeze(2).to_broadcast([128, 4, 64]), op=ALU.mult),
                     reads=[r_acc[a2], r_rc[r2]], writes=[r_yst[y2]])
        for i in range(4):
            t = 4 * g + i
            P.dma("sp", D["Y"][t * 128:(t + 1) * 128, ycol:ycol + 256], yst[y2][:, i, :], reads=[r_yst[y2]], owner=r_yst[y2])
    P.end_phase()


def phase_D(C, l):
    nc, P, D = C.nc, C.P, C.D
    A = C.arena
    A.off = C.base_off
    bank, rb = C.bank, C.rb
    tri = A.alloc(128)
    upp = A.alloc(128)
    caus = A.alloc(128)
    ones = A.alloc(128)
    r_k = P.res("dconst", dma=True)
    P.dma("sp", tri, D["c_tri"], writes=[r_k], owner=r_k)
    P.dma("sp", upp, D["c_upp"], writes=[r_k], owner=r_k)
    P.dma("sp", caus, D["c_caus"], writes=[r_k], owner=r_k)
    P.op("pool", lambda e: e.memset(ones, 1.0), writes=[r_k])
    cw = A.alloc(32).rearrange("p (m j) -> p m j", m=8)
    cb = A.alloc(8)
    for j in range(4):
        P.dma("sp", cw[:, :, j], D["conv_w"][l, j].rearrange("(m p) -> p m", p=128), writes=[r_k], owner=r_k, allow_slow_non_contiguous=True)
    P.dma("sp", cb, D["conv_b"][l].rearrange("(m p) -> p m", p=128), writes=[r_k], owner=r_k, allow_slow_non_contiguous=True)
    dtb = A.alloc(8)
    alog = A.alloc(8)
    dsk = A.alloc(8)
    nw = A.alloc(512)
    P.dma("sp", dtb, bcast_rows(D["dt_bias"][l]), writes=[r_k], owner=r_k)
    P.dma("sp", alog, bcast_rows(D["a_log"][l]), writes=[r_k], owner=r_k)
    P.dma("sp", dsk, bcast_rows(D["d_skip"][l]), writes=[r_k], owner=r_k)
    P.dma("sp", nw, bcast_rows(D["ssm_norm_w"][l]), writes=[r_k], owner=r_k)
    dtr = A.alloc(256).rearrange("p (t h) -> p t h", t=NT)
    r_dtr = P.res("dtr", dma=True)
    dsrc = D["dtr"].rearrange("(t p) h -> p t h", p=128)
    for q4 in range(4):
        P.dma("sp", dtr[:, q4 * 8:(q4 + 1) * 8, :], dsrc[:, q4 * 8:(q4 + 1) * 8, :], writes=[r_dtr], owner=r_dtr)

    BT = A.alloc(2 * S, BF16).rearrange("p (g n) -> p g n", g=2)
    CT = A.alloc(2 * S, BF16).rearrange("p (g n) -> p g n", g=2)
    r_BT, r_CT = P.res("BT"), P.res("CT")
    xtok = A.alloc(NT * 512).rearrange("p (t c) -> p t c", t=NT)
    r_xtok = P.res("xtok")
    Btok = A.alloc(NT * 256, BF16).rearrange("p (t c) -> p t c", t=NT)
    r_Btok = P.res("Btok")
    mark = A.off
    xin = A.alloc(S + 8)
    r_xin = P.res("xin", dma=True)
    cacc = A.alloc(S)
    r_cacc = P.res("cacc")
    P.op("dve", lambda e: e.memset(xin[:, 0:8], 0.0), writes=[r_xin])

    tcnt = 0
    for m in range(8):
        P.dma("sp", xin[:, 3:3 + S], D["xbcT"][m * 128:(m + 1) * 128, :], writes=[r_xin], owner=r_xin)
        P.op("dve", lambda e, m=m: e.tensor_scalar(out=cacc, in0=xin[:, 3:3 + S], scalar1=cw[:, m, 3:4], scalar2=None, op0=ALU.mult),
             reads=[r_xin, r_k], writes=[r_cacc])
        P.op("dve", lambda e, m=m: e.scalar_tensor_tensor(out=cacc, in0=xin[:, 2:2 + S], scalar=cw[:, m, 2:3], in1=cacc, op0=ALU.mult, op1=ALU.add),
             reads=[r_xin, r_k, r_cacc], writes=[r_cacc])
        P.op("dve", lambda e, m=m: e.scalar_tensor_tensor(out=cacc, in0=xin[:, 1:1 + S], scalar=cw[:, m, 1:2], in1=cacc, op0=ALU.mult, op1=ALU.add),
             reads=[r_xin, r_k, r_cacc], writes=[r_cacc])
        P.op("dve", lambda e, m=m: e.scalar_tensor_tensor(out=cacc, in0=xin[:, 0:S], scalar=cw[:, m, 0:1], in1=cacc, op0=ALU.mult, op1=ALU.add),
             reads=[r_xin, r_k, r_cacc], writes=[r_cacc])
        if m < 4:
            P.op("act", lambda e, m=m: e.activation(out=cacc, in_=cacc, func=AF.Silu, bias=cb[:, m:m + 1]), reads=[r_cacc, r_k], writes=[r_cacc])
            for c0 in range(0, NT, 4):
                bk = tcnt % 2
                tcnt += 1
                for cc in range(4):
                    c = c0 + cc
                    P.op("pe", lambda e, c=c, cc=cc, bk=bk: e.transpose(out=bank[bk][:, cc * 128:(cc + 1) * 128], in_=cacc[:, c * 128:(c + 1) * 128], identity=C.ident_f),
                         reads=[r_cacc], writes=[rb[bk]])
                eng = "act" if (tcnt % 2) else "dve"
                if eng == "act":
                    P.op("act", lambda e, c0=c0, m=m, bk=bk: e.copy(out=xtok[:, c0:c0 + 4, m * 128:(m + 1) * 128], in_=bank[bk][:].rearrange("p (c n) -> p c n", c=4)),
                         reads=[rb[bk]], writes=[r_xtok])
                else:
                    P.op("dve", lambda e, c0=c0, m=m, bk=bk: e.tensor_copy(out=xtok[:, c0:c0 + 4, m * 128:(m + 1) * 128], in_=bank[bk][:].rearrange("p (c n) -> p c n", c=4)),
                         reads=[rb[bk]], writes=[r_xtok])
        elif m < 6:
            gg = m - 4
            P.op("act", lambda e, m=m, gg=gg: e.activation(out=BT[:, gg, :], in_=cacc, func=AF.Silu, bias=cb[:, m:m + 1]), reads=[r_cacc, r_k], writes=[r_BT])
            for c0 in range(0, NT, 8):
                bk = tcnt % 2
                tcnt += 1
                pb = bank[bk][:].bitcast(BF16)
                for cc in range(8):
                    c = c0 + cc
                    P.op("pe", lambda e, c=c, cc=cc, pb=pb, gg=gg: e.transpose(out=pb[:, cc * 128:(cc + 1) * 128], in_=BT[:, gg, c * 128:(c + 1) * 128], identity=C.ident_b),
                         reads=[r_BT], writes=[rb[bk]])
                P.op("act", lambda e, c0=c0, gg=gg, pb=pb: e.copy(out=Btok[:, c0:c0 + 8, gg * 128:(gg + 1) * 128], in_=pb.rearrange("p (c n) -> p c n", c=8)),
                     reads=[rb[bk]], writes=[r_Btok])
        else:
            gg = m - 6
            P.op("act", lambda e, m=m, gg=gg: e.activation(out=CT[:, gg, :], in_=cacc, func=AF.Silu, bias=cb[:, m:m + 1]), reads=[r_cacc, r_k], writes=[r_CT])
    P.barrier()
    A.off = mark

    W = 256
    dtx = A.alloc(W)
    t_ax = A.alloc(W)
    t_e = A.alloc(W)
    dt_all = A.alloc(W)
    aneg = A.alloc(8)
    da_all = A.alloc(W)
    ea_all = A.alloc(W)
    ds_all = A.alloc(W)
    cd_all = A.alloc(W)
    dtds = A.alloc(W)
    r_t = P.res("dtables")
    v3 = lambda ap: ap.rearrange("p (t h) -> p t h", t=NT)
    P.op("dve", lambda e: e.tensor_tensor(out=v3(dtx), in0=dtr, in1=dtb.unsqueeze(1).to_broadcast([128, NT, 8]), op=ALU.add), reads=[r_dtr, r_k], writes=[r_t])
    P.op("dve", lambda e: e.scalar_tensor_tensor(out=t_ax, in0=dtx, scalar=-1.0, in1=dtx, op0=ALU.mult, op1=ALU.min), reads=[r_t], writes=[r_t])
    P.op("act", lambda e: e.activation(out=t_e, in_=t_ax, func=AF.Exp), reads=[r_t], writes=[r_t])
    P.op("act", lambda e: e.activation(out=t_e, in_=t_e, func=AF.Ln, bias=C.one_t[:, 0:1]), reads=[r_t], writes=[r_t])
    P.op("dve", lambda e: e.tensor_scalar_max(out=t_ax, in0=dtx, scalar1=0.0), reads=[r_t], writes=[r_t])
    P.op("dve", lambda e: e.tensor_tensor(out=dt_all, in0=t_ax, in1=t_e, op=ALU.add), reads=[r_t], writes=[r_t])
    P.op("act", lambda e: e.activation(out=aneg, in_=alog, func=AF.Exp), reads=[r_k], writes=[r_t])
    P.op("dve", lambda e: e.tensor_scalar(out=aneg, in0=aneg, scalar1=-1.0, scalar2=None, op0=ALU.mult), reads=[r_t], writes=[r_t])
    P.op("dve", lambda e: e.tensor_tensor(out=v3(da_all), in0=v3(dt_all), in1=aneg.unsqueeze(1).to_broadcast([128, NT, 8]), op=ALU.mult), reads=[r_t], writes=[r_t])
    for (lhs, dst) in ((tri, ea_all), (upp, ds_all), (ones, cd_all)):
        P.op("pe", lambda e, lhs=lhs: e.matmul(bank[2][:, 0:W], lhsT=lhs, rhs=da_all, start=True, stop=True), reads=[r_t, r_k], writes=[rb[2]])
        P.op("act", lambda e, dst=dst: e.activation(out=dst, in_=bank[2][:, 0:W], func=AF.Exp), reads=[rb[2]], writes=[r_t])
    P.op("dve", lambda e: e.tensor_tensor(out=dtds, in0=dt_all, in1=ds_all, op=ALU.mult), reads=[r_t], writes=[r_t])

    rT = [A.alloc(512).rearrange("p (h n) -> p h n", h=4) for _ in range(2)]
    r_rT = [P.res("rT%d" % i) for i in range(2)]
    Lt = [A.alloc(512).rearrange("p (h n) -> p h n", h=4) for _ in range(2)]
    r_Lt = [P.res("Lt%d" % i) for i in range(2)]
    Gm = [A.alloc(128) for _ in range(2)]
    r_Gm = [P.res("Gm%d" % i) for i in range(2)]
    sc = [A.alloc(512, BF16).rearrange("p (h n) -> p h n", h=4) for _ in range(2)]
    r_sc = [P.res("sc%d" % i) for i in range(2)]
    xd = [A.alloc(256, BF16).rearrange("p (h n) -> p h n", h=4) for _ in range(2)]
    r_xd = [P.res("xd%d" % i) for i in range(2)]
    xdd = [A.alloc(256, BF16).rearrange("p (h n) -> p h n", h=4) for _ in range(2)]
    r_xdd = [P.res("xdd%d" % i) for i in range(2)]
    St = [A.alloc(256).rearrange("p (h n) -> p h n", h=4) for _ in range(2)]
    r_St = [P.res("St%d" % i) for i in range(2)]
    tS = [A.alloc(256).rearrange("p (h n) -> p h n", h=4) for _ in range(2)]
    r_tS = [P.res("tS%d" % i) for i in range(2)]
    Sbf = [A.alloc(256, BF16) for _ in range(2)]
    r_Sbf = [P.res("Sbf%d" % i) for i in range(2)]
    t1 = [A.alloc(256).rearrange("p (h n) -> p h n", h=4) for _ in range(2)]
    r_t1 = [P.res("t1%d" % i) for i in range(2)]
    t3 = [A.alloc(256).rearrange("p (h n) -> p h n", h=4) for _ in range(2)]
    r_t3 = [P.res("t3%d" % i) for i in range(2)]
    yg = [A.alloc(512) for _ in range(2)]
    r_yg = [P.res("yg%d" % i) for i in range(2)]
    zt = [A.alloc(512) for _ in range(2)]
    r_zt = [P.res("zt%d" % i, dma=True) for i in range(2)]
    junk = A.alloc(256)
    r_junk = P.res("junkD")
    nst = [A.alloc(8) for _ in range(2)]
    r_nst = [P.res("nst%d" % i) for i in range(2)]
    yo = [A.alloc(512, BF16) for _ in range(2)]
    r_yo = [P.res("yo%d" % i, dma=True) for i in range(2)]

    def b4(ap2d, c, g):
        return v3(ap2d)[:, c, g * 4:g * 4 + 4].unsqueeze(2).to_broadcast([128, 4, 64])

    P.dma("sp", zt[0], D["z"][0:128, :], writes=[r_zt[0]], owner=r_zt[0])
    it = 0
    for c in range(NT):
        c2 = c % 2
        if c + 1 < NT:
            P.dma("sp", zt[(c + 1) % 2], D["z"][(c + 1) * 128:(c + 2) * 128, :], writes=[r_zt[(c + 1) % 2]], owner=r_zt[(c + 1) % 2])
        csl = slice(c * 128, (c + 1) * 128)
        for g in range(2):
            i2 = it % 2
            it += 1
            bX, bY, bZ, bW = 0 + i2, 2 + i2, 4 + i2, 6 + i2
            xg = xtok[:, c, g * 256:(g + 1) * 256].rearrange("p (h n) -> p h n", h=4)
            P.op("dve", lambda e, i2=i2, c=c, g=g: e.tensor_tensor(out=rT[i2], in0=tri.unsqueeze(1).to_broadcast([128, 4, 128]),
                                                             in1=v3(da_all)[:, c, g * 4:g * 4 + 4].unsqueeze(2).to_broadcast([128, 4, 128]), op=ALU.mult),
                 reads=[r_k, r_t], writes=[r_rT[i2]])
            P.op("pe", lambda e, i2=i2, bX=bX: e.matmul(bank[bX][:], lhsT=upp, rhs=rT[i2].rearrange("p h n -> p (h n)"), start=True, stop=True),
                 reads=[r_k, r_rT[i2]], writes=[rb[bX]])
            P.op("act", lambda e, i2=i2, bX=bX: e.activation(out=Lt[i2].rearrange("p h n -> p (h n)"), in_=bank[bX][:], func=AF.Exp), reads=[rb[bX]], writes=[r_Lt[i2]])
            P.op("pe", lambda e, bY=bY, g=g, csl=csl: e.matmul(bank[bY][:, 0:128], lhsT=BT[:, g, csl], rhs=CT[:, g, csl], start=True, stop=True),
                 reads=[r_BT, r_CT], writes=[rb[bY]])
            P.op("dve", lambda e, i2=i2, bY=bY: e.tensor_tensor(out=Gm[i2], in0=bank[bY][:, 0:128], in1=caus, op=ALU.mult), reads=[rb[bY], r_k], writes=[r_Gm[i2]])
            P.op("dve", lambda e, i2=i2: e.tensor_tensor(out=sc[i2], in0=Lt[i2], in1=Gm[i2].unsqueeze(1).to_broadcast([128, 4, 128]), op=ALU.mult),
                 reads=[r_Lt[i2], r_Gm[i2]], writes=[r_sc[i2]])
            P.op("pool", lambda e, i2=i2, xg=xg, c=c, g=g: e.tensor_tensor(out=xd[i2], in0=xg, in1=b4(dt_all, c, g), op=ALU.mult), reads=[r_xtok, r_t], writes=[r_xd[i2]])
            P.op("pool", lambda e, i2=i2, xg=xg, c=c, g=g: e.tensor_tensor(out=xdd[i2], in0=xg, in1=b4(dtds, c, g), op=ALU.mult), reads=[r_xtok, r_t], writes=[r_xdd[i2]])
            for hh in range(4):
                P.op("pe", lambda e, hh=hh, i2=i2, bZ=bZ: e.matmul(bank[bZ][:, hh * 64:(hh + 1) * 64], lhsT=sc[i2][:, hh, :], rhs=xd[i2][:, hh, :], start=True, stop=True),
                     reads=[r_sc[i2], r_xd[i2]], writes=[rb[bZ]])
            if c > 0:
                P.op("pe", lambda e, bZ=bZ, g=g, csl=csl: e.matmul(bank[bZ][:, 256:512], lhsT=CT[:, g, csl], rhs=Sbf[g], start=True, stop=True),
                     reads=[r_CT, r_Sbf[g]], writes=[rb[bZ]])
            if c + 1 < NT:
                P.op("pe", lambda e, bW=bW, c=c, g=g, i2=i2: e.matmul(bank[bW][:, 0:256], lhsT=Btok[:, c, g * 128:(g + 1) * 128], rhs=xdd[i2].rearrange("p h n -> p (h n)"), start=True, stop=True),
                     reads=[r_Btok, r_xdd[i2]], writes=[rb[bW]])
                pw = bank[bW][:, 0:256].rearrange("p (h n) -> p h n", h=4)
                if c == 0:
                    P.op("dve", lambda e, g=g, pw=pw: e.tensor_copy(out=St[g], in_=pw), reads=[rb[bW]], writes=[r_St[g]])
                else:
                    P.op("dve", lambda e, g=g, c=c: e.tensor_tensor(out=tS[g], in0=St[g], in1=b4(cd_all, c, g), op=ALU.mult), reads=[r_St[g], r_t], writes=[r_tS[g]])
                    P.op("dve", lambda e, g=g, pw=pw: e.tensor_tensor(out=St[g], in0=tS[g], in1=pw, op=ALU.add), reads=[r_tS[g], rb[bW]], writes=[r_St[g]])
                P.op("act", lambda e, g=g: e.copy(out=Sbf[g], in_=St[g].rearrange("p h n -> p (h n)")), reads=[r_St[g]], writes=[r_Sbf[g]])
            pz = bank[bZ][:]
            if c > 0:
                P.op("dve", lambda e, i2=i2, pz=pz, c=c, g=g: e.tensor_tensor(out=t1[i2], in0=pz[:, 256:512].rearrange("p (h n) -> p h n", h=4), in1=b4(ea_all, c, g), op=ALU.mult),
                     reads=[rb[bZ], r_t], writes=[r_t1[i2]])
                P.op("dve", lambda e, i2=i2, pz=pz: e.tensor_tensor(out=t1[i2], in0=t1[i2], in1=pz[:, 0:256].rearrange("p (h n) -> p h n", h=4), op=ALU.add),
                     reads=[rb[bZ], r_t1[i2]], writes=[r_t1[i2]])
            else:
                P.op("dve", lambda e, i2=i2, pz=pz: e.tensor_copy(out=t1[i2], in_=pz[:, 0:256].rearrange("p (h n) -> p h n", h=4)), reads=[rb[bZ]], writes=[r_t1[i2]])
            P.op("pool", lambda e, i2=i2, xg=xg, g=g: e.tensor_tensor(out=t3[i2], in0=xg, in1=dsk[:, g * 4:g * 4 + 4].unsqueeze(2).to_broadcast([128, 4, 64]), op=ALU.mult),
                 reads=[r_xtok, r_k], writes=[r_t3[i2]])
            P.op("pool", lambda e, i2=i2, c2=c2, g=g: e.tensor_tensor(out=yg[c2][:, g * 256:(g + 1) * 256].rearrange("p (h n) -> p h n", h=4), in0=t1[i2], in1=t3[i2], op=ALU.add),
                 reads=[r_t1[i2], r_t3[i2]], writes=[r_yg[c2]])
        P.op("act", lambda e, c2=c2: e.activation(out=zt[c2], in_=zt[c2], func=AF.Silu), reads=[r_zt[c2]], writes=[r_zt[c2]])
        P.op("dve", lambda e, c2=c2: e.tensor_tensor(out=yg[c2], in0=yg[c2], in1=zt[c2], op=ALU.mult), reads=[r_yg[c2], r_zt[c2]], writes=[r_yg[c2]])
        for g in range(2):
            P.op("act", lambda e, c2=c2, g=g: e.activation(out=junk, in_=yg[c2][:, g * 256:(g + 1) * 256], func=AF.Square, accum_out=nst[c2][:, g:g + 1]),
                 reads=[r_yg[c2]], writes=[r_junk, r_nst[c2]])
        P.op("act", lambda e, c2=c2: e.activation(out=nst[c2][:, 2:4], in_=nst[c2][:, 0:2], func=AF.Sqrt, scale=1.0 / 256, bias=C.eps_t[:, 0:1]), reads=[r_nst[c2]], writes=[r_nst[c2]])
        P.op("dve", lambda e, c2=c2: e.reciprocal(out=nst[c2][:, 4:6], in_=nst[c2][:, 2:4]), reads=[r_nst[c2]], writes=[r_nst[c2]])
        P.op("dve", lambda e, c2=c2: e.tensor_tensor(out=yg[c2].rearrange("p (g n) -> p g n", g=2), in0=yg[c2].rearrange("p (g n) -> p g n", g=2),
                                                in1=nst[c2][:, 4:6].unsqueeze(2).to_broadcast([128, 2, 256]), op=ALU.mult),
             reads=[r_yg[c2], r_nst[c2]], writes=[r_yg[c2]])
        P.op("pool", lambda e, c2=c2: e.tensor_tensor(out=yo[c2], in0=yg[c2], in1=nw, op=ALU.mult), reads=[r_yg[c2], r_k], writes=[r_yo[c2]])
        P.dma("sp", D["Y"][c * 128:(c + 1) * 128, 256:768], yo[c2], reads=[r_yo[c2]], owner=r_yo[c2])
    P.end_phase()


def phase_E(C, l, x_src, x_dst):
    nc, P, D = C.nc, C.P, C.D
    A = C.arena
    A.off = C.base_off
    bank, rb = C.bank, C.rb
    w1 = A.alloc(8 * DFF, BF16).rearrange("p (k n) -> p k n", k=8)
    w2 = A.alloc(32 * DM, BF16).rearrange("p (f n) -> p f n", f=32)
    r_w1 = P.res("w1", dma=True)
    r_w2 = P.res("w2", dma=True)
    mark = A.off
    wout = A.alloc(8 * DM, BF16).rearrange("p (k n) -> p k n", k=8)
    r_wout = P.res("wout", dma=True)
    for k in range(8):
        P.dma("pool", wout[:, k, :], D["w_out"][l, k * 128:(k + 1) * 128, :], writes=[r_wout], owner=r_wout)
    for k in range(8):
        P.dma("pool", w1[:, k, :], D["w_mlp_in"][l, k * 128:(k + 1) * 128, :], writes=[r_w1], owner=r_w1)
    w2src = D["w_mlp_out"][l].rearrange("(f p) n -> p f n", p=128)
    for q in range(8):
        P.dma("pool", w2[:, q * 4:(q + 1) * 4, :], w2src[:, q * 4:(q + 1) * 4, :], writes=[r_w2], owner=r_w2)
    n2w = A.alloc(DM)
    r_n2w = P.res("n2w", dma=True)
    P.dma("sp", n2w, bcast_rows(D["norm2_w"][l]), writes=[r_n2w], owner=r_n2w)
    yt = [A.alloc(DM, BF16) for _ in range(2)]
    r_yt = [P.res("yt%d" % i, dma=True) for i in range(2)]
    xs = [A.alloc(DM) for _ in range(2)]
    r_xs = [P.res("xsE%d" % i, dma=True) for i in range(2)]
    yT = [A.alloc(DM, BF16).rearrange("p (k n) -> p k n", k=8) for _ in range(2)]
    r_yT = [P.res("yT%d" % i) for i in range(2)]
    x1t = [A.alloc(DM) for _ in range(2)]
    r_x1t = [P.res("x1t%d" % i, dma=True) for i in range(2)]
    junk = A.alloc(DM)
    r_junk = P.res("junkE")
    st = [A.alloc(8) for _ in range(2)]
    r_st = [P.res("stE%d" % i) for i in range(2)]
    h2b = [A.alloc(DM, BF16) for _ in range(2)]
    r_h2b = [P.res("h2b%d" % i) for i in range(2)]
    h2s = [A.alloc(DM, BF16).rearrange("p (k n) -> p k n", k=8) for _ in range(2)]
    r_h2s = [P.res("h2s%d" % i, dma=True) for i in range(2)]
    Yv = D["Y"].rearrange("(t p) c -> t p c", p=128)
    xv = x_src.rearrange("(t p) c -> t p c", p=128)
    x1v = D["x1"].rearrange("(t p) c -> t p c", p=128)
    h2Tv = D["h2T"].rearrange("(k p) s -> p k s", p=128)
    psA = bank[0][:].bitcast(BF16)
    psB = bank[3][:].bitcast(BF16)

    def loadE1(t):
        P.dma("sp", yt[t % 2], Yv[t], writes=[r_yt[t % 2]], owner=r_yt[t % 2])
        P.dma("sp", xs[t % 2], xv[t], writes=[r_xs[t % 2]], owner=r_xs[t % 2])

    loadE1(0)
    for t in range(NT):
        p2 = t % 2
        if t + 1 < NT:
            loadE1(t + 1)
        for k in range(8):
            P.op("pe", lambda e, k=k, p2=p2: e.transpose(out=psA[:, k * 128:(k + 1) * 128], in_=yt[p2][:, k * 128:(k + 1) * 128], identity=C.ident_b),
                 reads=[r_yt[p2]], writes=[rb[0]])
        P.op("act", lambda e, p2=p2: e.copy(out=yT[p2], in_=psA.rearrange("p (k n) -> p k n", k=8)), reads=[rb[0]], writes=[r_yT[p2]])
        for cg in range(2):
            bk = 1 + cg
            for k in range(8):
                P.op("pe", lambda e, k=k, cg=cg, bk=bk, p2=p2: e.matmul(bank[bk][:], lhsT=yT[p2][:, k, :], rhs=wout[:, k, cg * 512:(cg + 1) * 512], start=(k == 0), stop=(k == 7)),
                     reads=[r_yT[p2], r_wout], writes=[rb[bk]])
            P.op("dve", lambda e, cg=cg, bk=bk, p2=p2: e.tensor_tensor(out=x1t[p2][:, cg * 512:(cg + 1) * 512], in0=xs[p2][:, cg * 512:(cg + 1) * 512], in1=bank[bk][:], op=ALU.add),
                 reads=[rb[bk], r_xs[p2]], writes=[r_x1t[p2]])
        P.dma("sp", x1v[t], x1t[p2], reads=[r_x1t[p2]], owner=r_x1t[p2])
        P.op("act", lambda e, p2=p2: e.activation(out=junk, in_=x1t[p2], func=AF.Square, accum_out=st[p2][:, 0:1]), reads=[r_x1t[p2]], writes=[r_junk, r_st[p2]])
        P.op("act", lambda e, p2=p2: e.activation(out=st[p2][:, 1:2], in_=st[p2][:, 0:1], func=AF.Sqrt, scale=1.0 / DM, bias=C.eps_t[:, 0:1]), reads=[r_st[p2]], writes=[r_st[p2]])
        P.op("dve", lambda e, p2=p2: e.reciprocal(out=st[p2][:, 2:3], in_=st[p2][:, 1:2]), reads=[r_st[p2]], writes=[r_st[p2]])
        P.op("dve", lambda e, p2=p2: e.scalar_tensor_tensor(out=h2b[p2], in0=x1t[p2], scalar=st[p2][:, 2:3], in1=n2w, op0=ALU.mult, op1=ALU.mult),
             reads=[r_x1t[p2], r_st[p2], r_n2w], writes=[r_h2b[p2]])
        for k in range(8):
            P.op("pe", lambda e, k=k, p2=p2: e.transpose(out=psB[:, k * 128:(k + 1) * 128], in_=h2b[p2][:, k * 128:(k + 1) * 128], identity=C.ident_b),
                 reads=[r_h2b[p2]], writes=[rb[3]])
        P.op("act", lambda e, p2=p2: e.copy(out=h2s[p2], in_=psB.rearrange("p (k n) -> p k n", k=8)), reads=[rb[3]], writes=[r_h2s[p2]])
        P.dma("sp", h2Tv[:, :, t * 128:(t + 1) * 128], h2s[p2], reads=[r_h2s[p2]], owner=r_h2s[p2])
    P.barrier()

    A.off = mark
    hid = A.alloc(32 * 512, BF16).rearrange("p (f n) -> p f n", f=32)
    r_hid = [P.res("hid%d" % f) for f in range(32)]
    h2g = [A.alloc(8 * 512, BF16).rearrange("p (k n) -> p k n", k=8) for _ in range(2)]
    r_h2g = [P.res("h2g%d" % i, dma=True) for i in range(2)]
    rl = [A.alloc(512) for _ in range(2)]
    r_rl = [P.res("rl%d" % i) for i in range(2)]
    x1b = [A.alloc(DM) for _ in range(2)]
    r_x1b = [P.res("x1b%d" % i, dma=True) for i in range(2)]
    ot = [A.alloc(DM) for _ in range(2)]
    r_ot = [P.res("ot%d" % i, dma=True) for i in range(2)]
    ov = x_dst.rearrange("(t p) c -> t p c", p=128)
    P.dma("sp", h2g[0], h2Tv[:, :, 0:512], writes=[r_h2g[0]], owner=r_h2g[0])
    fcnt = 0
    for g in range(8):
        g2 = g % 2
        if g + 1 < 8:
            P.dma("sp", h2g[(g + 1) % 2], h2Tv[:, :, (g + 1) * 512:(g + 2) * 512], writes=[r_h2g[(g + 1) % 2]], owner=r_h2g[(g + 1) % 2])
        for f in range(32):
            bk = fcnt % 4
            r2 = fcnt % 2
            fcnt += 1
            for k in range(8):
                P.op("pe", lambda e, k=k, f=f, bk=bk, g2=g2: e.matmul(bank[bk][:], lhsT=w1[:, k, f * 128:(f + 1) * 128], rhs=h2g[g2][:, k, :], start=(k == 0), stop=(k == 7)),
                     reads=[r_w1, r_h2g[g2]], writes=[rb[bk]])
            P.op("act", lambda e, bk=bk, r2=r2: e.activation(out=rl[r2], in_=bank[bk][:], func=AF.Relu), reads=[rb[bk]], writes=[r_rl[r2]])
            P.op("dve", lambda e, bk=bk, r2=r2, f=f: e.tensor_tensor(out=hid[:, f, :], in0=rl[r2], in1=bank[bk][:], op=ALU.mult), reads=[rb[bk], r_rl[r2]], writes=[r_hid[f]])
        for i in range(4):
            t = 4 * g + i
            p2 = t % 2
            P.dma("sp", x1b[p2], x1v[t], writes=[r_x1b[p2]], owner=r_x1b[p2])
            for cg in range(2):
                bk = 4 + (2 * i + cg) % 4
                for f in range(32):
                    P.op("pe", lambda e, f=f, cg=cg, bk=bk, i=i: e.matmul(bank[bk][:], lhsT=hid[:, f, i * 128:(i + 1) * 128], rhs=w2[:, f, cg * 512:(cg + 1) * 512], start=(f == 0), stop=(f == 31)),
                         reads=[r_hid[f], r_w2], writes=[rb[bk]])
                P.op("dve", lambda e, cg=cg, bk=bk, p2=p2: e.tensor_tensor(out=ot[p2][:, cg * 512:(cg + 1) * 512], in0=x1b[p2][:, cg * 512:(cg + 1) * 512], in1=bank[bk][:], op=ALU.add),
                     reads=[rb[bk], r_x1b[p2]], writes=[r_ot[p2]])
            P.dma("sp", ov[t], ot[p2], reads=[r_ot[p2]], owner=r_ot[p2])
    P.end_phase()


def build(layers=(0, 1), phases=("A", "B", "C", "D", "E"), feed=(), expose=()):
    nc = bass.Bass("TRN2", target_bir_lowering=False)
    D = {}
    D["x"] = nc.dram_tensor("x", [S, DM], F32, kind="ExternalInput").ap()
    for n, shp in PARAM_SHAPES.items():
        D[n] = nc.dram_tensor(n, shp, F32, kind="ExternalInput").ap()
    for n, shp in CONST_SHAPES.items():
        D["c_" + n] = nc.dram_tensor("c_" + n, shp, F32, kind="ExternalInput").ap()
    for n, (shp, dt) in SCRATCH.items():
        kind = "ExternalInput" if n in feed else ("ExternalOutput" if n in expose else "Internal")
        D[n] = nc.dram_tensor(n, shp, dt, kind=kind).ap()
    D["out"] = nc.dram_tensor("out", [S, DM], F32, kind="ExternalOutput").ap()

    with ExitStack() as es:
        C = Ctx()
        C.nc, C.D = nc, D
        C.P = P = Prog(nc, es)
        C.arena = A = Arena(nc)
        C.bank = [nc.alloc_psum_tensor("bank%d" % i, [128, 512], F32) for i in range(8)]
        C.rb = [P.res("bank%d" % i) for i in range(8)]
        for r in C.rb:
            r.excl = True
        idf = A.alloc(128)
        C.ident_f = idf
        C.ident_b = A.alloc(128, BF16)
        C.eps_t = A.alloc(8)
        C.pastm = A.alloc(32)
        r_c = P.res("consts", dma=True)
        P.dma("sp", idf, D["c_ident"], writes=[r_c], owner=r_c)
        P.dma("sp", C.pastm, D["c_pastm"], writes=[r_c], owner=r_c)
        P.op("dve", lambda e: e.tensor_copy(out=C.ident_b, in_=idf), reads=[r_c], writes=[r_c])
        P.op("dve", lambda e: e.memset(C.eps_t, EPS), writes=[r_c])
        C.one_t = A.alloc(8)
        P.op("dve", lambda e: e.memset(C.one_t, 1.0), writes=[r_c])
        P.barrier()
        C.base_off = A.off

        for l in layers:
            x_src = D["x"] if l == 0 else D["xcur"]
            x_dst = D["xcur"] if l == 0 else D["out"]
            if "A" in phases:
                phase_A(C, l, x_src)
            if "B" in phases:
                phase_attn(C, l, "A")
            if "C" in phases:
                phase_attn(C, l, "C")
            if "D" in phases:
                phase_D(C, l)
            if "E" in phases:
                phase_E(C, l, x_src, x_dst)
        stats = P.emit()
    return nc, stats


def make_in_map(inputs, b, consts):
    m = {"x": np.ascontiguousarray(inputs["x"][b])}
    for n in PARAM_SHAPES:
        m[n] = np.ascontiguousarray(inputs[n])
    for n, v in consts.items():
        m["c_" + n] = v
    return m


def kernel(**inputs):
    inputs = {k: np.asarray(v) for k, v in inputs.items()}
    nc, _ = build()
    consts = host_consts()
    in_maps = [make_in_map(inputs, c % 4, consts) for c in range(8)]
    res = run_bass_kernel_spmd(nc, in_maps, core_ids=list(range(8)))
    out = np.stack([res.results[b]["out"] for b in range(4)], axis=0)
    return out.astype(np.float32)
```

```python
import math
from contextlib import ExitStack

import numpy as np
import ml_dtypes

import concourse.bass as bass
import concourse.mybir as mybir
from concourse.bass_utils import run_bass_kernel_spmd

F32 = mybir.dt.float32
BF16 = mybir.dt.bfloat16
ALU = mybir.AluOpType
AF = mybir.ActivationFunctionType
AX = mybir.AxisListType

ENGS = ("pe", "act", "dve", "pool", "sp")
S = 4096
NT = 32
DM = 1024
DIN = 3080
DFF = 4096
EPS = 1e-6
NEG = -1e30
SLOPES = [2.0 ** (-8.0 * i / 8) for i in range(1, 9)]
SL_A = SLOPES[0::2]
SL_C = SLOPES[1::2]


class Slot:
    __slots__ = ("sem", "cnt")

    def __init__(self, sem):
        self.sem = sem
        self.cnt = 0


class Res:
    __slots__ = ("name", "w", "r", "slot", "excl")

    def __init__(self, name, slot=None):
        self.name = name
        self.w = None
        self.r = []
        self.slot = slot
        self.excl = False


class Op:
    __slots__ = ("eng", "fn", "deps", "need_inc", "inc_idx", "kind")

    def __init__(self, eng, fn, deps, kind="c"):
        self.eng = eng
        self.fn = fn
        self.deps = deps
        self.need_inc = False
        self.inc_idx = 0
        self.kind = kind


class Prog:
    def __init__(self, nc, es, n_dma=84):
        self.nc = nc
        self.q = {e: [] for e in ENGS}
        self.esem = {e: es.enter_context(nc.semaphore("prog_" + e)) for e in ENGS if e != "sp"}
        self.slots = [Slot(es.enter_context(nc.semaphore("dq%d" % i))) for i in range(n_dma)]
        self.free = list(self.slots)
        self.phase = []

    def res(self, name, dma=False):
        slot = None
        if dma:
            slot = self.free.pop()
            self.phase.append(slot)
        return Res(name, slot)

    def _collect(self, reads, writes):
        deps = []
        for r in reads:
            if r.w is not None:
                deps.append(r.w)
        for w in writes:
            if w.w is not None:
                deps.append(w.w)
            deps.extend(w.r)
        for d in deps:
            if isinstance(d, Op):
                d.need_inc = True
        return deps

    def op(self, eng, fn, reads=(), writes=()):
        ex = [r for r in reads if r.excl]
        if ex:
            reads = [r for r in reads if not r.excl]
            writes = list(writes) + ex
        o = Op(eng, fn, self._collect(reads, writes))
        self.q[eng].append(o)
        for r in reads:
            r.r.append(o)
        for w in writes:
            w.w = o
            w.r = []
        return o

    def dma(self, queue, out, in_, reads=(), writes=(), owner=None, **kw):
        slot = owner.slot
        deps = self._collect(reads, writes)
        slot.cnt += 16
        tok = ("d", slot, slot.cnt)

        def fn(e, out=out, in_=in_, sem=slot.sem):
            return e.dma_start(out=out, in_=in_, **kw).then_inc(sem, 16)

        self.q[queue].append(Op(queue, fn, deps, kind="dma"))
        for r in reads:
            r.r.append(tok)
        for w in writes:
            w.w = tok
            w.r = []
        return tok

    def barrier(self):
        deps = []
        for e in ENGS:
            for o in reversed(self.q[e]):
                if o.kind == "c":
                    deps.append(o)
                    o.need_inc = True
                    break
        for s in self.slots:
            if s.cnt > 0:
                deps.append(("d", s, s.cnt))
        for e in ENGS:
            self.q[e].append(Op(e, None, list(deps), kind="wait"))

    def end_phase(self):
        self.barrier()
        self.free.extend(self.phase)
        self.phase = []

    def emit(self):
        nc = self.nc
        for e in ENGS:
            c = 0
            for o in self.q[e]:
                if o.kind == "c" and o.need_inc:
                    c += 1
                    o.inc_idx = c
        stats = {}

        def replay(ename, eng):
            observed = {}
            nwait = 0
            for o in self.q[ename]:
                need = {}
                for d in o.deps:
                    if isinstance(d, Op):
                        if d.eng == ename and ename == "pe":
                            continue
                        key = d.eng
                        sem = self.esem[d.eng]
                        val = d.inc_idx
                    else:
                        _, s, val = d
                        key = id(s)
                        sem = s.sem
                    if observed.get(key, 0) >= val:
                        continue
                    if key not in need or need[key][1] < val:
                        need[key] = (sem, val)
                for key, (sem, val) in need.items():
                    eng.wait_ge(sem, val)
                    observed[key] = val
                    nwait += 1
                if o.fn is not None:
                    ins = o.fn(eng)
                    if o.kind == "c" and o.need_inc:
                        ins.then_inc(self.esem[ename], 1)
            stats[ename] = (len(self.q[ename]), nwait)

        with nc.Block() as block:
            @block.tensor
            def _(e):
                replay("pe", e)

            @block.scalar
            def _(e):
                replay("act", e)

            @block.vector
            def _(e):
                replay("dve", e)

            @block.gpsimd
            def _(e):
                replay("pool", e)

            @block.sync
            def _(e):
                replay("sp", e)
        return stats


DT_SIZE = {F32: 4, BF16: 2}


class Arena:
    def __init__(self, nc):
        nbytes = (nc.sbuf_bytes_remaining - 2048) // 64 * 64
        self.words = nbytes // 4
        self.t = nc.alloc_sbuf_tensor("arena", [128, self.words], F32)
        self.off = 0
        self.peak = 0

    def alloc(self, cols, dtype=F32):
        words = (cols * DT_SIZE[dtype] + 3) // 4
        words = (words + 7) // 8 * 8
        assert self.off + words <= self.words, ("SBUF arena overflow", self.off * 4, words * 4, self.words * 4)
        ap = self.t[:, self.off:self.off + words]
        self.off += words
        self.peak = max(self.peak, self.off)
        if dtype != F32:
            ap = ap.bitcast(dtype)
        return ap[:, 0:cols]


def bcast_rows(ap1d, reps=1):
    n = ap1d.shape[0]
    if reps == 1:
        return bass.AP(ap1d.tensor, ap1d.offset, [[0, 128], [1, n]])
    return bass.AP(ap1d.tensor, ap1d.offset, [[0, 128], [0, reps], [1, n]])


def host_consts():
    c = {}
    c["ident"] = np.eye(128, dtype=np.float32)
    kl = np.arange(128)[:, None]
    def mk(nof, slopes, mult_fn):
        col = np.arange(nof * 128)[None, :]
        delta = (col - kl).astype(np.float64)
        out = np.zeros((4, 128, nof * 128), np.float32)
        for h, sl in enumerate(slopes):
            m = mult_fn(delta) * np.exp(-sl * np.maximum(delta, 0.0))
            out[h] = np.where(delta >= 0, m, 0.0).astype(np.float32)
        return out
    def multA(d):
        return ((d <= 128).astype(np.float64) + ((d % 4 == 0) & (d <= 512)) + ((d % 16 == 0) & (d <= 2048)))
    c["maskA"] = mk(17, SL_A, multA).astype(ml_dtypes.bfloat16)
    c["maskC"] = mk(32, SL_C, lambda d: np.ones_like(d)).astype(ml_dtypes.bfloat16)
    k = np.arange(128)[:, None]
    j = np.arange(128)[None, :]
    c["tri"] = (k <= j).astype(np.float32)
    c["upp"] = (k > j).astype(np.float32)
    c["caus"] = (j >= k).astype(np.float32)
    pm = np.zeros((128, 32), np.float32)
    pm[:, 16:] = NEG
    c["pastm"] = pm
    return c


CONST_SHAPES = {"ident": [128, 128], "maskA": [4, 128, 17 * 128], "maskC": [4, 128, 32 * 128],
                "tri": [128, 128], "upp": [128, 128], "caus": [128, 128], "pastm": [128, 32]}

PARAM_SHAPES = {
    "norm1_w": [2, 1024], "w_in": [2, 1024, 3080], "a_q_norm": [2, 64], "a_k_norm": [2, 64],
    "c_q_norm": [2, 64], "c_k_norm": [2, 64], "conv_w": [2, 4, 1024], "conv_b": [2, 1024],
    "dt_bias": [2, 8], "a_log": [2, 8], "d_skip": [2, 8], "ssm_norm_w": [2, 512],
    "w_out": [2, 1024, 1024], "norm2_w": [2, 1024], "w_mlp_in": [2, 1024, 4096],
    "w_mlp_out": [2, 4096, 1024],
}

SCRATCH = {
    "qkT": ([8, 128, S], BF16), "v_a": ([S, 256], BF16), "v_c": ([S, 256], BF16),
    "sel": ([S, 64], F32), "z": ([S, 512], F32), "xbcT": ([1024, S], F32), "dtr": ([S, 8], F32),
    "Y": ([S, 1024], BF16), "x1": ([S, 1024], F32), "h2T": ([1024, S], BF16), "xcur": ([S, 1024], F32),
}


class Ctx:
    pass


def phase_A(C, l, x_src):
    nc, P, D = C.nc, C.P, C.D
    A = C.arena
    A.off = C.base_off
    bank, rb = C.bank, C.rb
    win = A.alloc(8 * DIN, BF16).rearrange("p (k n) -> p k n", k=8)
    r_win = P.res("win", dma=True)
    for k in range(8):
        P.dma("pool", win[:, k, :], D["w_in"][l, k * 128:(k + 1) * 128, :], writes=[r_win], owner=r_win)
    n1w = A.alloc(1024)
    qkwA = A.alloc(512)
    qwC = A.alloc(256)
    kwC = A.alloc(256)
    r_small = P.res("smallA", dma=True)
    P.dma("sp", n1w, bcast_rows(D["norm1_w"][l]), writes=[r_small], owner=r_small)
    P.dma("sp", qkwA[:, 0:256].rearrange("p (r n) -> p r n", r=4), bcast_rows(D["a_q_norm"][l], 4), writes=[r_small], owner=r_small)
    P.dma("sp", qkwA[:, 256:512].rearrange("p (r n) -> p r n", r=4), bcast_rows(D["a_k_norm"][l], 4), writes=[r_small], owner=r_small)
    P.dma("sp", qwC.rearrange("p (r n) -> p r n", r=4), bcast_rows(D["c_q_norm"][l], 4), writes=[r_small], owner=r_small)
    P.dma("sp", kwC.rearrange("p (r n) -> p r n", r=4), bcast_rows(D["c_k_norm"][l], 4), writes=[r_small], owner=r_small)

    NXB = 3
    xs = [A.alloc(1024) for _ in range(NXB)]
    r_xs = [P.res("xs%d" % i, dma=True) for i in range(NXB)]
    junk = A.alloc(1024)
    r_junk = P.res("junk")
    st = [A.alloc(8) for _ in range(2)]
    r_st = [P.res("st%d" % i) for i in range(2)]
    hb = [A.alloc(1024, BF16) for _ in range(2)]
    r_hb = [P.res("hb%d" % i) for i in range(2)]
    hT = [A.alloc(8 * 512, BF16).rearrange("p (k n) -> p k n", k=8) for _ in range(2)]
    r_hT = [P.res("hT%d" % i) for i in range(2)]
    sq = [A.alloc(512) for _ in range(2)]
    r_sq = [P.res("sq%d" % i) for i in range(2)]
    hst = [A.alloc(48) for _ in range(2)]
    r_hst = [P.res("hst%d" % i) for i in range(2)]
    qn = [A.alloc(512) for _ in range(2)]
    r_qn = [P.res("qn%d" % i) for i in range(2)]
    qkbA = [A.alloc(512, BF16) for _ in range(2)]
    r_qkbA = [P.res("qkbA%d" % i) for i in range(2)]
    qkC = [A.alloc(512) for _ in range(2)]
    r_qkC = [P.res("qkC%d" % i) for i in range(2)]
    stA = [A.alloc(4 * 512, BF16).rearrange("p (j n) -> p j n", j=4) for _ in range(2)]
    r_stA = [P.res("stA%d" % i, dma=True) for i in range(2)]
    stC = [A.alloc(4 * 512, BF16).rearrange("p (j n) -> p j n", j=4) for _ in range(2)]
    r_stC = [P.res("stC%d" % i, dma=True) for i in range(2)]
    stv = [A.alloc(512, BF16) for _ in range(2)]
    r_stv = [P.res("stv%d" % i, dma=True) for i in range(2)]
    stz = [A.alloc(512) for _ in range(2)]
    r_stz = [P.res("stz%d" % i, dma=True) for i in range(2)]
    stdt = [A.alloc(8) for _ in range(2)]
    r_stdt = [P.res("stdt%d" % i, dma=True) for i in range(2)]
    stx = [A.alloc(512) for _ in range(2)]
    r_stx = [P.res("stx%d" % i, dma=True) for i in range(2)]
    qTf = [A.alloc(256).rearrange("p (j n) -> p j n", j=2) for _ in range(2)]
    r_qTf = [P.res("qTf%d" % i) for i in range(2)]
    ksum = [A.alloc(2) for _ in range(2)]
    r_ksum = [P.res("ksum%d" % i) for i in range(2)]
    kmT = A.alloc(32).rearrange("p (j n) -> p j n", j=2)
    r_kmT = P.res("kmT")
    gm = [A.alloc(64) for _ in range(2)]
    r_gm = [P.res("gm%d" % i) for i in range(2)]
    mx8 = [A.alloc(32) for _ in range(2)]
    r_mx8 = [P.res("mx8%d" % i) for i in range(2)]
    selt = [A.alloc(64) for _ in range(2)]
    r_selt = [P.res("selt%d" % i, dma=True) for i in range(2)]

    P.op("dve", lambda e: e.memset(kmT, 0.0), writes=[r_kmT])

    psT = bank[0][:].bitcast(BF16)
    ps6 = bank[6][:].bitcast(BF16)
    x_view = x_src.rearrange("(t p) d -> t p d", p=128)

    def load_x(t):
        b = t % NXB
        P.dma("sp", xs[b], x_view[t], writes=[r_xs[b]], owner=r_xs[b])

    TB = [1, 2, 4, 5]

    def norm_T(t, hTg, r_hTg, i):
        if t + 2 < NT:
            load_x(t + 2)
        b = t % NXB
        p2 = t % 2
        P.op("act", lambda e: e.activation(out=junk, in_=xs[b], func=AF.Square, accum_out=st[p2][:, 0:1]),
             reads=[r_xs[b]], writes=[r_junk, r_st[p2]])
        P.op("act", lambda e: e.activation(out=st[p2][:, 1:2], in_=st[p2][:, 0:1], func=AF.Sqrt, scale=1.0 / DM, bias=C.eps_t[:, 0:1]),
             reads=[r_st[p2]], writes=[r_st[p2]])
        P.op("dve", lambda e: e.reciprocal(out=st[p2][:, 2:3], in_=st[p2][:, 1:2]), reads=[r_st[p2]], writes=[r_st[p2]])
        P.op("dve", lambda e: e.scalar_tensor_tensor(out=hb[p2], in0=xs[b], scalar=st[p2][:, 2:3], in1=n1w, op0=ALU.mult, op1=ALU.mult),
             reads=[r_xs[b], r_st[p2], r_small], writes=[r_hb[p2]])
        for k in range(8):
            P.op("pe", lambda e, k=k: e.transpose(out=psT[:, k * 128:(k + 1) * 128], in_=hb[p2][:, k * 128:(k + 1) * 128], identity=C.ident_b),
                 reads=[r_hb[p2]], writes=[rb[0]])
        P.op("act", lambda e: e.copy(out=hTg[:, :, i * 128:(i + 1) * 128], in_=psT.rearrange("p (k n) -> p k n", k=8)),
             reads=[rb[0]], writes=[r_hTg])

    def head_norm(pb_in, bk, p2, nh, sqs, c_ss, c_s, c_r, qn_out):
        P.op("act", lambda e: e.activation(out=sqs, in_=pb_in, func=AF.Square), reads=[rb[bk]], writes=[r_sq[p2]])
        P.op("dve", lambda e: e.tensor_reduce(out=c_ss, in_=sqs.rearrange("p (h n) -> p h n", h=nh), axis=AX.X, op=ALU.add),
             reads=[r_sq[p2]], writes=[r_hst[p2]])
        P.op("act", lambda e: e.activation(out=c_s, in_=c_ss, func=AF.Sqrt, scale=1.0 / 64, bias=C.eps_t[:, 0:1]),
             reads=[r_hst[p2]], writes=[r_hst[p2]])
        P.op("dve", lambda e: e.reciprocal(out=c_r, in_=c_s), reads=[r_hst[p2]], writes=[r_hst[p2]])
        P.op("dve", lambda e: e.tensor_tensor(out=qn_out.rearrange("p (h n) -> p h n", h=nh), in0=pb_in.rearrange("p (h n) -> p h n", h=nh),
                                              in1=c_r.unsqueeze(2).to_broadcast([128, nh, 64]), op=ALU.mult),
             reads=[rb[bk], r_hst[p2]], writes=[r_qn[p2]])

    def stage1(t, hTg, r_hTg, i):
        p2 = t % 2
        tsl = slice(i * 128, (i + 1) * 128)
        for c in range(4):
            bk = TB[c]
            for k in range(8):
                P.op("pe", lambda e, k=k, c=c, bk=bk: e.matmul(bank[bk][:], lhsT=hTg[:, k, tsl], rhs=win[:, k, c * 512:(c + 1) * 512], start=(k == 0), stop=(k == 7)),
                     reads=[r_hTg, r_win], writes=[rb[bk]])
            pb = bank[bk][:]
            H = hst[p2]
            if c == 0:
                head_norm(pb, bk, p2, 8, sq[p2], H[:, 0:8], H[:, 16:24], H[:, 32:40], qn[p2])
                P.op("pool", lambda e: e.tensor_tensor(out=qkbA[p2], in0=qn[p2], in1=qkwA, op=ALU.mult),
                     reads=[r_qn[p2], r_small], writes=[r_qkbA[p2]])
            elif c == 1:
                P.op("act", lambda e, pb=pb: e.copy(out=stv[p2][:, 0:256], in_=pb[:, 0:256]), reads=[rb[bk]], writes=[r_stv[p2]])
                head_norm(pb[:, 256:512], bk, p2, 4, sq[p2][:, 0:256], H[:, 8:12], H[:, 24:28], H[:, 40:44], qn[p2][:, 0:256])
                P.op("pool", lambda e: e.tensor_tensor(out=qkC[p2][:, 0:256], in0=qn[p2][:, 0:256], in1=qwC, op=ALU.mult),
                     reads=[r_qn[p2], r_small], writes=[r_qkC[p2]])
            elif c == 2:
                P.op("act", lambda e, pb=pb: e.copy(out=stv[p2][:, 256:512], in_=pb[:, 256:512]), reads=[rb[bk]], writes=[r_stv[p2]])
                head_norm(pb[:, 0:256], bk, p2, 4, sq[p2][:, 256:512], H[:, 12:16], H[:, 28:32], H[:, 44:48], qn[p2][:, 256:512])
                P.op("pool", lambda e: e.tensor_tensor(out=qkC[p2][:, 256:512], in0=qn[p2][:, 256:512], in1=kwC, op=ALU.mult),
                     reads=[r_qn[p2], r_small], writes=[r_qkC[p2]])
                P.dma("sp", D["v_a"][t * 128:(t + 1) * 128, :], stv[p2][:, 0:256], reads=[r_stv[p2]], owner=r_stv[p2])
                P.dma("sp", D["v_c"][t * 128:(t + 1) * 128, :], stv[p2][:, 256:512], reads=[r_stv[p2]], owner=r_stv[p2])
            else:
                P.op("act", lambda e, pb=pb: e.copy(out=stz[p2], in_=pb), reads=[rb[bk]], writes=[r_stz[p2]])
                P.dma("sp", D["z"][t * 128:(t + 1) * 128, :], stz[p2], reads=[r_stz[p2]], owner=r_stz[p2])
        for k in range(8):
            P.op("pe", lambda e, k=k: e.matmul(bank[3][:, 0:8], lhsT=hTg[:, k, tsl], rhs=win[:, k, 3072:3080], start=(k == 0), stop=(k == 7)),
                 reads=[r_hTg, r_win], writes=[rb[3]])
        P.op("act", lambda e: e.copy(out=stdt[p2], in_=bank[3][:, 0:8]), reads=[rb[3]], writes=[r_stdt[p2]])
        P.dma("sp", D["dtr"][t * 128:(t + 1) * 128, :], stdt[p2], reads=[r_stdt[p2]], owner=r_stdt[p2])

    def stage2(t):
        p2 = t % 2
        g, i = t // 4, t % 4
        blk = t // 2
        tsl = slice(i * 128, (i + 1) * 128)
        for j in range(4):
            P.op("pe", lambda e, j=j: e.transpose(out=ps6[:, j * 128:(j + 1) * 128], in_=qkbA[p2][:, j * 128:(j + 1) * 128], identity=C.ident_b),
                 reads=[r_qkbA[p2]], writes=[rb[6]])
        P.op("act", lambda e: e.copy(out=stA[g % 2][:, :, tsl], in_=ps6[:, 0:512].rearrange("p (j n) -> p j n", j=4)),
             reads=[rb[6]], writes=[r_stA[g % 2]])
        for j in range(4):
            P.op("pe", lambda e, j=j: e.transpose(out=bank[7][:, j * 128:(j + 1) * 128], in_=qkC[p2][:, j * 128:(j + 1) * 128], identity=C.ident_f),
                 reads=[r_qkC[p2]], writes=[rb[7]])
        P.op("act", lambda e: e.copy(out=stC[g % 2][:, :, tsl], in_=bank[7][:].rearrange("p (j n) -> p j n", j=4)),
             reads=[rb[7]], writes=[r_stC[g % 2]])
        P.op("dve", lambda e: e.tensor_copy(out=qTf[p2], in_=bank[7][:, 0:256].rearrange("p (j n) -> p j n", j=2)),
             reads=[rb[7]], writes=[r_qTf[p2]])
        P.op("dve", lambda e: e.tensor_reduce(out=ksum[p2], in_=bank[7][:, 256:512].rearrange("p (j n) -> p j n", j=2), axis=AX.X, op=ALU.add),
             reads=[rb[7]], writes=[r_ksum[p2]])
        for h in range(4):
            hp, pr = h % 2, h // 2
            P.op("pe", lambda e, h=h, hp=hp, pr=pr: e.matmul(bank[3][:, 32 + h * 16:48 + h * 16], lhsT=qTf[p2][hp * 64:(hp + 1) * 64, pr, :],
                                                           rhs=kmT[hp * 64:(hp + 1) * 64, pr, :], start=True, stop=True),
                 reads=[r_qTf[p2], r_kmT], writes=[rb[3]])
        P.op("dve", lambda e: e.tensor_tensor(out=gm[p2].rearrange("p (h n) -> p h n", h=4), in0=bank[3][:, 32:96].rearrange("p (h n) -> p h n", h=4),
                                              in1=C.pastm[:, 16 - blk:32 - blk].unsqueeze(1).to_broadcast([128, 4, 16]), op=ALU.add),
             reads=[rb[3]], writes=[r_gm[p2]])
        for h in range(4):
            P.op("dve", lambda e, h=h: e.max(out=mx8[p2][:, h * 8:(h + 1) * 8], in_=gm[p2][:, h * 16:(h + 1) * 16]),
                 reads=[r_gm[p2]], writes=[r_mx8[p2]])
        P.op("dve", lambda e: e.tensor_tensor(out=selt[p2].rearrange("p (h n) -> p h n", h=4), in0=gm[p2].rearrange("p (h n) -> p h n", h=4),
                                              in1=mx8[p2].rearrange("p (h n) -> p h n", h=4)[:, :, 2:3].to_broadcast([128, 4, 16]), op=ALU.is_ge),
             reads=[r_gm[p2], r_mx8[p2]], writes=[r_selt[p2]])
        P.op("dve", lambda e: e.memset(selt[p2].rearrange("p (h n) -> p h n", h=4)[:, :, blk:blk + 1], 1.0),
             reads=[], writes=[r_selt[p2]])
        P.dma("sp", D["sel"][t * 128:(t + 1) * 128, :], selt[p2], reads=[r_selt[p2]], owner=r_selt[p2])
        P.op("dve", lambda e: e.scalar_tensor_tensor(out=kmT[:, :, blk], in0=ksum[p2], scalar=1.0 / 256, in1=kmT[:, :, blk], op0=ALU.mult, op1=ALU.add),
             reads=[r_ksum[p2], r_kmT], writes=[r_kmT])
        if i == 3:
            for j in range(4):
                P.dma("sp", D["qkT"][j][:, g * 512:(g + 1) * 512], stA[g % 2][:, j, :], reads=[r_stA[g % 2]], owner=r_stA[g % 2])
                P.dma("sp", D["qkT"][4 + j][:, g * 512:(g + 1) * 512], stC[g % 2][:, j, :], reads=[r_stC[g % 2]], owner=r_stC[g % 2])

    load_x(0)
    load_x(1)
    for i in range(4):
        norm_T(i, hT[0], r_hT[0], i)
    prev = None
    xcnt = 0
    for g in range(8):
        hTg, r_hTg = hT[g % 2], r_hT[g % 2]
        for i in range(4):
            t = 4 * g + i
            stage1(t, hTg, r_hTg, i)
            if prev is not None:
                stage2(prev)
            prev = t
            if g + 1 < 8:
                norm_T(4 * (g + 1) + i, hT[(g + 1) % 2], r_hT[(g + 1) % 2], i)
        for m in range(8):
            bk = TB[xcnt % 4]
            xcnt += 1
            for k in range(8):
                P.op("pe", lambda e, k=k, m=m, bk=bk, hTg=hTg: e.matmul(bank[bk][:], lhsT=win[:, k, 2048 + m * 128:2048 + (m + 1) * 128], rhs=hTg[:, k, :], start=(k == 0), stop=(k == 7)),
                     reads=[r_hTg, r_win], writes=[rb[bk]])
            P.op("act", lambda e, bk=bk, m=m: e.copy(out=stx[m % 2], in_=bank[bk][:]), reads=[rb[bk]], writes=[r_stx[m % 2]])
            P.dma("sp", D["xbcT"][m * 128:(m + 1) * 128, g * 512:(g + 1) * 512], stx[m % 2], reads=[r_stx[m % 2]], owner=r_stx[m % 2])
    stage2(prev)
    P.end_phase()


def phase_attn(C, l, kind):
    nc, P, D = C.nc, C.P, C.D
    A = C.arena
    A.off = C.base_off
    bank, rb = C.bank, C.rb
    isA = kind == "A"
    nof = 17 if isA else 32
    qi0, ki0 = (0, 2) if isA else (4, 6)
    ycol = 0 if isA else 768
    qT = [A.alloc(S, BF16) for _ in range(2)]
    kT = [A.alloc(S, BF16) for _ in range(2)]
    r_qk = P.res("qk", dma=True)
    for p in range(2):
        P.dma("sp", qT[p], D["qkT"][qi0 + p], writes=[r_qk], owner=r_qk)
        P.dma("sp", kT[p], D["qkT"][ki0 + p], writes=[r_qk], owner=r_qk)
    V = A.alloc(NT * 4 * 65, BF16).rearrange("p (t h e) -> p t h e", t=NT, h=4)
    r_V = P.res("V", dma=True)
    vsrc = D["v_a" if isA else "v_c"].rearrange("(t p) (h e) -> p t h e", p=128, h=4)
    for t in range(NT):
        P.dma("sp", V[:, t, :, 0:64], vsrc[:, t], writes=[r_V], owner=r_V)
    P.op("pool", lambda e: e.memset(V[:, :, :, 64:65], 1.0), writes=[r_V])
    mask = A.alloc(4 * nof * 128, BF16).rearrange("p (h n) -> p h n", h=4)
    r_mask = P.res("mask", dma=True)
    msrc = D["c_maskA" if isA else "c_maskC"]
    for h in range(4):
        P.dma("sp", mask[:, h, :], msrc[h], writes=[r_mask], owner=r_mask)
    if not isA:
        selall = A.alloc(NT * 64).rearrange("p (t c) -> p t c", t=NT)
        r_sel = P.res("selall", dma=True)
        ssrc = D["sel"].rearrange("(t p) c -> p t c", p=128)
        for q4 in range(4):
            P.dma("sp", selall[:, q4 * 8:(q4 + 1) * 8, :], ssrc[:, q4 * 8:(q4 + 1) * 8, :], writes=[r_sel], owner=r_sel)
        acc = [A.alloc(4 * 65).rearrange("p (i e) -> p i e", i=4) for _ in range(2)]
        r_acc = [P.res("acc%d" % i) for i in range(2)]
        tmp = [A.alloc(4 * 65).rearrange("p (i e) -> p i e", i=4) for _ in range(3)]
        r_tmp = [P.res("tmp%d" % i) for i in range(3)]
    NSB = 4
    POOL_EVERY = 10 ** 9
    ex = [A.alloc(512, BF16) for _ in range(NSB)]
    r_ex = [P.res("ex%d" % i) for i in range(NSB)]
    pT = [A.alloc(512, BF16) for _ in range(NSB)]
    r_pT = [P.res("pT%d" % i) for i in range(NSB)]
    rc = [A.alloc(4) for _ in range(2)]
    r_rc = [P.res("rc%d" % i) for i in range(2)]
    yst = [A.alloc(4 * 256, BF16).rearrange("p (i c) -> p i c", i=4) for _ in range(2)]
    r_yst = [P.res("yst%d" % i, dma=True) for i in range(2)]

    cnt = {"step": 0, "ob": 0, "tmp": 0}

    def emit_qk(h, j, i0, i1):
        sb = cnt["step"] % NSB
        cnt["step"] += 1
        pr, hp = h // 2, h % 2
        psl = slice(hp * 64, hp * 64 + 64)
        N = (i1 - i0 + 1) * 128
        P.op("pe", lambda e: e.matmul(bank[sb][:, 0:N], lhsT=kT[pr][psl, j * 128:(j + 1) * 128], rhs=qT[pr][psl, i0 * 128:(i1 + 1) * 128], start=True, stop=True),
             reads=[r_qk], writes=[rb[sb]])
        P.op("act", lambda e: e.activation(out=ex[sb][:, 0:N], in_=bank[sb][:, 0:N], func=AF.Exp, scale=0.125), reads=[rb[sb]], writes=[r_ex[sb]])
        meng = "pool" if (cnt["step"] % POOL_EVERY == 0) else "dve"
        P.op(meng, lambda e: e.tensor_tensor(out=pT[sb][:, 0:N], in0=ex[sb][:, 0:N], in1=mask[:, h, (i0 - j) * 128:(i1 - j + 1) * 128], op=ALU.mult),
             reads=[r_ex[sb], r_mask], writes=[r_pT[sb]])
        return sb

    def emit_pv(sb, h, j, i0, i1, g, ob, started):
        for i in range(i0, i1 + 1):
            st = not started[0]
            started[0] = True
            c0 = (i - 4 * g) * 65
            P.op("pe", lambda e, i=i, st=st, c0=c0: e.matmul(bank[ob][:, c0:c0 + 65], lhsT=pT[sb][:, (i - i0) * 128:(i - i0 + 1) * 128], rhs=V[:, j, h, :],
                                                           start=st, stop=(j == i), skip_group_check=True),
                 reads=[r_pT[sb], r_V], writes=[rb[ob]])

    def store_y(g, y2):
        for i in range(4):
            t = 4 * g + i
            P.dma("sp", D["Y"][t * 128:(t + 1) * 128, ycol:ycol + 256], yst[y2][:, i, :], reads=[r_yst[y2]], owner=r_yst[y2])

    pend = []

    def flush(n_keep):
        while len(pend) > n_keep:
            args, post = pend.pop(0)
            emit_pv(*args)
            if post is not None:
                post()

    def run_steps(steps, h, g, ob, post):
        started = [False]
        for si, (j, i0, i1) in enumerate(steps):
            sb = emit_qk(h, j, i0, i1)
            pend.append(((sb, h, j, i0, i1, g, ob, started), post if si == len(steps) - 1 else None))
            flush(NSB - 2)

    for g in range(8):
        y2 = g % 2
        for h in range(4):
            if isA:
                ob = 4 + cnt["ob"] % 2
                cnt["ob"] += 1
                steps = []
                for j in range(max(0, 4 * g - 16), 4 * g + 4):
                    i0, i1 = max(4 * g, j), min(4 * g + 3, j + 16)
                    if i0 <= i1:
                        steps.append((j, i0, i1))
                def postA(ob=ob, h=h, y2=y2, g=g, r2=cnt["ob"] % 2):
                    src = bank[ob][:, 0:260].rearrange("p (i e) -> p i e", i=4)
                    P.op("dve", lambda e: e.reciprocal(out=rc[r2].unsqueeze(2), in_=src[:, :, 64:65]), reads=[rb[ob]], writes=[r_rc[r2]])
                    P.op("dve", lambda e: e.tensor_tensor(out=yst[y2][:, :, h * 64:(h + 1) * 64], in0=src[:, :, 0:64],
                                                          in1=rc[r2].unsqueeze(2).to_broadcast([128, 4, 64]), op=ALU.mult),
                         reads=[rb[ob], r_rc[r2]], writes=[r_yst[y2]])
                    if h == 3:
                        store_y(g, y2)
                run_steps(steps, h, g, ob, postA)
            else:
                a2 = h % 2
                for n in range(0, 2 * g + 2):
                    ob = 4 + cnt["ob"] % 3
                    cnt["ob"] += 1
                    steps = []
                    for j in (2 * n, 2 * n + 1):
                        i0, i1 = max(4 * g, j), 4 * g + 3
                        if i0 <= i1:
                            steps.append((j, i0, i1))
                    def postC(ob=ob, h=h, y2=y2, g=g, n=n, a2=a2, last=(n == 2 * g + 1)):
                        ia = max(4 * g, 2 * n) - 4 * g
                        ni = 4 - ia
                        src = bank[ob][:, 0:260].rearrange("p (i e) -> p i e", i=4)[:, ia:4, :]
                        selv = selall[:, 4 * g + ia:4 * g + 4, h * 16 + n:h * 16 + n + 1].to_broadcast([128, ni, 65])
                        if n == 0:
                            P.op("dve", lambda e: e.tensor_tensor(out=acc[a2], in0=src, in1=selv, op=ALU.mult),
                                 reads=[rb[ob], r_sel], writes=[r_acc[a2]])
                        else:
                            t3 = cnt["tmp"] % 3
                            cnt["tmp"] += 1
                            P.op("dve", lambda e: e.tensor_tensor(out=tmp[t3][:, ia:4, :], in0=src, in1=selv, op=ALU.mult),
                                 reads=[rb[ob], r_sel], writes=[r_tmp[t3]])
                            P.op("pool", lambda e: e.tensor_tensor(out=acc[a2][:, ia:4, :], in0=acc[a2][:, ia:4, :], in1=tmp[t3][:, ia:4, :], op=ALU.add),
                                 reads=[r_tmp[t3], r_acc[a2]], writes=[r_acc[a2]])
                        if last:
                            r2 = h % 2
                            P.op("dve", lambda e: e.reciprocal(out=rc[r2].unsqueeze(2), in_=acc[a2][:, :, 64:65]), reads=[r_acc[a2]], writes=[r_rc[r2]])
                            P.op("dve", lambda e: e.tensor_tensor(out=yst[y2][:, :, h * 64:(h + 1) * 64], in0=acc[a2][:, :, 0:64],
                                                                  in1=rc[r2].unsqueeze(2).to_broadcast([128, 4, 64]), op=ALU.mult),
                                 reads=[r_acc[a2], r_rc[r2]], writes=[r_yst[y2]])
                            if h == 3:
                                store_y(g, y2)
                    run_steps(steps, h, g, ob, postC)
    flush(0)
    P.end_phase()


def phase_D(C, l):
    nc, P, D = C.nc, C.P, C.D
    A = C.arena
    A.off = C.base_off
    bank, rb = C.bank, C.rb
    tri = A.alloc(128)
    upp = A.alloc(128)
    caus = A.alloc(128)
    ones = A.alloc(128)
    r_k = P.res("dconst", dma=True)
    P.dma("sp", tri, D["c_tri"], writes=[r_k], owner=r_k)
    P.dma("sp", upp, D["c_upp"], writes=[r_k], owner=r_k)
    P.dma("sp", caus, D["c_caus"], writes=[r_k], owner=r_k)
    P.op("pool", lambda e: e.memset(ones, 1.0), writes=[r_k])
    cw = A.alloc(32).rearrange("p (m j) -> p m j", m=8)
    cb = A.alloc(8)
    for j in range(4):
        P.dma("sp", cw[:, :, j], D["conv_w"][l, j].rearrange("(m p) -> p m", p=128), writes=[r_k], owner=r_k, allow_slow_non_contiguous=True)
    P.dma("sp", cb, D["conv_b"][l].rearrange("(m p) -> p m", p=128), writes=[r_k], owner=r_k, allow_slow_non_contiguous=True)
    dtb = A.alloc(8)
    alog = A.alloc(8)
    dsk = A.alloc(8)
    nw = A.alloc(512)
    P.dma("sp", dtb, bcast_rows(D["dt_bias"][l]), writes=[r_k], owner=r_k)
    P.dma("sp", alog, bcast_rows(D["a_log"][l]), writes=[r_k], owner=r_k)
    P.dma("sp", dsk, bcast_rows(D["d_skip"][l]), writes=[r_k], owner=r_k)
    P.dma("sp", nw, bcast_rows(D["ssm_norm_w"][l]), writes=[r_k], owner=r_k)
    dtr = A.alloc(256).rearrange("p (t h) -> p t h", t=NT)
    r_dtr = P.res("dtr", dma=True)
    dsrc = D["dtr"].rearrange("(t p) h -> p t h", p=128)
    for q4 in range(4):
        P.dma("sp", dtr[:, q4 * 8:(q4 + 1) * 8, :], dsrc[:, q4 * 8:(q4 + 1) * 8, :], writes=[r_dtr], owner=r_dtr)

    BT = A.alloc(2 * S, BF16).rearrange("p (g n) -> p g n", g=2)
    CT = A.alloc(2 * S, BF16).rearrange("p (g n) -> p g n", g=2)
    r_BT, r_CT = P.res("BT"), P.res("CT")
    xtok = A.alloc(NT * 512).rearrange("p (t c) -> p t c", t=NT)
    r_xtok = P.res("xtok")
    Btok = A.alloc(NT * 256, BF16).rearrange("p (t c) -> p t c", t=NT)
    r_Btok = P.res("Btok")
    mark = A.off
    xin = A.alloc(S + 8)
    r_xin = P.res("xin", dma=True)
    cacc = A.alloc(S)
    r_cacc = P.res("cacc")
    P.op("dve", lambda e: e.memset(xin[:, 0:8], 0.0), writes=[r_xin])

    tcnt = 0
    for m in range(8):
        P.dma("sp", xin[:, 3:3 + S], D["xbcT"][m * 128:(m + 1) * 128, :], writes=[r_xin], owner=r_xin)
        P.op("dve", lambda e, m=m: e.tensor_scalar(out=cacc, in0=xin[:, 3:3 + S], scalar1=cw[:, m, 3:4], scalar2=None, op0=ALU.mult),
             reads=[r_xin, r_k], writes=[r_cacc])
        P.op("dve", lambda e, m=m: e.scalar_tensor_tensor(out=cacc, in0=xin[:, 2:2 + S], scalar=cw[:, m, 2:3], in1=cacc, op0=ALU.mult, op1=ALU.add),
             reads=[r_xin, r_k, r_cacc], writes=[r_cacc])
        P.op("dve", lambda e, m=m: e.scalar_tensor_tensor(out=cacc, in0=xin[:, 1:1 + S], scalar=cw[:, m, 1:2], in1=cacc, op0=ALU.mult, op1=ALU.add),
             reads=[r_xin, r_k, r_cacc], writes=[r_cacc])
        P.op("dve", lambda e, m=m: e.scalar_tensor_tensor(out=cacc, in0=xin[:, 0:S], scalar=cw[:, m, 0:1], in1=cacc, op0=ALU.mult, op1=ALU.add),
             reads=[r_xin, r_k, r_cacc], writes=[r_cacc])
        if m < 4:
            P.op("act", lambda e, m=m: e.activation(out=cacc, in_=cacc, func=AF.Silu, bias=cb[:, m:m + 1]), reads=[r_cacc, r_k], writes=[r_cacc])
            for c0 in range(0, NT, 4):
                bk = tcnt % 2
                tcnt += 1
                for cc in range(4):
                    c = c0 + cc
                    P.op("pe", lambda e, c=c, cc=cc, bk=bk: e.transpose(out=bank[bk][:, cc * 128:(cc + 1) * 128], in_=cacc[:, c * 128:(c + 1) * 128], identity=C.ident_f),
                         reads=[r_cacc], writes=[rb[bk]])
                eng = "act" if (tcnt % 2) else "dve"
                if eng == "act":
                    P.op("act", lambda e, c0=c0, m=m, bk=bk: e.copy(out=xtok[:, c0:c0 + 4, m * 128:(m + 1) * 128], in_=bank[bk][:].rearrange("p (c n) -> p c n", c=4)),
                         reads=[rb[bk]], writes=[r_xtok])
                else:
                    P.op("dve", lambda e, c0=c0, m=m, bk=bk: e.tensor_copy(out=xtok[:, c0:c0 + 4, m * 128:(m + 1) * 128], in_=bank[bk][:].rearrange("p (c n) -> p c n", c=4)),
                         reads=[rb[bk]], writes=[r_xtok])
        elif m < 6:
            gg = m - 4
            P.op("act", lambda e, m=m, gg=gg: e.activation(out=BT[:, gg, :], in_=cacc, func=AF.Silu, bias=cb[:, m:m + 1]), reads=[r_cacc, r_k], writes=[r_BT])
            for c0 in range(0, NT, 8):
                bk = tcnt % 2
                tcnt += 1
                pb = bank[bk][:].bitcast(BF16)
                for cc in range(8):
                    c = c0 + cc
                    P.op("pe", lambda e, c=c, cc=cc, pb=pb, gg=gg: e.transpose(out=pb[:, cc * 128:(cc + 1) * 128], in_=BT[:, gg, c * 128:(c + 1) * 128], identity=C.ident_b),
                         reads=[r_BT], writes=[rb[bk]])
                P.op("act", lambda e, c0=c0, gg=gg, pb=pb: e.copy(out=Btok[:, c0:c0 + 8, gg * 128:(gg + 1) * 128], in_=pb.rearrange("p (c n) -> p c n", c=8)),
                     reads=[rb[bk]], writes=[r_Btok])
        else:
            gg = m - 6
            P.op("act", lambda e, m=m, gg=gg: e.activation(out=CT[:, gg, :], in_=cacc, func=AF.Silu, bias=cb[:, m:m + 1]), reads=[r_cacc, r_k], writes=[r_CT])
    P.barrier()
    A.off = mark

    W = 256
    dtx = A.alloc(W)
    t_ax = A.alloc(W)
    t_e = A.alloc(W)
    dt_all = A.alloc(W)
    aneg = A.alloc(8)
    da_all = A.alloc(W)
    ea_all = A.alloc(W)
    ds_all = A.alloc(W)
    cd_all = A.alloc(W)
    dtds = A.alloc(W)
    r_t = P.res("dtables")
    v3 = lambda ap: ap.rearrange("p (t h) -> p t h", t=NT)
    P.op("dve", lambda e: e.tensor_tensor(out=v3(dtx), in0=dtr, in1=dtb.unsqueeze(1).to_broadcast([128, NT, 8]), op=ALU.add), reads=[r_dtr, r_k], writes=[r_t])
    P.op("dve", lambda e: e.scalar_tensor_tensor(out=t_ax, in0=dtx, scalar=-1.0, in1=dtx, op0=ALU.mult, op1=ALU.min), reads=[r_t], writes=[r_t])
    P.op("act", lambda e: e.activation(out=t_e, in_=t_ax, func=AF.Exp), reads=[r_t], writes=[r_t])
    P.op("act", lambda e: e.activation(out=t_e, in_=t_e, func=AF.Ln, bias=C.one_t[:, 0:1]), reads=[r_t], writes=[r_t])
    P.op("dve", lambda e: e.tensor_scalar_max(out=t_ax, in0=dtx, scalar1=0.0), reads=[r_t], writes=[r_t])
    P.op("dve", lambda e: e.tensor_tensor(out=dt_all, in0=t_ax, in1=t_e, op=ALU.add), reads=[r_t], writes=[r_t])
    P.op("act", lambda e: e.activation(out=aneg, in_=alog, func=AF.Exp), reads=[r_k], writes=[r_t])
    P.op("dve", lambda e: e.tensor_scalar(out=aneg, in0=aneg, scalar1=-1.0, scalar2=None, op0=ALU.mult), reads=[r_t], writes=[r_t])
    P.op("dve", lambda e: e.tensor_tensor(out=v3(da_all), in0=v3(dt_all), in1=aneg.unsqueeze(1).to_broadcast([128, NT, 8]), op=ALU.mult), reads=[r_t], writes=[r_t])
    for (lhs, dst) in ((tri, ea_all), (upp, ds_all), (ones, cd_all)):
        P.op("pe", lambda e, lhs=lhs: e.matmul(bank[2][:, 0:W], lhsT=lhs, rhs=da_all, start=True, stop=True), reads=[r_t, r_k], writes=[rb[2]])
        P.op("act", lambda e, dst=dst: e.activation(out=dst, in_=bank[2][:, 0:W], func=AF.Exp), reads=[rb[2]], writes=[r_t])
    P.op("dve", lambda e: e.tensor_tensor(out=dtds, in0=dt_all, in1=ds_all, op=ALU.mult), reads=[r_t], writes=[r_t])

    rT = [A.alloc(512).rearrange("p (h n) -> p h n", h=4) for _ in range(2)]
    r_rT = [P.res("rT%d" % i) for i in range(2)]
    Lt = [A.alloc(512).rearrange("p (h n) -> p h n", h=4) for _ in range(2)]
    r_Lt = [P.res("Lt%d" % i) for i in range(2)]
    Gm = [A.alloc(128) for _ in range(2)]
    r_Gm = [P.res("Gm%d" % i) for i in range(2)]
    sc = [A.alloc(512, BF16).rearrange("p (h n) -> p h n", h=4) for _ in range(2)]
    r_sc = [P.res("sc%d" % i) for i in range(2)]
    xd = [A.alloc(256, BF16).rearrange("p (h n) -> p h n", h=4) for _ in range(2)]
    r_xd = [P.res("xd%d" % i) for i in range(2)]
    xdd = [A.alloc(256, BF16).rearrange("p (h n) -> p h n", h=4) for _ in range(2)]
    r_xdd = [P.res("xdd%d" % i) for i in range(2)]
    St = [A.alloc(256).rearrange("p (h n) -> p h n", h=4) for _ in range(2)]
    r_St = [P.res("St%d" % i) for i in range(2)]
    tS = [A.alloc(256).rearrange("p (h n) -> p h n", h=4) for _ in range(2)]
    r_tS = [P.res("tS%d" % i) for i in range(2)]
    Sbf = [A.alloc(256, BF16) for _ in range(2)]
    r_Sbf = [P.res("Sbf%d" % i) for i in range(2)]
    t1 = [A.alloc(256).rearrange("p (h n) -> p h n", h=4) for _ in range(2)]
    r_t1 = [P.res("t1%d" % i) for i in range(2)]
    t3 = [A.alloc(256).rearrange("p (h n) -> p h n", h=4) for _ in range(2)]
    r_t3 = [P.res("t3%d" % i) for i in range(2)]
    yg = [A.alloc(512) for _ in range(2)]
    r_yg = [P.res("yg%d" % i) for i in range(2)]
    zt = [A.alloc(512) for _ in range(2)]
    r_zt = [P.res("zt%d" % i, dma=True) for i in range(2)]
    junk = A.alloc(256)
    r_junk = P.res("junkD")
    nst = [A.alloc(8) for _ in range(2)]
    r_nst = [P.res("nst%d" % i) for i in range(2)]
    yo = [A.alloc(512, BF16) for _ in range(2)]
    r_yo = [P.res("yo%d" % i, dma=True) for i in range(2)]

    def b4(ap2d, c, g):
        return v3(ap2d)[:, c, g * 4:g * 4 + 4].unsqueeze(2).to_broadcast([128, 4, 64])

    P.dma("sp", zt[0], D["z"][0:128, :], writes=[r_zt[0]], owner=r_zt[0])
    it = 0
    for c in range(NT):
        c2 = c % 2
        if c + 1 < NT:
            P.dma("sp", zt[(c + 1) % 2], D["z"][(c + 1) * 128:(c + 2) * 128, :], writes=[r_zt[(c + 1) % 2]], owner=r_zt[(c + 1) % 2])
        csl = slice(c * 128, (c + 1) * 128)
        for g in range(2):
            i2 = it % 2
            it += 1
            bX, bY, bZ, bW = 0 + i2, 2 + i2, 4 + i2, 6 + i2
            xg = xtok[:, c, g * 256:(g + 1) * 256].rearrange("p (h n) -> p h n", h=4)
            P.op("dve", lambda e, i2=i2, c=c, g=g: e.tensor_tensor(out=rT[i2], in0=tri.unsqueeze(1).to_broadcast([128, 4, 128]),
                                                             in1=v3(da_all)[:, c, g * 4:g * 4 + 4].unsqueeze(2).to_broadcast([128, 4, 128]), op=ALU.mult),
                 reads=[r_k, r_t], writes=[r_rT[i2]])
            P.op("pe", lambda e, i2=i2, bX=bX: e.matmul(bank[bX][:], lhsT=upp, rhs=rT[i2].rearrange("p h n -> p (h n)"), start=True, stop=True),
                 reads=[r_k, r_rT[i2]], writes=[rb[bX]])
            P.op("act", lambda e, i2=i2, bX=bX: e.activation(out=Lt[i2].rearrange("p h n -> p (h n)"), in_=bank[bX][:], func=AF.Exp), reads=[rb[bX]], writes=[r_Lt[i2]])
            P.op("pe", lambda e, bY=bY, g=g, csl=csl: e.matmul(bank[bY][:, 0:128], lhsT=BT[:, g, csl], rhs=CT[:, g, csl], start=True, stop=True),
                 reads=[r_BT, r_CT], writes=[rb[bY]])
            P.op("dve", lambda e, i2=i2, bY=bY: e.tensor_tensor(out=Gm[i2], in0=bank[bY][:, 0:128], in1=caus, op=ALU.mult), reads=[rb[bY], r_k], writes=[r_Gm[i2]])
            P.op("dve", lambda e, i2=i2: e.tensor_tensor(out=sc[i2], in0=Lt[i2], in1=Gm[i2].unsqueeze(1).to_broadcast([128, 4, 128]), op=ALU.mult),
                 reads=[r_Lt[i2], r_Gm[i2]], writes=[r_sc[i2]])
            P.op("pool", lambda e, i2=i2, xg=xg, c=c, g=g: e.tensor_tensor(out=xd[i2], in0=xg, in1=b4(dt_all, c, g), op=ALU.mult), reads=[r_xtok, r_t], writes=[r_xd[i2]])
            P.op("pool", lambda e, i2=i2, xg=xg, c=c, g=g: e.tensor_tensor(out=xdd[i2], in0=xg, in1=b4(dtds, c, g), op=ALU.mult), reads=[r_xtok, r_t], writes=[r_xdd[i2]])
            for hh in range(4):
                P.op("pe", lambda e, hh=hh, i2=i2, bZ=bZ: e.matmul(bank[bZ][:, hh * 64:(hh + 1) * 64], lhsT=sc[i2][:, hh, :], rhs=xd[i2][:, hh, :], start=True, stop=True),
                     reads=[r_sc[i2], r_xd[i2]], writes=[rb[bZ]])
            if c > 0:
                P.op("pe", lambda e, bZ=bZ, g=g, csl=csl: e.matmul(bank[bZ][:, 256:512], lhsT=CT[:, g, csl], rhs=Sbf[g], start=True, stop=True),
                     reads=[r_CT, r_Sbf[g]], writes=[rb[bZ]])
            if c + 1 < NT:
                P.op("pe", lambda e, bW=bW, c=c, g=g, i2=i2: e.matmul(bank[bW][:, 0:256], lhsT=Btok[:, c, g * 128:(g + 1) * 128], rhs=xdd[i2].rearrange("p h n -> p (h n)"), start=True, stop=True),
                     reads=[r_Btok, r_xdd[i2]], writes=[rb[bW]])
                pw = bank[bW][:, 0:256].rearrange("p (h n) -> p h n", h=4)
                if c == 0:
                    P.op("dve", lambda e, g=g, pw=pw: e.tensor_copy(out=St[g], in_=pw), reads=[rb[bW]], writes=[r_St[g]])
                else:
                    P.op("dve", lambda e, g=g, c=c: e.tensor_tensor(out=tS[g], in0=St[g], in1=b4(cd_all, c, g), op=ALU.mult), reads=[r_St[g], r_t], writes=[r_tS[g]])
                    P.op("dve", lambda e, g=g, pw=pw: e.tensor_tensor(out=St[g], in0=tS[g], in1=pw, op=ALU.add), reads=[r_tS[g], rb[bW]], writes=[r_St[g]])
                P.op("act", lambda e, g=g: e.copy(out=Sbf[g], in_=St[g].rearrange("p h n -> p (h n)")), reads=[r_St[g]], writes=[r_Sbf[g]])
            pz = bank[bZ][:]
            if c > 0:
                P.op("dve", lambda e, i2=i2, pz=pz, c=c, g=g: e.tensor_tensor(out=t1[i2], in0=pz[:, 256:512].rearrange("p (h n) -> p h n", h=4), in1=b4(ea_all, c, g), op=ALU.mult),
                     reads=[rb[bZ], r_t], writes=[r_t1[i2]])
                P.op("dve", lambda e, i2=i2, pz=pz: e.tensor_tensor(out=t1[i2], in0=t1[i2], in1=pz[:, 0:256].rearrange("p (h n) -> p h n", h=4), op=ALU.add),
                     reads=[rb[bZ], r_t1[i2]], writes=[r_t1[i2]])
            else:
                P.op("dve", lambda e, i2=i2, pz=pz: e.tensor_copy(out=t1[i2], in_=pz[:, 0:256].rearrange("p (h n) -> p h n", h=4)), reads=[rb[bZ]], writes=[r_t1[i2]])
            P.op("pool", lambda e, i2=i2, xg=xg, g=g: e.tensor_tensor(out=t3[i2], in0=xg, in1=dsk[:, g * 4:g * 4 + 4].unsqueeze(2).to_broadcast([128, 4, 64]), op=ALU.mult),
                 reads=[r_xtok, r_k], writes=[r_t3[i2]])
            P.op("pool", lambda e, i2=i2, c2=c2, g=g: e.tensor_tensor(out=yg[c2][:, g * 256:(g + 1) * 256].rearrange("p (h n) -> p h n", h=4), in0=t1[i2], in1=t3[i2], op=ALU.add),
                 reads=[r_t1[i2], r_t3[i2]], writes=[r_yg[c2]])
        P.op("act", lambda e, c2=c2: e.activation(out=zt[c2], in_=zt[c2], func=AF.Silu), reads=[r_zt[c2]], writes=[r_zt[c2]])
        P.op("dve", lambda e, c2=c2: e.tensor_tensor(out=yg[c2], in0=yg[c2], in1=zt[c2], op=ALU.mult), reads=[r_yg[c2], r_zt[c2]], writes=[r_yg[c2]])
        for g in range(2):
            P.op("act", lambda e, c2=c2, g=g: e.activation(out=junk, in_=yg[c2][:, g * 256:(g + 1) * 256], func=AF.Square, accum_out=nst[c2][:, g:g + 1]),
                 reads=[r_yg[c2]], writes=[r_junk, r_nst[c2]])
        P.op("act", lambda e, c2=c2: e.activation(out=nst[c2][:, 2:4], in_=nst[c2][:, 0:2], func=AF.Sqrt, scale=1.0 / 256, bias=C.eps_t[:, 0:1]), reads=[r_nst[c2]], writes=[r_nst[c2]])
        P.op("dve", lambda e, c2=c2: e.reciprocal(out=nst[c2][:, 4:6], in_=nst[c2][:, 2:4]), reads=[r_nst[c2]], writes=[r_nst[c2]])
        P.op("dve", lambda e, c2=c2: e.tensor_tensor(out=yg[c2].rearrange("p (g n) -> p g n", g=2), in0=yg[c2].rearrange("p (g n) -> p g n", g=2),
                                                in1=nst[c2][:, 4:6].unsqueeze(2).to_broadcast([128, 2, 256]), op=ALU.mult),
             reads=[r_yg[c2], r_nst[c2]], writes=[r_yg[c2]])
        P.op("pool", lambda e, c2=c2: e.tensor_tensor(out=yo[c2], in0=yg[c2], in1=nw, op=ALU.mult), reads=[r_yg[c2], r_k], writes=[r_yo[c2]])
        P.dma("sp", D["Y"][c * 128:(c + 1) * 128, 256:768], yo[c2], reads=[r_yo[c2]], owner=r_yo[c2])
    P.end_phase()


def phase_E(C, l, x_src, x_dst):
    nc, P, D = C.nc, C.P, C.D
    A = C.arena
    A.off = C.base_off
    bank, rb = C.bank, C.rb
    w1 = A.alloc(8 * DFF, BF16).rearrange("p (k n) -> p k n", k=8)
    w2 = A.alloc(32 * DM, BF16).rearrange("p (f n) -> p f n", f=32)
    r_w1 = P.res("w1", dma=True)
    r_w2 = P.res("w2", dma=True)
    mark = A.off
    wout = A.alloc(8 * DM, BF16).rearrange("p (k n) -> p k n", k=8)
    r_wout = P.res("wout", dma=True)
    for k in range(8):
        P.dma("pool", wout[:, k, :], D["w_out"][l, k * 128:(k + 1) * 128, :], writes=[r_wout], owner=r_wout)
    for k in range(8):
        P.dma("pool", w1[:, k, :], D["w_mlp_in"][l, k * 128:(k + 1) * 128, :], writes=[r_w1], owner=r_w1)
    w2src = D["w_mlp_out"][l].rearrange("(f p) n -> p f n", p=128)
    for q in range(8):
        P.dma("pool", w2[:, q * 4:(q + 1) * 4, :], w2src[:, q * 4:(q + 1) * 4, :], writes=[r_w2], owner=r_w2)
    n2w = A.alloc(DM)
    r_n2w = P.res("n2w", dma=True)
    P.dma("sp", n2w, bcast_rows(D["norm2_w"][l]), writes=[r_n2w], owner=r_n2w)
    yt = [A.alloc(DM, BF16) for _ in range(2)]
    r_yt = [P.res("yt%d" % i, dma=True) for i in range(2)]
    xs = [A.alloc(DM) for _ in range(2)]
    r_xs = [P.res("xsE%d" % i, dma=True) for i in range(2)]
    yT = [A.alloc(DM, BF16).rearrange("p (k n) -> p k n", k=8) for _ in range(2)]
    r_yT = [P.res("yT%d" % i) for i in range(2)]
    x1t = [A.alloc(DM) for _ in range(2)]
    r_x1t = [P.res("x1t%d" % i, dma=True) for i in range(2)]
    junk = A.alloc(DM)
    r_junk = P.res("junkE")
    st = [A.alloc(8) for _ in range(2)]
    r_st = [P.res("stE%d" % i) for i in range(2)]
    h2b = [A.alloc(DM, BF16) for _ in range(2)]
    r_h2b = [P.res("h2b%d" % i) for i in range(2)]
    h2s = [A.alloc(DM, BF16).rearrange("p (k n) -> p k n", k=8) for _ in range(2)]
    r_h2s = [P.res("h2s%d" % i, dma=True) for i in range(2)]
    Yv = D["Y"].rearrange("(t p) c -> t p c", p=128)
    xv = x_src.rearrange("(t p) c -> t p c", p=128)
    x1v = D["x1"].rearrange("(t p) c -> t p c", p=128)
    h2Tv = D["h2T"].rearrange("(k p) s -> p k s", p=128)
    psA = bank[0][:].bitcast(BF16)
    psB = bank[3][:].bitcast(BF16)

    def loadE1(t):
        P.dma("sp", yt[t % 2], Yv[t], writes=[r_yt[t % 2]], owner=r_yt[t % 2])
        P.dma("sp", xs[t % 2], xv[t], writes=[r_xs[t % 2]], owner=r_xs[t % 2])

    loadE1(0)
    for t in range(NT):
        p2 = t % 2
        if t + 1 < NT:
            loadE1(t + 1)
        for k in range(8):
            P.op("pe", lambda e, k=k, p2=p2: e.transpose(out=psA[:, k * 128:(k + 1) * 128], in_=yt[p2][:, k * 128:(k + 1) * 128], identity=C.ident_b),
                 reads=[r_yt[p2]], writes=[rb[0]])
        P.op("act", lambda e, p2=p2: e.copy(out=yT[p2], in_=psA.rearrange("p (k n) -> p k n", k=8)), reads=[rb[0]], writes=[r_yT[p2]])
        for cg in range(2):
            bk = 1 + cg
            for k in range(8):
                P.op("pe", lambda e, k=k, cg=cg, bk=bk, p2=p2: e.matmul(bank[bk][:], lhsT=yT[p2][:, k, :], rhs=wout[:, k, cg * 512:(cg + 1) * 512], start=(k == 0), stop=(k == 7)),
                     reads=[r_yT[p2], r_wout], writes=[rb[bk]])
            P.op("dve", lambda e, cg=cg, bk=bk, p2=p2: e.tensor_tensor(out=x1t[p2][:, cg * 512:(cg + 1) * 512], in0=xs[p2][:, cg * 512:(cg + 1) * 512], in1=bank[bk][:], op=ALU.add),
                 reads=[rb[bk], r_xs[p2]], writes=[r_x1t[p2]])
        P.dma("sp", x1v[t], x1t[p2], reads=[r_x1t[p2]], owner=r_x1t[p2])
        P.op("act", lambda e, p2=p2: e.activation(out=junk, in_=x1t[p2], func=AF.Square, accum_out=st[p2][:, 0:1]), reads=[r_x1t[p2]], writes=[r_junk, r_st[p2]])
        P.op("act", lambda e, p2=p2: e.activation(out=st[p2][:, 1:2], in_=st[p2][:, 0:1], func=AF.Sqrt, scale=1.0 / DM, bias=C.eps_t[:, 0:1]), reads=[r_st[p2]], writes=[r_st[p2]])
        P.op("dve", lambda e, p2=p2: e.reciprocal(out=st[p2][:, 2:3], in_=st[p2][:, 1:2]), reads=[r_st[p2]], writes=[r_st[p2]])
        P.op("dve", lambda e, p2=p2: e.scalar_tensor_tensor(out=h2b[p2], in0=x1t[p2], scalar=st[p2][:, 2:3], in1=n2w, op0=ALU.mult, op1=ALU.mult),
             reads=[r_x1t[p2], r_st[p2], r_n2w], writes=[r_h2b[p2]])
        for k in range(8):
            P.op("pe", lambda e, k=k, p2=p2: e.transpose(out=psB[:, k * 128:(k + 1) * 128], in_=h2b[p2][:, k * 128:(k + 1) * 128], identity=C.ident_b),
                 reads=[r_h2b[p2]], writes=[rb[3]])
        P.op("act", lambda e, p2=p2: e.copy(out=h2s[p2], in_=psB.rearrange("p (k n) -> p k n", k=8)), reads=[rb[3]], writes=[r_h2s[p2]])
        P.dma("sp", h2Tv[:, :, t * 128:(t + 1) * 128], h2s[p2], reads=[r_h2s[p2]], owner=r_h2s[p2])
    P.barrier()

    A.off = mark
    hid = A.alloc(32 * 512, BF16).rearrange("p (f n) -> p f n", f=32)
    r_hid = [P.res("hid%d" % f) for f in range(32)]
    h2g = [A.alloc(8 * 512, BF16).rearrange("p (k n) -> p k n", k=8) for _ in range(2)]
    r_h2g = [P.res("h2g%d" % i, dma=True) for i in range(2)]
    rl = [A.alloc(512) for _ in range(2)]
    r_rl = [P.res("rl%d" % i) for i in range(2)]
    x1b = [A.alloc(DM) for _ in range(2)]
    r_x1b = [P.res("x1b%d" % i, dma=True) for i in range(2)]
    ot = [A.alloc(DM) for _ in range(2)]
    r_ot = [P.res("ot%d" % i, dma=True) for i in range(2)]
    ov = x_dst.rearrange("(t p) c -> t p c", p=128)
    P.dma("sp", h2g[0], h2Tv[:, :, 0:512], writes=[r_h2g[0]], owner=r_h2g[0])
    fcnt = 0
    for g in range(8):
        g2 = g % 2
        if g + 1 < 8:
            P.dma("sp", h2g[(g + 1) % 2], h2Tv[:, :, (g + 1) * 512:(g + 2) * 512], writes=[r_h2g[(g + 1) % 2]], owner=r_h2g[(g + 1) % 2])
        for f in range(32):
            bk = fcnt % 4
            r2 = fcnt % 2
            fcnt += 1
            for k in range(8):
                P.op("pe", lambda e, k=k, f=f, bk=bk, g2=g2: e.matmul(bank[bk][:], lhsT=w1[:, k, f * 128:(f + 1) * 128], rhs=h2g[g2][:, k, :], start=(k == 0), stop=(k == 7)),
                     reads=[r_w1, r_h2g[g2]], writes=[rb[bk]])
            P.op("act", lambda e, bk=bk, r2=r2: e.activation(out=rl[r2], in_=bank[bk][:], func=AF.Relu), reads=[rb[bk]], writes=[r_rl[r2]])
            P.op("dve", lambda e, bk=bk, r2=r2, f=f: e.tensor_tensor(out=hid[:, f, :], in0=rl[r2], in1=bank[bk][:], op=ALU.mult), reads=[rb[bk], r_rl[r2]], writes=[r_hid[f]])
        for i in range(4):
            t = 4 * g + i
            p2 = t % 2
            P.dma("sp", x1b[p2], x1v[t], writes=[r_x1b[p2]], owner=r_x1b[p2])
            for cg in range(2):
                bk = 4 + (2 * i + cg) % 4
                for f in range(32):
                    P.op("pe", lambda e, f=f, cg=cg, bk=bk, i=i: e.matmul(bank[bk][:], lhsT=hid[:, f, i * 128:(i + 1) * 128], rhs=w2[:, f, cg * 512:(cg + 1) * 512], start=(f == 0), stop=(f == 31)),
                         reads=[r_hid[f], r_w2], writes=[rb[bk]])
                P.op("dve", lambda e, cg=cg, bk=bk, p2=p2: e.tensor_tensor(out=ot[p2][:, cg * 512:(cg + 1) * 512], in0=x1b[p2][:, cg * 512:(cg + 1) * 512], in1=bank[bk][:], op=ALU.add),
                     reads=[rb[bk], r_x1b[p2]], writes=[r_ot[p2]])
            P.dma("sp", ov[t], ot[p2], reads=[r_ot[p2]], owner=r_ot[p2])
    P.end_phase()


def build(layers=(0, 1), phases=("A", "B", "C", "D", "E"), feed=(), expose=()):
    nc = bass.Bass("TRN2", target_bir_lowering=False)
    D = {}
    D["x"] = nc.dram_tensor("x", [S, DM], F32, kind="ExternalInput").ap()
    for n, shp in PARAM_SHAPES.items():
        D[n] = nc.dram_tensor(n, shp, F32, kind="ExternalInput").ap()
    for n, shp in CONST_SHAPES.items():
        D["c_" + n] = nc.dram_tensor("c_" + n, shp, BF16 if n.startswith("mask") else F32, kind="ExternalInput").ap()
    for n, (shp, dt) in SCRATCH.items():
        kind = "ExternalInput" if n in feed else ("ExternalOutput" if n in expose else "Internal")
        D[n] = nc.dram_tensor(n, shp, dt, kind=kind).ap()
    D["out"] = nc.dram_tensor("out", [S, DM], F32, kind="ExternalOutput").ap()

    with ExitStack() as es:
        C = Ctx()
        C.nc, C.D = nc, D
        C.P = P = Prog(nc, es)
        C.arena = A = Arena(nc)
        C.bank = [nc.alloc_psum_tensor("bank%d" % i, [128, 512], F32) for i in range(8)]
        C.rb = [P.res("bank%d" % i) for i in range(8)]
        for r in C.rb:
            r.excl = True
        idf = A.alloc(128)
        C.ident_f = idf
        C.ident_b = A.alloc(128, BF16)
        C.eps_t = A.alloc(8)
        C.pastm = A.alloc(32)
        r_c = P.res("consts", dma=True)
        P.dma("sp", idf, D["c_ident"], writes=[r_c], owner=r_c)
        P.dma("sp", C.pastm, D["c_pastm"], writes=[r_c], owner=r_c)
        P.op("dve", lambda e: e.tensor_copy(out=C.ident_b, in_=idf), reads=[r_c], writes=[r_c])
        P.op("dve", lambda e: e.memset(C.eps_t, EPS), writes=[r_c])
        C.one_t = A.alloc(8)
        P.op("dve", lambda e: e.memset(C.one_t, 1.0), writes=[r_c])
        P.barrier()
        C.base_off = A.off

        for l in layers:
            x_src = D["x"] if l == 0 else D["xcur"]
            x_dst = D["xcur"] if l == 0 else D["out"]
            if "A" in phases:
                phase_A(C, l, x_src)
            if "B" in phases:
                phase_attn(C, l, "A")
            if "C" in phases:
                phase_attn(C, l, "C")
            if "D" in phases:
                phase_D(C, l)
            if "E" in phases:
                phase_E(C, l, x_src, x_dst)
        stats = P.emit()
    return nc, stats


def make_in_map(inputs, b, consts):
    m = {"x": np.ascontiguousarray(inputs["x"][b])}
    for n in PARAM_SHAPES:
        m[n] = np.ascontiguousarray(inputs[n])
    for n, v in consts.items():
        m["c_" + n] = v
    return m


def kernel(**inputs):
    inputs = {k: np.asarray(v) for k, v in inputs.items()}
    nc, _ = build()
    consts = host_consts()
    in_maps = [make_in_map(inputs, c % 4, consts) for c in range(8)]
    res = run_bass_kernel_spmd(nc, in_maps, core_ids=list(range(8)))
    out = np.stack([res.results[b]["out"] for b in range(4)], axis=0)
    return out.astype(np.float32)
```

```python
import math
from contextlib import ExitStack

import numpy as np
import ml_dtypes

import concourse.bass as bass
import concourse.mybir as mybir
from concourse.bass_utils import run_bass_kernel_spmd

F32 = mybir.dt.float32
BF16 = mybir.dt.bfloat16
ALU = mybir.AluOpType
AF = mybir.ActivationFunctionType
AX = mybir.AxisListType

ENGS = ("pe", "act", "dve", "pool", "sp")
S = 4096
NT = 32
DM = 1024
DIN = 3080
DFF = 4096
EPS = 1e-6
NEG = -1e30
SLOPES = [2.0 ** (-8.0 * i / 8) for i in range(1, 9)]
SL_A = SLOPES[0::2]
SL_C = SLOPES[1::2]


class Slot:
    __slots__ = ("sem", "cnt")

    def __init__(self, sem):
        self.sem = sem
        self.cnt = 0


class Res:
    __slots__ = ("name", "w", "r", "slot", "excl")

    def __init__(self, name, slot=None):
        self.name = name
        self.w = None
        self.r = []
        self.slot = slot
        self.excl = False


class Op:
    __slots__ = ("eng", "fn", "deps", "need_inc", "inc_idx", "kind")

    def __init__(self, eng, fn, deps, kind="c"):
        self.eng = eng
        self.fn = fn
        self.deps = deps
        self.need_inc = False
        self.inc_idx = 0
        self.kind = kind


class Prog:
    def __init__(self, nc, es, n_dma=84):
        self.nc = nc
        self.q = {e: [] for e in ENGS}
        self.esem = {e: es.enter_context(nc.semaphore("prog_" + e)) for e in ENGS if e != "sp"}
        self.slots = [Slot(es.enter_context(nc.semaphore("dq%d" % i))) for i in range(n_dma)]
        self.free = list(self.slots)
        self.phase = []

    def res(self, name, dma=False):
        slot = None
        if dma:
            slot = self.free.pop()
            self.phase.append(slot)
        return Res(name, slot)

    def _collect(self, reads, writes):
        deps = []
        for r in reads:
            if r.w is not None:
                deps.append(r.w)
        for w in writes:
            if w.w is not None:
                deps.append(w.w)
            deps.extend(w.r)
        for d in deps:
            if isinstance(d, Op):
                d.need_inc = True
        return deps

    def op(self, eng, fn, reads=(), writes=()):
        ex = [r for r in reads if r.excl]
        if ex:
            reads = [r for r in reads if not r.excl]
            writes = list(writes) + ex
        o = Op(eng, fn, self._collect(reads, writes))
        self.q[eng].append(o)
        for r in reads:
            r.r.append(o)
        for w in writes:
            w.w = o
            w.r = []
        return o

    def dma(self, queue, out, in_, reads=(), writes=(), owner=None, **kw):
        slot = owner.slot
        deps = self._collect(reads, writes)
        slot.cnt += 16
        tok = ("d", slot, slot.cnt)

        def fn(e, out=out, in_=in_, sem=slot.sem):
            return e.dma_start(out=out, in_=in_, **kw).then_inc(sem, 16)

        self.q[queue].append(Op(queue, fn, deps, kind="dma"))
        for r in reads:
            r.r.append(tok)
        for w in writes:
            w.w = tok
            w.r = []
        return tok

    def barrier(self):
        deps = []
        for e in ENGS:
            for o in reversed(self.q[e]):
                if o.kind == "c":
                    deps.append(o)
                    o.need_inc = True
                    break
        for s in self.slots:
            if s.cnt > 0:
                deps.append(("d", s, s.cnt))
        for e in ENGS:
            self.q[e].append(Op(e, None, list(deps), kind="wait"))

    def end_phase(self):
        self.barrier()
        self.free.extend(self.phase)
        self.phase = []

    def emit(self):
        nc = self.nc
        for e in ENGS:
            c = 0
            for o in self.q[e]:
                if o.kind == "c" and o.need_inc:
                    c += 1
                    o.inc_idx = c
        stats = {}

        def replay(ename, eng):
            observed = {}
            nwait = 0
            for o in self.q[ename]:
                need = {}
                for d in o.deps:
                    if isinstance(d, Op):
                        if d.eng == ename and ename == "pe":
                            continue
                        key = d.eng
                        sem = self.esem[d.eng]
                        val = d.inc_idx
                    else:
                        _, s, val = d
                        key = id(s)
                        sem = s.sem
                    if observed.get(key, 0) >= val:
                        continue
                    if key not in need or need[key][1] < val:
                        need[key] = (sem, val)
                for key, (sem, val) in need.items():
                    eng.wait_ge(sem, val)
                    observed[key] = val
                    nwait += 1
                if o.fn is not None:
                    ins = o.fn(eng)
                    if o.kind == "c" and o.need_inc:
                        ins.then_inc(self.esem[ename], 1)
            stats[ename] = (len(self.q[ename]), nwait)

        with nc.Block() as block:
            @block.tensor
            def _(e):
                replay("pe", e)

            @block.scalar
            def _(e):
                replay("act", e)

            @block.vector
            def _(e):
                replay("dve", e)

            @block.gpsimd
            def _(e):
                replay("pool", e)

            @block.sync
            def _(e):
                replay("sp", e)
        return stats


DT_SIZE = {F32: 4, BF16: 2}


class Arena:
    def __init__(self, nc):
        nbytes = (nc.sbuf_bytes_remaining - 2048) // 64 * 64
        self.words = nbytes // 4
        self.t = nc.alloc_sbuf_tensor("arena", [128, self.words], F32)
        self.off = 0
        self.peak = 0

    def alloc(self, cols, dtype=F32):
        words = (cols * DT_SIZE[dtype] + 3) // 4
        words = (words + 7) // 8 * 8
        assert self.off + words <= self.words, ("SBUF arena overflow", self.off * 4, words * 4, self.words * 4)
        ap = self.t[:, self.off:self.off + words]
        self.off += words
        self.peak = max(self.peak, self.off)
        if dtype != F32:
            ap = ap.bitcast(dtype)
        return ap[:, 0:cols]


def bcast_rows(ap1d, reps=1):
    n = ap1d.shape[0]
    if reps == 1:
        return bass.AP(ap1d.tensor, ap1d.offset, [[0, 128], [1, n]])
    return bass.AP(ap1d.tensor, ap1d.offset, [[0, 128], [0, reps], [1, n]])


def host_consts():
    c = {}
    c["ident"] = np.eye(128, dtype=np.float32)
    kl = np.arange(128)[:, None]
    def mk(nof, slopes, mult_fn):
        col = np.arange(nof * 128)[None, :]
        delta = (col - kl).astype(np.float64)
        out = np.zeros((4, 128, nof * 128), np.float32)
        for h, sl in enumerate(slopes):
            m = mult_fn(delta) * np.exp(-sl * np.maximum(delta, 0.0))
            out[h] = np.where(delta >= 0, m, 0.0).astype(np.float32)
        return out
    def multA(d):
        return ((d <= 128).astype(np.float64) + ((d % 4 == 0) & (d <= 512)) + ((d % 16 == 0) & (d <= 2048)))
    c["maskA"] = mk(17, SL_A, multA).astype(ml_dtypes.bfloat16)
    c["maskC"] = mk(32, SL_C, lambda d: np.ones_like(d)).astype(ml_dtypes.bfloat16)
    k = np.arange(128)[:, None]
    j = np.arange(128)[None, :]
    c["tri"] = (k <= j).astype(np.float32)
    c["upp"] = (k > j).astype(np.float32)
    c["caus"] = (j >= k).astype(np.float32)
    pm = np.zeros((128, 32), np.float32)
    pm[:, 16:] = NEG
    c["pastm"] = pm
    return c


CONST_SHAPES = {"ident": [128, 128], "maskA": [4, 128, 17 * 128], "maskC": [4, 128, 32 * 128],
                "tri": [128, 128], "upp": [128, 128], "caus": [128, 128], "pastm": [128, 32]}

PARAM_SHAPES = {
    "norm1_w": [2, 1024], "w_in": [2, 1024, 3080], "a_q_norm": [2, 64], "a_k_norm": [2, 64],
    "c_q_norm": [2, 64], "c_k_norm": [2, 64], "conv_w": [2, 4, 1024], "conv_b": [2, 1024],
    "dt_bias": [2, 8], "a_log": [2, 8], "d_skip": [2, 8], "ssm_norm_w": [2, 512],
    "w_out": [2, 1024, 1024], "norm2_w": [2, 1024], "w_mlp_in": [2, 1024, 4096],
    "w_mlp_out": [2, 4096, 1024],
}

SCRATCH = {
    "qkT": ([8, 128, S], BF16), "v_a": ([S, 256], BF16), "v_c": ([S, 256], BF16),
    "sel": ([S, 64], F32), "z": ([S, 512], F32), "xbcT": ([1024, S], F32), "dtr": ([S, 8], F32),
    "Y": ([S, 1024], BF16), "x1": ([S, 1024], F32), "h2T": ([1024, S], BF16), "xcur": ([S, 1024], F32),
}


class Ctx:
    pass


def phase_A(C, l, x_src):
    nc, P, D = C.nc, C.P, C.D
    A = C.arena
    A.off = C.base_off
    bank, rb = C.bank, C.rb
    win = A.alloc(8 * DIN, BF16).rearrange("p (k n) -> p k n", k=8)
    r_win = P.res("win", dma=True)
    for k in range(8):
        P.dma("pool", win[:, k, :], D["w_in"][l, k * 128:(k + 1) * 128, :], writes=[r_win], owner=r_win)
    n1w = A.alloc(1024)
    qkwA = A.alloc(512)
    qwC = A.alloc(256)
    kwC = A.alloc(256)
    r_small = P.res("smallA", dma=True)
    P.dma("sp", n1w, bcast_rows(D["norm1_w"][l]), writes=[r_small], owner=r_small)
    P.dma("sp", qkwA[:, 0:256].rearrange("p (r n) -> p r n", r=4), bcast_rows(D["a_q_norm"][l], 4), writes=[r_small], owner=r_small)
    P.dma("sp", qkwA[:, 256:512].rearrange("p (r n) -> p r n", r=4), bcast_rows(D["a_k_norm"][l], 4), writes=[r_small], owner=r_small)
    P.dma("sp", qwC.rearrange("p (r n) -> p r n", r=4), bcast_rows(D["c_q_norm"][l], 4), writes=[r_small], owner=r_small)
    P.dma("sp", kwC.rearrange("p (r n) -> p r n", r=4), bcast_rows(D["c_k_norm"][l], 4), writes=[r_small], owner=r_small)

    NXB = 3
    xs = [A.alloc(1024) for _ in range(NXB)]
    r_xs = [P.res("xs%d" % i, dma=True) for i in range(NXB)]
    junk = A.alloc(1024)
    r_junk = P.res("junk")
    st = [A.alloc(8) for _ in range(2)]
    r_st = [P.res("st%d" % i) for i in range(2)]
    hb = [A.alloc(1024, BF16) for _ in range(2)]
    r_hb = [P.res("hb%d" % i) for i in range(2)]
    hT = [A.alloc(8 * 512, BF16).rearrange("p (k n) -> p k n", k=8) for _ in range(2)]
    r_hT = [P.res("hT%d" % i) for i in range(2)]
    sq = [A.alloc(512) for _ in range(2)]
    r_sq = [P.res("sq%d" % i) for i in range(2)]
    hst = [A.alloc(48) for _ in range(2)]
    r_hst = [P.res("hst%d" % i) for i in range(2)]
    qn = [A.alloc(512) for _ in range(2)]
    r_qn = [P.res("qn%d" % i) for i in range(2)]
    qkbA = [A.alloc(512, BF16) for _ in range(2)]
    r_qkbA = [P.res("qkbA%d" % i) for i in range(2)]
    qkC = [A.alloc(512) for _ in range(2)]
    r_qkC = [P.res("qkC%d" % i) for i in range(2)]
    stA = [A.alloc(4 * 512, BF16).rearrange("p (j n) -> p j n", j=4) for _ in range(2)]
    r_stA = [P.res("stA%d" % i, dma=True) for i in range(2)]
    stC = [A.alloc(4 * 512, BF16).rearrange("p (j n) -> p j n", j=4) for _ in range(2)]
    r_stC = [P.res("stC%d" % i, dma=True) for i in range(2)]
    stv = [A.alloc(512, BF16) for _ in range(2)]
    r_stv = [P.res("stv%d" % i, dma=True) for i in range(2)]
    stz = [A.alloc(512) for _ in range(2)]
    r_stz = [P.res("stz%d" % i, dma=True) for i in range(2)]
    stdt = [A.alloc(8) for _ in range(2)]
    r_stdt = [P.res("stdt%d" % i, dma=True) for i in range(2)]
    stx = [A.alloc(512) for _ in range(2)]
    r_stx = [P.res("stx%d" % i, dma=True) for i in range(2)]
    qTf = [A.alloc(256).rearrange("p (j n) -> p j n", j=2) for _ in range(2)]
    r_qTf = [P.res("qTf%d" % i) for i in range(2)]
    ksum = [A.alloc(2) for _ in range(2)]
    r_ksum = [P.res("ksum%d" % i) for i in range(2)]
    kmT = A.alloc(32).rearrange("p (j n) -> p j n", j=2)
    r_kmT = P.res("kmT")
    gm = [A.alloc(64) for _ in range(2)]
    r_gm = [P.res("gm%d" % i) for i in range(2)]
    mx8 = [A.alloc(32) for _ in range(2)]
    r_mx8 = [P.res("mx8%d" % i) for i in range(2)]
    selt = [A.alloc(64) for _ in range(2)]
    r_selt = [P.res("selt%d" % i, dma=True) for i in range(2)]

    P.op("dve", lambda e: e.memset(kmT, 0.0), writes=[r_kmT])

    psT = bank[0][:].bitcast(BF16)
    ps6 = bank[6][:].bitcast(BF16)
    x_view = x_src.rearrange("(t p) d -> t p d", p=128)

    def load_x(t):
        b = t % NXB
        P.dma("sp", xs[b], x_view[t], writes=[r_xs[b]], owner=r_xs[b])

    TB = [1, 2, 4, 5]

    def norm_T(t, hTg, r_hTg, i):
        if t + 2 < NT:
            load_x(t + 2)
        b = t % NXB
        p2 = t % 2
        P.op("act", lambda e: e.activation(out=junk, in_=xs[b], func=AF.Square, accum_out=st[p2][:, 0:1]),
             reads=[r_xs[b]], writes=[r_junk, r_st[p2]])
        P.op("act", lambda e: e.activation(out=st[p2][:, 1:2], in_=st[p2][:, 0:1], func=AF.Sqrt, scale=1.0 / DM, bias=C.eps_t[:, 0:1]),
             reads=[r_st[p2]], writes=[r_st[p2]])
        P.op("dve", lambda e: e.reciprocal(out=st[p2][:, 2:3], in_=st[p2][:, 1:2]), reads=[r_st[p2]], writes=[r_st[p2]])
        P.op("dve", lambda e: e.scalar_tensor_tensor(out=hb[p2], in0=xs[b], scalar=st[p2][:, 2:3], in1=n1w, op0=ALU.mult, op1=ALU.mult),
             reads=[r_xs[b], r_st[p2], r_small], writes=[r_hb[p2]])
        for k in range(8):
            P.op("pe", lambda e, k=k: e.transpose(out=psT[:, k * 128:(k + 1) * 128], in_=hb[p2][:, k * 128:(k + 1) * 128], identity=C.ident_b),
                 reads=[r_hb[p2]], writes=[rb[0]])
        P.op("act", lambda e: e.copy(out=hTg[:, :, i * 128:(i + 1) * 128], in_=psT.rearrange("p (k n) -> p k n", k=8)),
             reads=[rb[0]], writes=[r_hTg])

    def head_norm(pb_in, bk, p2, nh, sqs, c_ss, c_s, c_r, qn_out):
        P.op("act", lambda e: e.activation(out=sqs, in_=pb_in, func=AF.Square), reads=[rb[bk]], writes=[r_sq[p2]])
        P.op("dve", lambda e: e.tensor_reduce(out=c_ss, in_=sqs.rearrange("p (h n) -> p h n", h=nh), axis=AX.X, op=ALU.add),
             reads=[r_sq[p2]], writes=[r_hst[p2]])
        P.op("act", lambda e: e.activation(out=c_s, in_=c_ss, func=AF.Sqrt, scale=1.0 / 64, bias=C.eps_t[:, 0:1]),
             reads=[r_hst[p2]], writes=[r_hst[p2]])
        P.op("dve", lambda e: e.reciprocal(out=c_r, in_=c_s), reads=[r_hst[p2]], writes=[r_hst[p2]])
        P.op("dve", lambda e: e.tensor_tensor(out=qn_out.rearrange("p (h n) -> p h n", h=nh), in0=pb_in.rearrange("p (h n) -> p h n", h=nh),
                                              in1=c_r.unsqueeze(2).to_broadcast([128, nh, 64]), op=ALU.mult),
             reads=[rb[bk], r_hst[p2]], writes=[r_qn[p2]])

    def stage1(t, hTg, r_hTg, i):
        p2 = t % 2
        tsl = slice(i * 128, (i + 1) * 128)
        for c in range(4):
            bk = TB[c]
            for k in range(8):
                P.op("pe", lambda e, k=k, c=c, bk=bk: e.matmul(bank[bk][:], lhsT=hTg[:, k, tsl], rhs=win[:, k, c * 512:(c + 1) * 512], start=(k == 0), stop=(k == 7)),
                     reads=[r_hTg, r_win], writes=[rb[bk]])
            pb = bank[bk][:]
            H = hst[p2]
            if c == 0:
                head_norm(pb, bk, p2, 8, sq[p2], H[:, 0:8], H[:, 16:24], H[:, 32:40], qn[p2])
                P.op("pool", lambda e: e.tensor_tensor(out=qkbA[p2], in0=qn[p2], in1=qkwA, op=ALU.mult),
                     reads=[r_qn[p2], r_small], writes=[r_qkbA[p2]])
            elif c == 1:
                P.op("act", lambda e, pb=pb: e.copy(out=stv[p2][:, 0:256], in_=pb[:, 0:256]), reads=[rb[bk]], writes=[r_stv[p2]])
                head_norm(pb[:, 256:512], bk, p2, 4, sq[p2][:, 0:256], H[:, 8:12], H[:, 24:28], H[:, 40:44], qn[p2][:, 0:256])
                P.op("pool", lambda e: e.tensor_tensor(out=qkC[p2][:, 0:256], in0=qn[p2][:, 0:256], in1=qwC, op=ALU.mult),
                     reads=[r_qn[p2], r_small], writes=[r_qkC[p2]])
            elif c == 2:
                P.op("act", lambda e, pb=pb: e.copy(out=stv[p2][:, 256:512], in_=pb[:, 256:512]), reads=[rb[bk]], writes=[r_stv[p2]])
                head_norm(pb[:, 0:256], bk, p2, 4, sq[p2][:, 256:512], H[:, 12:16], H[:, 28:32], H[:, 44:48], qn[p2][:, 256:512])
                P.op("pool", lambda e: e.tensor_tensor(out=qkC[p2][:, 256:512], in0=qn[p2][:, 256:512], in1=kwC, op=ALU.mult),
                     reads=[r_qn[p2], r_small], writes=[r_qkC[p2]])
                P.dma("sp", D["v_a"][t * 128:(t + 1) * 128, :], stv[p2][:, 0:256], reads=[r_stv[p2]], owner=r_stv[p2])
                P.dma("sp", D["v_c"][t * 128:(t + 1) * 128, :], stv[p2][:, 256:512], reads=[r_stv[p2]], owner=r_stv[p2])
            else:
                P.op("act", lambda e, pb=pb: e.copy(out=stz[p2], in_=pb), reads=[rb[bk]], writes=[r_stz[p2]])
                P.dma("sp", D["z"][t * 128:(t + 1) * 128, :], stz[p2], reads=[r_stz[p2]], owner=r_stz[p2])
        for k in range(8):
            P.op("pe", lambda e, k=k: e.matmul(bank[3][:, 0:8], lhsT=hTg[:, k, tsl], rhs=win[:, k, 3072:3080], start=(k == 0), stop=(k == 7)),
                 reads=[r_hTg, r_win], writes=[rb[3]])
        P.op("act", lambda e: e.copy(out=stdt[p2], in_=bank[3][:, 0:8]), reads=[rb[3]], writes=[r_stdt[p2]])
        P.dma("sp", D["dtr"][t * 128:(t + 1) * 128, :], stdt[p2], reads=[r_stdt[p2]], owner=r_stdt[p2])

    def stage2(t):
        p2 = t % 2
        g, i = t // 4, t % 4
        blk = t // 2
        tsl = slice(i * 128, (i + 1) * 128)
        for j in range(4):
            P.op("pe", lambda e, j=j: e.transpose(out=ps6[:, j * 128:(j + 1) * 128], in_=qkbA[p2][:, j * 128:(j + 1) * 128], identity=C.ident_b),
                 reads=[r_qkbA[p2]], writes=[rb[6]])
        P.op("act", lambda e: e.copy(out=stA[g % 2][:, :, tsl], in_=ps6[:, 0:512].rearrange("p (j n) -> p j n", j=4)),
             reads=[rb[6]], writes=[r_stA[g % 2]])
        for j in range(4):
            P.op("pe", lambda e, j=j: e.transpose(out=bank[7][:, j * 128:(j + 1) * 128], in_=qkC[p2][:, j * 128:(j + 1) * 128], identity=C.ident_f),
                 reads=[r_qkC[p2]], writes=[rb[7]])
        P.op("act", lambda e: e.copy(out=stC[g % 2][:, :, tsl], in_=bank[7][:].rearrange("p (j n) -> p j n", j=4)),
             reads=[rb[7]], writes=[r_stC[g % 2]])
        P.op("dve", lambda e: e.tensor_copy(out=qTf[p2], in_=bank[7][:, 0:256].rearrange("p (j n) -> p j n", j=2)),
             reads=[rb[7]], writes=[r_qTf[p2]])
        P.op("dve", lambda e: e.tensor_reduce(out=ksum[p2], in_=bank[7][:, 256:512].rearrange("p (j n) -> p j n", j=2), axis=AX.X, op=ALU.add),
             reads=[rb[7]], writes=[r_ksum[p2]])
        for h in range(4):
            hp, pr = h % 2, h // 2
            P.op("pe", lambda e, h=h, hp=hp, pr=pr: e.matmul(bank[3][:, 32 + h * 16:48 + h * 16], lhsT=qTf[p2][hp * 64:(hp + 1) * 64, pr, :],
                                                           rhs=kmT[hp * 64:(hp + 1) * 64, pr, :], start=True, stop=True),
                 reads=[r_qTf[p2], r_kmT], writes=[rb[3]])
        P.op("dve", lambda e: e.tensor_tensor(out=gm[p2].rearrange("p (h n) -> p h n", h=4), in0=bank[3][:, 32:96].rearrange("p (h n) -> p h n", h=4),
                                              in1=C.pastm[:, 16 - blk:32 - blk].unsqueeze(1).to_broadcast([128, 4, 16]), op=ALU.add),
             reads=[rb[3]], writes=[r_gm[p2]])
        for h in range(4):
            P.op("dve", lambda e, h=h: e.max(out=mx8[p2][:, h * 8:(h + 1) * 8], in_=gm[p2][:, h * 16:(h + 1) * 16]),
                 reads=[r_gm[p2]], writes=[r_mx8[p2]])
        P.op("dve", lambda e: e.tensor_tensor(out=selt[p2].rearrange("p (h n) -> p h n", h=4), in0=gm[p2].rearrange("p (h n) -> p h n", h=4),
                                              in1=mx8[p2].rearrange("p (h n) -> p h n", h=4)[:, :, 2:3].to_broadcast([128, 4, 16]), op=ALU.is_ge),
             reads=[r_gm[p2], r_mx8[p2]], writes=[r_selt[p2]])
        P.op("dve", lambda e: e.memset(selt[p2].rearrange("p (h n) -> p h n", h=4)[:, :, blk:blk + 1], 1.0),
             reads=[], writes=[r_selt[p2]])
        P.dma("sp", D["sel"][t * 128:(t + 1) * 128, :], selt[p2], reads=[r_selt[p2]], owner=r_selt[p2])
        P.op("dve", lambda e: e.scalar_tensor_tensor(out=kmT[:, :, blk], in0=ksum[p2], scalar=1.0 / 256, in1=kmT[:, :, blk], op0=ALU.mult, op1=ALU.add),
             reads=[r_ksum[p2], r_kmT], writes=[r_kmT])
        if i == 3:
            for j in range(4):
                P.dma("sp", D["qkT"][j][:, g * 512:(g + 1) * 512], stA[g % 2][:, j, :], reads=[r_stA[g % 2]], owner=r_stA[g % 2])
                P.dma("sp", D["qkT"][4 + j][:, g * 512:(g + 1) * 512], stC[g % 2][:, j, :], reads=[r_stC[g % 2]], owner=r_stC[g % 2])

    load_x(0)
    load_x(1)
    for i in range(4):
        norm_T(i, hT[0], r_hT[0], i)
    prev = None
    xcnt = 0
    for g in range(8):
        hTg, r_hTg = hT[g % 2], r_hT[g % 2]
        for i in range(4):
            t = 4 * g + i
            stage1(t, hTg, r_hTg, i)
            if prev is not None:
                stage2(prev)
            prev = t
            if g + 1 < 8:
                norm_T(4 * (g + 1) + i, hT[(g + 1) % 2], r_hT[(g + 1) % 2], i)
        for m in range(8):
            bk = TB[xcnt % 4]
            xcnt += 1
            for k in range(8):
                P.op("pe", lambda e, k=k, m=m, bk=bk, hTg=hTg: e.matmul(bank[bk][:], lhsT=win[:, k, 2048 + m * 128:2048 + (m + 1) * 128], rhs=hTg[:, k, :], start=(k == 0), stop=(k == 7)),
                     reads=[r_hTg, r_win], writes=[rb[bk]])
            P.op("act", lambda e, bk=bk, m=m: e.copy(out=stx[m % 2], in_=bank[bk][:]), reads=[rb[bk]], writes=[r_stx[m % 2]])
            P.dma("sp", D["xbcT"][m * 128:(m + 1) * 128, g * 512:(g + 1) * 512], stx[m % 2], reads=[r_stx[m % 2]], owner=r_stx[m % 2])
    stage2(prev)
    P.end_phase()


def phase_attn(C, l, kind):
    nc, P, D = C.nc, C.P, C.D
    A = C.arena
    A.off = C.base_off
    bank, rb = C.bank, C.rb
    isA = kind == "A"
    nof = 17 if isA else 32
    qi0, ki0 = (0, 2) if isA else (4, 6)
    ycol = 0 if isA else 768
    qT = [A.alloc(S, BF16) for _ in range(2)]
    kT = [A.alloc(S, BF16) for _ in range(2)]
    r_qk = P.res("qk", dma=True)
    for p in range(2):
        P.dma("sp", qT[p], D["qkT"][qi0 + p], writes=[r_qk], owner=r_qk)
        P.dma("sp", kT[p], D["qkT"][ki0 + p], writes=[r_qk], owner=r_qk)
    V = A.alloc(NT * 4 * 65, BF16).rearrange("p (t h e) -> p t h e", t=NT, h=4)
    r_V = P.res("V", dma=True)
    vsrc = D["v_a" if isA else "v_c"].rearrange("(t p) (h e) -> p t h e", p=128, h=4)
    for t in range(NT):
        P.dma("sp", V[:, t, :, 0:64], vsrc[:, t], writes=[r_V], owner=r_V)
    P.op("pool", lambda e: e.memset(V[:, :, :, 64:65], 1.0), writes=[r_V])
    mask = A.alloc(4 * nof * 128, BF16).rearrange("p (h n) -> p h n", h=4)
    r_mask = P.res("mask", dma=True)
    msrc = D["c_maskA" if isA else "c_maskC"]
    for h in range(4):
        P.dma("sp", mask[:, h, :], msrc[h], writes=[r_mask], owner=r_mask)
    if not isA:
        selall = A.alloc(NT * 64).rearrange("p (t c) -> p t c", t=NT)
        r_sel = P.res("selall", dma=True)
        ssrc = D["sel"].rearrange("(t p) c -> p t c", p=128)
        for q4 in range(4):
            P.dma("sp", selall[:, q4 * 8:(q4 + 1) * 8, :], ssrc[:, q4 * 8:(q4 + 1) * 8, :], writes=[r_sel], owner=r_sel)
        acc = [A.alloc(4 * 65).rearrange("p (i e) -> p i e", i=4) for _ in range(2)]
        r_acc = [P.res("acc%d" % i) for i in range(2)]
        tmp = [A.alloc(4 * 65).rearrange("p (i e) -> p i e", i=4) for _ in range(3)]
        r_tmp = [P.res("tmp%d" % i) for i in range(3)]
    NSB = 4
    POOL_EVERY = 10 ** 9
    ex = [A.alloc(512, BF16) for _ in range(NSB)]
    r_ex = [P.res("ex%d" % i) for i in range(NSB)]
    pT = [A.alloc(512, BF16) for _ in range(NSB)]
    r_pT = [P.res("pT%d" % i) for i in range(NSB)]
    rc = [A.alloc(4) for _ in range(2)]
    r_rc = [P.res("rc%d" % i) for i in range(2)]
    yst = [A.alloc(4 * 256, BF16).rearrange("p (i c) -> p i c", i=4) for _ in range(2)]
    r_yst = [P.res("yst%d" % i, dma=True) for i in range(2)]

    cnt = {"step": 0, "ob": 0, "tmp": 0}

    def emit_qk(h, j, i0, i1):
        sb = cnt["step"] % NSB
        cnt["step"] += 1
        pr, hp = h // 2, h % 2
        psl = slice(hp * 64, hp * 64 + 64)
        N = (i1 - i0 + 1) * 128
        P.op("pe", lambda e: e.matmul(bank[sb][:, 0:N], lhsT=kT[pr][psl, j * 128:(j + 1) * 128], rhs=qT[pr][psl, i0 * 128:(i1 + 1) * 128], start=True, stop=True),
             reads=[r_qk], writes=[rb[sb]])
        P.op("act", lambda e: e.activation(out=ex[sb][:, 0:N], in_=bank[sb][:, 0:N], func=AF.Exp, scale=0.125), reads=[rb[sb]], writes=[r_ex[sb]])
        meng = "pool" if (cnt["step"] % POOL_EVERY == 0) else "dve"
        P.op(meng, lambda e: e.tensor_tensor(out=pT[sb][:, 0:N], in0=ex[sb][:, 0:N], in1=mask[:, h, (i0 - j) * 128:(i1 - j + 1) * 128], op=ALU.mult),
             reads=[r_ex[sb], r_mask], writes=[r_pT[sb]])
        return sb

    def emit_pv(sb, h, j, i0, i1, g, ob, started):
        for i in range(i0, i1 + 1):
            st = not started[0]
            started[0] = True
            c0 = (i - 4 * g) * 65
            P.op("pe", lambda e, i=i, st=st, c0=c0: e.matmul(bank[ob][:, c0:c0 + 65], lhsT=pT[sb][:, (i - i0) * 128:(i - i0 + 1) * 128], rhs=V[:, j, h, :],
                                                           start=st, stop=(j == i), skip_group_check=True),
                 reads=[r_pT[sb], r_V], writes=[rb[ob]])

    def store_y(g, y2):
        for i in range(4):
            t = 4 * g + i
            P.dma("sp", D["Y"][t * 128:(t + 1) * 128, ycol:ycol + 256], yst[y2][:, i, :], reads=[r_yst[y2]], owner=r_yst[y2])

    pend = []

    def flush(n_keep):
        while len(pend) > n_keep:
            args, post = pend.pop(0)
            emit_pv(*args)
            if post is not None:
                post()

    def run_steps(steps, h, g, ob, post):
        started = [False]
        for si, (j, i0, i1) in enumerate(steps):
            sb = emit_qk(h, j, i0, i1)
            pend.append(((sb, h, j, i0, i1, g, ob, started), post if si == len(steps) - 1 else None))
            flush(NSB - 2)

    for g in range(8):
        y2 = g % 2
        for h in range(4):
            if isA:
                ob = 4 + cnt["ob"] % 2
                cnt["ob"] += 1
                steps = []
                for j in range(max(0, 4 * g - 16), 4 * g + 4):
                    i0, i1 = max(4 * g, j), min(4 * g + 3, j + 16)
                    if i0 <= i1:
                        steps.append((j, i0, i1))
                def postA(ob=ob, h=h, y2=y2, g=g, r2=cnt["ob"] % 2):
                    src = bank[ob][:, 0:260].rearrange("p (i e) -> p i e", i=4)
                    P.op("dve", lambda e: e.reciprocal(out=rc[r2].unsqueeze(2), in_=src[:, :, 64:65]), reads=[rb[ob]], writes=[r_rc[r2]])
                    P.op("dve", lambda e: e.tensor_tensor(out=yst[y2][:, :, h * 64:(h + 1) * 64], in0=src[:, :, 0:64],
                                                          in1=rc[r2].unsqueeze(2).to_broadcast([128, 4, 64]), op=ALU.mult),
                         reads=[rb[ob], r_rc[r2]], writes=[r_yst[y2]])
                    if h == 3:
                        store_y(g, y2)
                run_steps(steps, h, g, ob, postA)
            else:
                a2 = h % 2
                for n in range(0, 2 * g + 2):
                    ob = 4 + cnt["ob"] % 3
                    cnt["ob"] += 1
                    steps = []
                    for j in (2 * n, 2 * n + 1):
                        i0, i1 = max(4 * g, j), 4 * g + 3
                        if i0 <= i1:
                            steps.append((j, i0, i1))
                    def postC(ob=ob, h=h, y2=y2, g=g, n=n, a2=a2, last=(n == 2 * g + 1)):
                        ia = max(4 * g, 2 * n) - 4 * g
                        ni = 4 - ia
                        src = bank[ob][:, 0:260].rearrange("p (i e) -> p i e", i=4)[:, ia:4, :]
                        selv = selall[:, 4 * g + ia:4 * g + 4, h * 16 + n:h * 16 + n + 1].to_broadcast([128, ni, 65])
                        if n == 0:
                            P.op("dve", lambda e: e.tensor_tensor(out=acc[a2], in0=src, in1=selv, op=ALU.mult),
                                 reads=[rb[ob], r_sel], writes=[r_acc[a2]])
                        else:
                            t3 = cnt["tmp"] % 3
                            cnt["tmp"] += 1
                            P.op("dve", lambda e: e.tensor_tensor(out=tmp[t3][:, ia:4, :], in0=src, in1=selv, op=ALU.mult),
                                 reads=[rb[ob], r_sel], writes=[r_tmp[t3]])
                            P.op("pool", lambda e: e.tensor_tensor(out=acc[a2][:, ia:4, :], in0=acc[a2][:, ia:4, :], in1=tmp[t3][:, ia:4, :], op=ALU.add),
                                 reads=[r_tmp[t3], r_acc[a2]], writes=[r_acc[a2]])
                        if last:
                            r2 = h % 2
                            P.op("dve", lambda e: e.reciprocal(out=rc[r2].unsqueeze(2), in_=acc[a2][:, :, 64:65]), reads=[r_acc[a2]], writes=[r_rc[r2]])
                            P.op("dve", lambda e: e.tensor_tensor(out=yst[y2][:, :, h * 64:(h + 1) * 64], in0=acc[a2][:, :, 0:64],
                                                                  in1=rc[r2].unsqueeze(2).to_broadcast([128, 4, 64]), op=ALU.mult),
                                 reads=[r_acc[a2], r_rc[r2]], writes=[r_yst[y2]])
                            if h == 3:
                                store_y(g, y2)
                    run_steps(steps, h, g, ob, postC)
    flush(0)
    P.end_phase()


def phase_D(C, l):
    nc, P, D = C.nc, C.P, C.D
    A = C.arena
    A.off = C.base_off
    bank, rb = C.bank, C.rb
    tri = A.alloc(128)
    upp = A.alloc(128)
    caus = A.alloc(128)
    ones = A.alloc(128)
    r_k = P.res("dconst", dma=True)
    P.dma("sp", tri, D["c_tri"], writes=[r_k], owner=r_k)
    P.dma("sp", upp, D["c_upp"], writes=[r_k], owner=r_k)
    P.dma("sp", caus, D["c_caus"], writes=[r_k], owner=r_k)
    P.op("pool", lambda e: e.memset(ones, 1.0), writes=[r_k])
    cw = A.alloc(32).rearrange("p (m j) -> p m j", m=8)
    cb = A.alloc(8)
    for j in range(4):
        P.dma("sp", cw[:, :, j], D["conv_w"][l, j].rearrange("(m p) -> p m", p=128), writes=[r_k], owner=r_k, allow_slow_non_contiguous=True)
    P.dma("sp", cb, D["conv_b"][l].rearrange("(m p) -> p m", p=128), writes=[r_k], owner=r_k, allow_slow_non_contiguous=True)
    dtb = A.alloc(8)
    alog = A.alloc(8)
    dsk = A.alloc(8)
    nw = A.alloc(512)
    P.dma("sp", dtb, bcast_rows(D["dt_bias"][l]), writes=[r_k], owner=r_k)
    P.dma("sp", alog, bcast_rows(D["a_log"][l]), writes=[r_k], owner=r_k)
    P.dma("sp", dsk, bcast_rows(D["d_skip"][l]), writes=[r_k], owner=r_k)
    P.dma("sp", nw, bcast_rows(D["ssm_norm_w"][l]), writes=[r_k], owner=r_k)
    dtr = A.alloc(256).rearrange("p (t h) -> p t h", t=NT)
    r_dtr = P.res("dtr", dma=True)
    dsrc = D["dtr"].rearrange("(t p) h -> p t h", p=128)
    for q4 in range(4):
        P.dma("sp", dtr[:, q4 * 8:(q4 + 1) * 8, :], dsrc[:, q4 * 8:(q4 + 1) * 8, :], writes=[r_dtr], owner=r_dtr)

    BT = A.alloc(2 * S, BF16).rearrange("p (g n) -> p g n", g=2)
    CT = A.alloc(2 * S, BF16).rearrange("p (g n) -> p g n", g=2)
    r_BT, r_CT = P.res("BT"), P.res("CT")
    xtok = A.alloc(NT * 512).rearrange("p (t c) -> p t c", t=NT)
    r_xtok = P.res("xtok")
    Btok = A.alloc(NT * 256, BF16).rearrange("p (t c) -> p t c", t=NT)
    r_Btok = P.res("Btok")
    mark = A.off
    xin2 = [A.alloc(S + 8) for _ in range(2)]
    r_xin2 = [P.res("xin%d" % i, dma=True) for i in range(2)]
    cacc2 = [A.alloc(S) for _ in range(2)]
    r_cacc2 = [P.res("cacc%d" % i) for i in range(2)]
    for i in range(2):
        P.op("dve", lambda e, i=i: e.memset(xin2[i][:, 0:8], 0.0), writes=[r_xin2[i]])

    tcnt = 0
    for m in range(8):
        xin, r_xin, cacc, r_cacc = xin2[m % 2], r_xin2[m % 2], cacc2[m % 2], r_cacc2[m % 2]
        P.dma("sp", xin[:, 3:3 + S], D["xbcT"][m * 128:(m + 1) * 128, :], writes=[r_xin], owner=r_xin)
        P.op("dve", lambda e, m=m, xin=xin, cacc=cacc: e.tensor_scalar(out=cacc, in0=xin[:, 3:3 + S], scalar1=cw[:, m, 3:4], scalar2=None, op0=ALU.mult),
             reads=[r_xin, r_k], writes=[r_cacc])
        P.op("dve", lambda e, m=m, xin=xin, cacc=cacc: e.scalar_tensor_tensor(out=cacc, in0=xin[:, 2:2 + S], scalar=cw[:, m, 2:3], in1=cacc, op0=ALU.mult, op1=ALU.add),
             reads=[r_xin, r_k, r_cacc], writes=[r_cacc])
        P.op("dve", lambda e, m=m, xin=xin, cacc=cacc: e.scalar_tensor_tensor(out=cacc, in0=xin[:, 1:1 + S], scalar=cw[:, m, 1:2], in1=cacc, op0=ALU.mult, op1=ALU.add),
             reads=[r_xin, r_k, r_cacc], writes=[r_cacc])
        P.op("dve", lambda e, m=m, xin=xin, cacc=cacc: e.scalar_tensor_tensor(out=cacc, in0=xin[:, 0:S], scalar=cw[:, m, 0:1], in1=cacc, op0=ALU.mult, op1=ALU.add),
             reads=[r_xin, r_k, r_cacc], writes=[r_cacc])
        if m < 4:
            P.op("act", lambda e, m=m, xin=xin, cacc=cacc: e.activation(out=cacc, in_=cacc, func=AF.Silu, bias=cb[:, m:m + 1]), reads=[r_cacc, r_k], writes=[r_cacc])
            for c0 in range(0, NT, 4):
                bk = tcnt % 2
                tcnt += 1
                for cc in range(4):
                    c = c0 + cc
                    P.op("pe", lambda e, c=c, cc=cc, bk=bk, cacc=cacc: e.transpose(out=bank[bk][:, cc * 128:(cc + 1) * 128], in_=cacc[:, c * 128:(c + 1) * 128], identity=C.ident_f),
                         reads=[r_cacc], writes=[rb[bk]])
                eng = "act" if (tcnt % 2) else "dve"
                if eng == "act":
                    P.op("act", lambda e, c0=c0, m=m, bk=bk: e.copy(out=xtok[:, c0:c0 + 4, m * 128:(m + 1) * 128], in_=bank[bk][:].rearrange("p (c n) -> p c n", c=4)),
                         reads=[rb[bk]], writes=[r_xtok])
                else:
                    P.op("dve", lambda e, c0=c0, m=m, bk=bk: e.tensor_copy(out=xtok[:, c0:c0 + 4, m * 128:(m + 1) * 128], in_=bank[bk][:].rearrange("p (c n) -> p c n", c=4)),
                         reads=[rb[bk]], writes=[r_xtok])
        elif m < 6:
            gg = m - 4
            P.op("act", lambda e, m=m, gg=gg, cacc=cacc: e.activation(out=BT[:, gg, :], in_=cacc, func=AF.Silu, bias=cb[:, m:m + 1]), reads=[r_cacc, r_k], writes=[r_BT])
            for c0 in range(0, NT, 8):
                bk = tcnt % 2
                tcnt += 1
                pb = bank[bk][:].bitcast(BF16)
                for cc in range(8):
                    c = c0 + cc
                    P.op("pe", lambda e, c=c, cc=cc, pb=pb, gg=gg: e.transpose(out=pb[:, cc * 128:(cc + 1) * 128], in_=BT[:, gg, c * 128:(c + 1) * 128], identity=C.ident_b),
                         reads=[r_BT], writes=[rb[bk]])
                P.op("act", lambda e, c0=c0, gg=gg, pb=pb: e.copy(out=Btok[:, c0:c0 + 8, gg * 128:(gg + 1) * 128], in_=pb.rearrange("p (c n) -> p c n", c=8)),
                     reads=[rb[bk]], writes=[r_Btok])
        else:
            gg = m - 6
            P.op("act", lambda e, m=m, gg=gg, cacc=cacc: e.activation(out=CT[:, gg, :], in_=cacc, func=AF.Silu, bias=cb[:, m:m + 1]), reads=[r_cacc, r_k], writes=[r_CT])
    P.barrier()
    A.off = mark

    W = 256
    dtx = A.alloc(W)
    t_ax = A.alloc(W)
    t_e = A.alloc(W)
    dt_all = A.alloc(W)
    aneg = A.alloc(8)
    da_all = A.alloc(W)
    ea_all = A.alloc(W)
    ds_all = A.alloc(W)
    cd_all = A.alloc(W)
    dtds = A.alloc(W)
    r_t = P.res("dtables")
    v3 = lambda ap: ap.rearrange("p (t h) -> p t h", t=NT)
    P.op("dve", lambda e: e.tensor_tensor(out=v3(dtx), in0=dtr, in1=dtb.unsqueeze(1).to_broadcast([128, NT, 8]), op=ALU.add), reads=[r_dtr, r_k], writes=[r_t])
    P.op("dve", lambda e: e.scalar_tensor_tensor(out=t_ax, in0=dtx, scalar=-1.0, in1=dtx, op0=ALU.mult, op1=ALU.min), reads=[r_t], writes=[r_t])
    P.op("act", lambda e: e.activation(out=t_e, in_=t_ax, func=AF.Exp), reads=[r_t], writes=[r_t])
    P.op("act", lambda e: e.activation(out=t_e, in_=t_e, func=AF.Ln, bias=C.one_t[:, 0:1]), reads=[r_t], writes=[r_t])
    P.op("dve", lambda e: e.tensor_scalar_max(out=t_ax, in0=dtx, scalar1=0.0), reads=[r_t], writes=[r_t])
    P.op("dve", lambda e: e.tensor_tensor(out=dt_all, in0=t_ax, in1=t_e, op=ALU.add), reads=[r_t], writes=[r_t])
    P.op("act", lambda e: e.activation(out=aneg, in_=alog, func=AF.Exp), reads=[r_k], writes=[r_t])
    P.op("dve", lambda e: e.tensor_scalar(out=aneg, in0=aneg, scalar1=-1.0, scalar2=None, op0=ALU.mult), reads=[r_t], writes=[r_t])
    P.op("dve", lambda e: e.tensor_tensor(out=v3(da_all), in0=v3(dt_all), in1=aneg.unsqueeze(1).to_broadcast([128, NT, 8]), op=ALU.mult), reads=[r_t], writes=[r_t])
    for (lhs, dst) in ((tri, ea_all), (upp, ds_all), (ones, cd_all)):
        P.op("pe", lambda e, lhs=lhs: e.matmul(bank[2][:, 0:W], lhsT=lhs, rhs=da_all, start=True, stop=True), reads=[r_t, r_k], writes=[rb[2]])
        P.op("act", lambda e, dst=dst: e.activation(out=dst, in_=bank[2][:, 0:W], func=AF.Exp), reads=[rb[2]], writes=[r_t])
    P.op("dve", lambda e: e.tensor_tensor(out=dtds, in0=dt_all, in1=ds_all, op=ALU.mult), reads=[r_t], writes=[r_t])

    NB = 4
    mk3 = lambda n, dt=F32: [A.alloc(n, dt).rearrange("p (h n) -> p h n", h=4) for _ in range(NB)]
    rT, r_rT = mk3(512), [P.res("rT%d" % i) for i in range(NB)]
    Lt, r_Lt = mk3(512), [P.res("Lt%d" % i) for i in range(NB)]
    Gm, r_Gm = [A.alloc(128) for _ in range(NB)], [P.res("Gm%d" % i) for i in range(NB)]
    sc, r_sc = mk3(512, BF16), [P.res("sc%d" % i) for i in range(NB)]
    xd, r_xd = mk3(256, BF16), [P.res("xd%d" % i) for i in range(NB)]
    xdd, r_xdd = mk3(256, BF16), [P.res("xdd%d" % i) for i in range(NB)]
    t1, r_t1 = mk3(256), [P.res("t1%d" % i) for i in range(NB)]
    t3, r_t3 = mk3(256), [P.res("t3%d" % i) for i in range(NB)]
    St = [A.alloc(256).rearrange("p (h n) -> p h n", h=4) for _ in range(2)]
    r_St = [P.res("St%d" % i) for i in range(2)]
    tS = [A.alloc(256).rearrange("p (h n) -> p h n", h=4) for _ in range(2)]
    r_tS = [P.res("tS%d" % i) for i in range(2)]
    Sbf = [A.alloc(256, BF16) for _ in range(2)]
    r_Sbf = [P.res("Sbf%d" % i) for i in range(2)]
    yg = [A.alloc(512) for _ in range(2)]
    r_yg = [P.res("yg%d" % i) for i in range(2)]
    zt = [A.alloc(512) for _ in range(2)]
    r_zt = [P.res("zt%d" % i, dma=True) for i in range(2)]
    junk = A.alloc(256)
    r_junk = P.res("junkD")
    nst = [A.alloc(8) for _ in range(2)]
    r_nst = [P.res("nst%d" % i) for i in range(2)]
    yo = [A.alloc(512, BF16) for _ in range(2)]
    r_yo = [P.res("yo%d" % i, dma=True) for i in range(2)]

    def b4(ap2d, c, g):
        return v3(ap2d)[:, c, g * 4:g * 4 + 4].unsqueeze(2).to_broadcast([128, 4, 64])

    def xg_of(c, g):
        return xtok[:, c, g * 256:(g + 1) * 256].rearrange("p (h n) -> p h n", h=4)

    def S0(it):
        c, g = it // 2, it % 2
        b = it % NB
        bX, bY = it % 3, 6 + it % 2
        csl = slice(c * 128, (c + 1) * 128)
        xg = xg_of(c, g)
        P.op("dve", lambda e: e.tensor_tensor(out=rT[b], in0=tri.unsqueeze(1).to_broadcast([128, 4, 128]),
                                              in1=v3(da_all)[:, c, g * 4:g * 4 + 4].unsqueeze(2).to_broadcast([128, 4, 128]), op=ALU.mult),
             reads=[r_k, r_t], writes=[r_rT[b]])
        P.op("pe", lambda e: e.matmul(bank[bX][:], lhsT=upp, rhs=rT[b].rearrange("p h n -> p (h n)"), start=True, stop=True),
             reads=[r_k, r_rT[b]], writes=[rb[bX]])
        P.op("pe", lambda e: e.matmul(bank[bY][:, 0:128], lhsT=BT[:, g, csl], rhs=CT[:, g, csl], start=True, stop=True),
             reads=[r_BT, r_CT], writes=[rb[bY]])
        P.op("pool", lambda e: e.tensor_tensor(out=xd[b], in0=xg, in1=b4(dt_all, c, g), op=ALU.mult), reads=[r_xtok, r_t], writes=[r_xd[b]])
        P.op("pool", lambda e: e.tensor_tensor(out=xdd[b], in0=xg, in1=b4(dtds, c, g), op=ALU.mult), reads=[r_xtok, r_t], writes=[r_xdd[b]])
        P.op("pool", lambda e: e.tensor_tensor(out=t3[b], in0=xg, in1=dsk[:, g * 4:g * 4 + 4].unsqueeze(2).to_broadcast([128, 4, 64]), op=ALU.mult),
             reads=[r_xtok, r_k], writes=[r_t3[b]])

    def S1(it):
        b = it % NB
        bX, bY = it % 3, 6 + it % 2
        P.op("act", lambda e: e.activation(out=Lt[b].rearrange("p h n -> p (h n)"), in_=bank[bX][:], func=AF.Exp), reads=[rb[bX]], writes=[r_Lt[b]])
        P.op("dve", lambda e: e.tensor_tensor(out=Gm[b], in0=bank[bY][:, 0:128], in1=caus, op=ALU.mult), reads=[rb[bY], r_k], writes=[r_Gm[b]])
        P.op("dve", lambda e: e.tensor_tensor(out=sc[b], in0=Lt[b], in1=Gm[b].unsqueeze(1).to_broadcast([128, 4, 128]), op=ALU.mult),
             reads=[r_Lt[b], r_Gm[b]], writes=[r_sc[b]])

    def S2(it):
        c, g = it // 2, it % 2
        b = it % NB
        bZ, bW = 3 + it % 3, 6 + it % 2
        csl = slice(c * 128, (c + 1) * 128)
        for hh in range(4):
            P.op("pe", lambda e, hh=hh: e.matmul(bank[bZ][:, hh * 64:(hh + 1) * 64], lhsT=sc[b][:, hh, :], rhs=xd[b][:, hh, :], start=True, stop=True),
                 reads=[r_sc[b], r_xd[b]], writes=[rb[bZ]])
        if c > 0:
            P.op("pe", lambda e: e.matmul(bank[bZ][:, 256:512], lhsT=CT[:, g, csl], rhs=Sbf[g], start=True, stop=True),
                 reads=[r_CT, r_Sbf[g]], writes=[rb[bZ]])
        if c + 1 < NT:
            P.op("pe", lambda e: e.matmul(bank[bW][:, 256:512], lhsT=Btok[:, c, g * 128:(g + 1) * 128], rhs=xdd[b].rearrange("p h n -> p (h n)"), start=True, stop=True),
                 reads=[r_Btok, r_xdd[b]], writes=[rb[bW]])

    def S3(it):
        c, g = it // 2, it % 2
        b = it % NB
        c2 = c % 2
        bZ, bW = 3 + it % 3, 6 + it % 2
        if c + 1 < NT:
            pw = bank[bW][:, 256:512].rearrange("p (h n) -> p h n", h=4)
            if c == 0:
                P.op("dve", lambda e: e.tensor_copy(out=St[g], in_=pw), reads=[rb[bW]], writes=[r_St[g]])
            else:
                P.op("dve", lambda e: e.tensor_tensor(out=tS[g], in0=St[g], in1=b4(cd_all, c, g), op=ALU.mult), reads=[r_St[g], r_t], writes=[r_tS[g]])
                P.op("dve", lambda e: e.tensor_tensor(out=St[g], in0=tS[g], in1=pw, op=ALU.add), reads=[r_tS[g], rb[bW]], writes=[r_St[g]])
            P.op("act", lambda e: e.copy(out=Sbf[g], in_=St[g].rearrange("p h n -> p (h n)")), reads=[r_St[g]], writes=[r_Sbf[g]])
        pz = bank[bZ][:]
        if c > 0:
            P.op("dve", lambda e: e.tensor_tensor(out=t1[b], in0=pz[:, 256:512].rearrange("p (h n) -> p h n", h=4), in1=b4(ea_all, c, g), op=ALU.mult),
                 reads=[rb[bZ], r_t], writes=[r_t1[b]])
            P.op("dve", lambda e: e.tensor_tensor(out=t1[b], in0=t1[b], in1=pz[:, 0:256].rearrange("p (h n) -> p h n", h=4), op=ALU.add),
                 reads=[rb[bZ], r_t1[b]], writes=[r_t1[b]])
        else:
            P.op("dve", lambda e: e.tensor_copy(out=t1[b], in_=pz[:, 0:256].rearrange("p (h n) -> p h n", h=4)), reads=[rb[bZ]], writes=[r_t1[b]])
        P.op("pool", lambda e: e.tensor_tensor(out=yg[c2][:, g * 256:(g + 1) * 256].rearrange("p (h n) -> p h n", h=4), in0=t1[b], in1=t3[b], op=ALU.add),
             reads=[r_t1[b], r_t3[b]], writes=[r_yg[c2]])

    def S4(c):
        c2 = c % 2
        if c + 2 < NT:
            pass
        P.op("act", lambda e: e.activation(out=zt[c2], in_=zt[c2], func=AF.Silu), reads=[r_zt[c2]], writes=[r_zt[c2]])
        P.op("dve", lambda e: e.tensor_tensor(out=yg[c2], in0=yg[c2], in1=zt[c2], op=ALU.mult), reads=[r_yg[c2], r_zt[c2]], writes=[r_yg[c2]])
        for g in range(2):
            P.op("act", lambda e, g=g: e.activation(out=junk, in_=yg[c2][:, g * 256:(g + 1) * 256], func=AF.Square, accum_out=nst[c2][:, g:g + 1]),
                 reads=[r_yg[c2]], writes=[r_junk, r_nst[c2]])
        P.op("act", lambda e: e.activation(out=nst[c2][:, 2:4], in_=nst[c2][:, 0:2], func=AF.Sqrt, scale=1.0 / 256, bias=C.eps_t[:, 0:1]), reads=[r_nst[c2]], writes=[r_nst[c2]])
        P.op("dve", lambda e: e.reciprocal(out=nst[c2][:, 4:6], in_=nst[c2][:, 2:4]), reads=[r_nst[c2]], writes=[r_nst[c2]])
        P.op("dve", lambda e: e.tensor_tensor(out=yg[c2].rearrange("p (g n) -> p g n", g=2), in0=yg[c2].rearrange("p (g n) -> p g n", g=2),
                                              in1=nst[c2][:, 4:6].unsqueeze(2).to_broadcast([128, 2, 256]), op=ALU.mult),
             reads=[r_yg[c2], r_nst[c2]], writes=[r_yg[c2]])
        P.op("pool", lambda e: e.tensor_tensor(out=yo[c2], in0=yg[c2], in1=nw, op=ALU.mult), reads=[r_yg[c2], r_k], writes=[r_yo[c2]])
        P.dma("sp", D["Y"][c * 128:(c + 1) * 128, 256:768], yo[c2], reads=[r_yo[c2]], owner=r_yo[c2])
        if c + 2 < NT:
            P.dma("sp", zt[c2], D["z"][(c + 2) * 128:(c + 3) * 128, :], writes=[r_zt[c2]], owner=r_zt[c2])

    P.dma("sp", zt[0], D["z"][0:128, :], writes=[r_zt[0]], owner=r_zt[0])
    P.dma("sp", zt[1], D["z"][128:256, :], writes=[r_zt[1]], owner=r_zt[1])
    NI = 2 * NT
    for s_ in range(NI + 3):
        if s_ < NI:
            S0(s_)
        if 0 <= s_ - 1 < NI:
            S1(s_ - 1)
        if 0 <= s_ - 2 < NI:
            S2(s_ - 2)
        if 0 <= s_ - 3 < NI:
            S3(s_ - 3)
            if (s_ - 3) % 2 == 1:
                S4((s_ - 3) // 2)
    P.end_phase()


def phase_E(C, l, x_src, x_dst):
    nc, P, D = C.nc, C.P, C.D
    A = C.arena
    A.off = C.base_off
    bank, rb = C.bank, C.rb
    w1 = A.alloc(8 * DFF, BF16).rearrange("p (k n) -> p k n", k=8)
    w2 = A.alloc(32 * DM, BF16).rearrange("p (f n) -> p f n", f=32)
    r_w1 = P.res("w1", dma=True)
    r_w2 = P.res("w2", dma=True)
    mark = A.off
    wout = A.alloc(8 * DM, BF16).rearrange("p (k n) -> p k n", k=8)
    r_wout = P.res("wout", dma=True)
    for k in range(8):
        P.dma("pool", wout[:, k, :], D["w_out"][l, k * 128:(k + 1) * 128, :], writes=[r_wout], owner=r_wout)
    for k in range(8):
        P.dma("pool", w1[:, k, :], D["w_mlp_in"][l, k * 128:(k + 1) * 128, :], writes=[r_w1], owner=r_w1)
    w2src = D["w_mlp_out"][l].rearrange("(f p) n -> p f n", p=128)
    for q in range(8):
        P.dma("pool", w2[:, q * 4:(q + 1) * 4, :], w2src[:, q * 4:(q + 1) * 4, :], writes=[r_w2], owner=r_w2)
    n2w = A.alloc(DM)
    r_n2w = P.res("n2w", dma=True)
    P.dma("sp", n2w, bcast_rows(D["norm2_w"][l]), writes=[r_n2w], owner=r_n2w)
    yt = [A.alloc(DM, BF16) for _ in range(2)]
    r_yt = [P.res("yt%d" % i, dma=True) for i in range(2)]
    xs = [A.alloc(DM) for _ in range(2)]
    r_xs = [P.res("xsE%d" % i, dma=True) for i in range(2)]
    yT = [A.alloc(DM, BF16).rearrange("p (k n) -> p k n", k=8) for _ in range(2)]
    r_yT = [P.res("yT%d" % i) for i in range(2)]
    x1t = [A.alloc(DM) for _ in range(2)]
    r_x1t = [P.res("x1t%d" % i, dma=True) for i in range(2)]
    junk = A.alloc(DM)
    r_junk = P.res("junkE")
    st = [A.alloc(8) for _ in range(2)]
    r_st = [P.res("stE%d" % i) for i in range(2)]
    h2b = [A.alloc(DM, BF16) for _ in range(2)]
    r_h2b = [P.res("h2b%d" % i) for i in range(2)]
    h2s = [A.alloc(DM, BF16).rearrange("p (k n) -> p k n", k=8) for _ in range(2)]
    r_h2s = [P.res("h2s%d" % i, dma=True) for i in range(2)]
    Yv = D["Y"].rearrange("(t p) c -> t p c", p=128)
    xv = x_src.rearrange("(t p) c -> t p c", p=128)
    x1v = D["x1"].rearrange("(t p) c -> t p c", p=128)
    h2Tv = D["h2T"].rearrange("(k p) s -> p k s", p=128)
    psA = bank[0][:].bitcast(BF16)
    psB = bank[3][:].bitcast(BF16)

    def loadE1(t):
        P.dma("sp", yt[t % 2], Yv[t], writes=[r_yt[t % 2]], owner=r_yt[t % 2])
        P.dma("sp", xs[t % 2], xv[t], writes=[r_xs[t % 2]], owner=r_xs[t % 2])

    def stageY(t):
        p2 = t % 2
        if t + 1 < NT:
            loadE1(t + 1)
        for k in range(8):
            P.op("pe", lambda e, k=k: e.transpose(out=psA[:, k * 128:(k + 1) * 128], in_=yt[p2][:, k * 128:(k + 1) * 128], identity=C.ident_b),
                 reads=[r_yt[p2]], writes=[rb[0]])
        P.op("act", lambda e: e.copy(out=yT[p2], in_=psA.rearrange("p (k n) -> p k n", k=8)), reads=[rb[0]], writes=[r_yT[p2]])

    def stageP(t):
        p2 = t % 2
        for cg in range(2):
            bk = 1 + cg
            for k in range(8):
                P.op("pe", lambda e, k=k, cg=cg, bk=bk: e.matmul(bank[bk][:], lhsT=yT[p2][:, k, :], rhs=wout[:, k, cg * 512:(cg + 1) * 512], start=(k == 0), stop=(k == 7)),
                     reads=[r_yT[p2], r_wout], writes=[rb[bk]])
            P.op("dve", lambda e, cg=cg, bk=bk: e.tensor_tensor(out=x1t[p2][:, cg * 512:(cg + 1) * 512], in0=xs[p2][:, cg * 512:(cg + 1) * 512], in1=bank[bk][:], op=ALU.add),
                 reads=[rb[bk], r_xs[p2]], writes=[r_x1t[p2]])
        P.dma("sp", x1v[t], x1t[p2], reads=[r_x1t[p2]], owner=r_x1t[p2])
        P.op("act", lambda e: e.activation(out=junk, in_=x1t[p2], func=AF.Square, accum_out=st[p2][:, 0:1]), reads=[r_x1t[p2]], writes=[r_junk, r_st[p2]])
        P.op("act", lambda e: e.activation(out=st[p2][:, 1:2], in_=st[p2][:, 0:1], func=AF.Sqrt, scale=1.0 / DM, bias=C.eps_t[:, 0:1]), reads=[r_st[p2]], writes=[r_st[p2]])
        P.op("dve", lambda e: e.reciprocal(out=st[p2][:, 2:3], in_=st[p2][:, 1:2]), reads=[r_st[p2]], writes=[r_st[p2]])
        P.op("dve", lambda e: e.scalar_tensor_tensor(out=h2b[p2], in0=x1t[p2], scalar=st[p2][:, 2:3], in1=n2w, op0=ALU.mult, op1=ALU.mult),
             reads=[r_x1t[p2], r_st[p2], r_n2w], writes=[r_h2b[p2]])

    def stageH(t):
        p2 = t % 2
        for k in range(8):
            P.op("pe", lambda e, k=k: e.transpose(out=psB[:, k * 128:(k + 1) * 128], in_=h2b[p2][:, k * 128:(k + 1) * 128], identity=C.ident_b),
                 reads=[r_h2b[p2]], writes=[rb[3]])
        P.op("act", lambda e: e.copy(out=h2s[p2], in_=psB.rearrange("p (k n) -> p k n", k=8)), reads=[rb[3]], writes=[r_h2s[p2]])
        P.dma("sp", h2Tv[:, :, t * 128:(t + 1) * 128], h2s[p2], reads=[r_h2s[p2]], owner=r_h2s[p2])

    loadE1(0)
    stageY(0)
    for t in range(NT):
        stageP(t)
        if t + 1 < NT:
            stageY(t + 1)
        if t > 0:
            stageH(t - 1)
    stageH(NT - 1)
    P.barrier()

    A.off = mark
    hid = A.alloc(32 * 512, BF16).rearrange("p (f n) -> p f n", f=32)
    r_hid = [P.res("hid%d" % f) for f in range(32)]
    h2g = [A.alloc(8 * 512, BF16).rearrange("p (k n) -> p k n", k=8) for _ in range(2)]
    r_h2g = [P.res("h2g%d" % i, dma=True) for i in range(2)]
    rl = [A.alloc(512) for _ in range(2)]
    r_rl = [P.res("rl%d" % i) for i in range(2)]
    x1b = [A.alloc(DM) for _ in range(2)]
    r_x1b = [P.res("x1b%d" % i, dma=True) for i in range(2)]
    ot = [A.alloc(DM) for _ in range(2)]
    r_ot = [P.res("ot%d" % i, dma=True) for i in range(2)]
    ov = x_dst.rearrange("(t p) c -> t p c", p=128)
    P.dma("sp", h2g[0], h2Tv[:, :, 0:512], writes=[r_h2g[0]], owner=r_h2g[0])
    fcnt = 0
    for g in range(8):
        g2 = g % 2
        if g + 1 < 8:
            P.dma("sp", h2g[(g + 1) % 2], h2Tv[:, :, (g + 1) * 512:(g + 2) * 512], writes=[r_h2g[(g + 1) % 2]], owner=r_h2g[(g + 1) % 2])
        for f in range(32):
            bk = fcnt % 4
            r2 = fcnt % 2
            fcnt += 1
            for k in range(8):
                P.op("pe", lambda e, k=k, f=f, bk=bk, g2=g2: e.matmul(bank[bk][:], lhsT=w1[:, k, f * 128:(f + 1) * 128], rhs=h2g[g2][:, k, :], start=(k == 0), stop=(k == 7)),
                     reads=[r_w1, r_h2g[g2]], writes=[rb[bk]])
            P.op("act", lambda e, bk=bk, r2=r2: e.activation(out=rl[r2], in_=bank[bk][:], func=AF.Relu), reads=[rb[bk]], writes=[r_rl[r2]])
            P.op("dve", lambda e, bk=bk, r2=r2, f=f: e.tensor_tensor(out=hid[:, f, :], in0=rl[r2], in1=bank[bk][:], op=ALU.mult), reads=[rb[bk], r_rl[r2]], writes=[r_hid[f]])
        for i in range(4):
            t = 4 * g + i
            p2 = t % 2
            P.dma("sp", x1b[p2], x1v[t], writes=[r_x1b[p2]], owner=r_x1b[p2])
            for cg in range(2):
                bk = 4 + (2 * i + cg) % 4
                for f in range(32):
                    P.op("pe", lambda e, f=f, cg=cg, bk=bk, i=i: e.matmul(bank[bk][:], lhsT=hid[:, f, i * 128:(i + 1) * 128], rhs=w2[:, f, cg * 512:(cg + 1) * 512], start=(f == 0), stop=(f == 31)),
                         reads=[r_hid[f], r_w2], writes=[rb[bk]])
                P.op("dve", lambda e, cg=cg, bk=bk, p2=p2: e.tensor_tensor(out=ot[p2][:, cg * 512:(cg + 1) * 512], in0=x1b[p2][:, cg * 512:(cg + 1) * 512], in1=bank[bk][:], op=ALU.add),
                     reads=[rb[bk], r_x1b[p2]], writes=[r_ot[p2]])
            P.dma("sp", ov[t], ot[p2], reads=[r_ot[p2]], owner=r_ot[p2])
    P.end_phase()


def build(layers=(0, 1), phases=("A", "B", "C", "D", "E"), feed=(), expose=()):
    nc = bass.Bass("TRN2", target_bir_lowering=False)
    D = {}
    D["x"] = nc.dram_tensor("x", [S, DM], F32, kind="ExternalInput").ap()
    for n, shp in PARAM_SHAPES.items():
        D[n] = nc.dram_tensor(n, shp, F32, kind="ExternalInput").ap()
    for n, shp in CONST_SHAPES.items():
        D["c_" + n] = nc.dram_tensor("c_" + n, shp, BF16 if n.startswith("mask") else F32, kind="ExternalInput").ap()
    for n, (shp, dt) in SCRATCH.items():
        kind = "ExternalInput" if n in feed else ("ExternalOutput" if n in expose else "Internal")
        D[n] = nc.dram_tensor(n, shp, dt, kind=kind).ap()
    D["out"] = nc.dram_tensor("out", [S, DM], F32, kind="ExternalOutput").ap()

    with ExitStack() as es:
        C = Ctx()
        C.nc, C.D = nc, D
        C.P = P = Prog(nc, es)
        C.arena = A = Arena(nc)
        C.bank = [nc.alloc_psum_tensor("bank%d" % i, [128, 512], F32) for i in range(8)]
        C.rb = [P.res("bank%d" % i) for i in range(8)]
        for r in C.rb:
            r.excl = True
        idf = A.alloc(128)
        C.ident_f = idf
        C.ident_b = A.alloc(128, BF16)
        C.eps_t = A.alloc(8)
        C.pastm = A.alloc(32)
        r_c = P.res("consts", dma=True)
        P.dma("sp", idf, D["c_ident"], writes=[r_c], owner=r_c)
        P.dma("sp", C.pastm, D["c_pastm"], writes=[r_c], owner=r_c)
        P.op("dve", lambda e: e.tensor_copy(out=C.ident_b, in_=idf), reads=[r_c], writes=[r_c])
        P.op("dve", lambda e: e.memset(C.eps_t, EPS), writes=[r_c])
        C.one_t = A.alloc(8)
        P.op("dve", lambda e: e.memset(C.one_t, 1.0), writes=[r_c])
        P.barrier()
        C.base_off = A.off

        for l in layers:
            x_src = D["x"] if l == 0 else D["xcur"]
            x_dst = D["xcur"] if l == 0 else D["out"]
            if "A" in phases:
                phase_A(C, l, x_src)
            if "B" in phases:
                phase_attn(C, l, "A")
            if "C" in phases:
                phase_attn(C, l, "C")
            if "D" in phases:
                phase_D(C, l)
            if "E" in phases:
                phase_E(C, l, x_src, x_dst)
        stats = P.emit()
    return nc, stats


def make_in_map(inputs, b, consts):
    m = {"x": np.ascontiguousarray(inputs["x"][b])}
    for n in PARAM_SHAPES:
        m[n] = np.ascontiguousarray(inputs[n])
    for n, v in consts.items():
        m["c_" + n] = v
    return m


def kernel(**inputs):
    inputs = {k: np.asarray(v) for k, v in inputs.items()}
    nc, _ = build()
    consts = host_consts()
    in_maps = [make_in_map(inputs, c % 4, consts) for c in range(8)]
    res = run_bass_kernel_spmd(nc, in_maps, core_ids=list(range(8)))
    out = np.stack([res.results[b]["out"] for b in range(4)], axis=0)
    return out.astype(np.float32)
```

```python
import math
from contextlib import ExitStack

import numpy as np
import ml_dtypes

import concourse.bass as bass
import concourse.mybir as mybir
from concourse.bass_utils import run_bass_kernel_spmd

F32 = mybir.dt.float32
BF16 = mybir.dt.bfloat16
ALU = mybir.AluOpType
AF = mybir.ActivationFunctionType
AX = mybir.AxisListType

ENGS = ("pe", "act", "dve", "pool", "sp")
S = 4096
NT = 32
DM = 1024
DIN = 3080
DFF = 4096
EPS = 1e-6
NEG = -1e30
SLOPES = [2.0 ** (-8.0 * i / 8) for i in range(1, 9)]
SL_A = SLOPES[0::2]
SL_C = SLOPES[1::2]


class Slot:
    __slots__ = ("sem", "cnt")

    def __init__(self, sem):
        self.sem = sem
        self.cnt = 0


class Res:
    __slots__ = ("name", "w", "r", "slot", "excl")

    def __init__(self, name, slot=None):
        self.name = name
        self.w = None
        self.r = []
        self.slot = slot
        self.excl = False


class Op:
    __slots__ = ("eng", "fn", "deps", "need_inc", "inc_idx", "kind")

    def __init__(self, eng, fn, deps, kind="c"):
        self.eng = eng
        self.fn = fn
        self.deps = deps
        self.need_inc = False
        self.inc_idx = 0
        self.kind = kind


class Prog:
    def __init__(self, nc, es, n_dma=84):
        self.nc = nc
        self.q = {e: [] for e in ENGS}
        self.esem = {e: es.enter_context(nc.semaphore("prog_" + e)) for e in ENGS if e != "sp"}
        self.slots = [Slot(es.enter_context(nc.semaphore("dq%d" % i))) for i in range(n_dma)]
        self.free = list(self.slots)
        self.phase = []

    def res(self, name, dma=False):
        slot = None
        if dma:
            slot = self.free.pop()
            self.phase.append(slot)
        return Res(name, slot)

    def _collect(self, reads, writes):
        deps = []
        for r in reads:
            if r.w is not None:
                deps.append(r.w)
        for w in writes:
            if w.w is not None:
                deps.append(w.w)
            deps.extend(w.r)
        for d in deps:
            if isinstance(d, Op):
                d.need_inc = True
        return deps

    def op(self, eng, fn, reads=(), writes=()):
        ex = [r for r in reads if r.excl]
        if ex:
            reads = [r for r in reads if not r.excl]
            writes = list(writes) + ex
        o = Op(eng, fn, self._collect(reads, writes))
        self.q[eng].append(o)
        for r in reads:
            r.r.append(o)
        for w in writes:
            w.w = o
            w.r = []
        return o

    def dma(self, queue, out, in_, reads=(), writes=(), owner=None, **kw):
        slot = owner.slot
        deps = self._collect(reads, writes)
        slot.cnt += 16
        tok = ("d", slot, slot.cnt)

        def fn(e, out=out, in_=in_, sem=slot.sem):
            return e.dma_start(out=out, in_=in_, **kw).then_inc(sem, 16)

        self.q[queue].append(Op(queue, fn, deps, kind="dma"))
        for r in reads:
            r.r.append(tok)
        for w in writes:
            w.w = tok
            w.r = []
        return tok

    def barrier(self):
        deps = []
        for e in ENGS:
            for o in reversed(self.q[e]):
                if o.kind == "c":
                    deps.append(o)
                    o.need_inc = True
                    break
        for s in self.slots:
            if s.cnt > 0:
                deps.append(("d", s, s.cnt))
        for e in ENGS:
            self.q[e].append(Op(e, None, list(deps), kind="wait"))

    def end_phase(self):
        self.barrier()
        self.free.extend(self.phase)
        self.phase = []

    def emit(self):
        nc = self.nc
        for e in ENGS:
            c = 0
            for o in self.q[e]:
                if o.kind == "c" and o.need_inc:
                    c += 1
                    o.inc_idx = c
        stats = {}

        def replay(ename, eng):
            observed = {}
            nwait = 0
            for o in self.q[ename]:
                need = {}
                for d in o.deps:
                    if isinstance(d, Op):
                        if d.eng == ename and ename == "pe":
                            continue
                        key = d.eng
                        sem = self.esem[d.eng]
                        val = d.inc_idx
                    else:
                        _, s, val = d
                        key = id(s)
                        sem = s.sem
                    if observed.get(key, 0) >= val:
                        continue
                    if key not in need or need[key][1] < val:
                        need[key] = (sem, val)
                for key, (sem, val) in need.items():
                    eng.wait_ge(sem, val)
                    observed[key] = val
                    nwait += 1
                if o.fn is not None:
                    ins = o.fn(eng)
                    if o.kind == "c" and o.need_inc:
                        ins.then_inc(self.esem[ename], 1)
            stats[ename] = (len(self.q[ename]), nwait)

        with nc.Block() as block:
            @block.tensor
            def _(e):
                replay("pe", e)

            @block.scalar
            def _(e):
                replay("act", e)

            @block.vector
            def _(e):
                replay("dve", e)

            @block.gpsimd
            def _(e):
                replay("pool", e)

            @block.sync
            def _(e):
                replay("sp", e)
        return stats


DT_SIZE = {F32: 4, BF16: 2}


class Arena:
    def __init__(self, nc):
        nbytes = (nc.sbuf_bytes_remaining - 2048) // 64 * 64
        self.words = nbytes // 4
        self.t = nc.alloc_sbuf_tensor("arena", [128, self.words], F32)
        self.off = 0
        self.peak = 0

    def alloc(self, cols, dtype=F32):
        words = (cols * DT_SIZE[dtype] + 3) // 4
        words = (words + 7) // 8 * 8
        assert self.off + words <= self.words, ("SBUF arena overflow", self.off * 4, words * 4, self.words * 4)
        ap = self.t[:, self.off:self.off + words]
        self.off += words
        self.peak = max(self.peak, self.off)
        if dtype != F32:
            ap = ap.bitcast(dtype)
        return ap[:, 0:cols]


def bcast_rows(ap1d, reps=1):
    n = ap1d.shape[0]
    if reps == 1:
        return bass.AP(ap1d.tensor, ap1d.offset, [[0, 128], [1, n]])
    return bass.AP(ap1d.tensor, ap1d.offset, [[0, 128], [0, reps], [1, n]])


def host_consts():
    c = {}
    c["ident"] = np.eye(128, dtype=np.float32)
    kl = np.arange(128)[:, None]
    def mk(nof, slopes, mult_fn):
        col = np.arange(nof * 128)[None, :]
        delta = (col - kl).astype(np.float64)
        out = np.zeros((4, 128, nof * 128), np.float32)
        for h, sl in enumerate(slopes):
            m = mult_fn(delta) * np.exp(-sl * np.maximum(delta, 0.0))
            out[h] = np.where(delta >= 0, m, 0.0).astype(np.float32)
        return out
    def multA(d):
        return ((d <= 128).astype(np.float64) + ((d % 4 == 0) & (d <= 512)) + ((d % 16 == 0) & (d <= 2048)))
    c["maskA"] = mk(17, SL_A, multA).astype(ml_dtypes.bfloat16)
    c["maskC"] = mk(32, SL_C, lambda d: np.ones_like(d)).astype(ml_dtypes.bfloat16)
    k = np.arange(128)[:, None]
    j = np.arange(128)[None, :]
    c["tri"] = (k <= j).astype(np.float32)
    c["upp"] = (k > j).astype(np.float32)
    c["caus"] = (j >= k).astype(np.float32)
    pm = np.zeros((128, 32), np.float32)
    pm[:, 16:] = NEG
    c["pastm"] = pm
    return c


CONST_SHAPES = {"ident": [128, 128], "maskA": [4, 128, 17 * 128], "maskC": [4, 128, 32 * 128],
                "tri": [128, 128], "upp": [128, 128], "caus": [128, 128], "pastm": [128, 32]}

PARAM_SHAPES = {
    "norm1_w": [2, 1024], "w_in": [2, 1024, 3080], "a_q_norm": [2, 64], "a_k_norm": [2, 64],
    "c_q_norm": [2, 64], "c_k_norm": [2, 64], "conv_w": [2, 4, 1024], "conv_b": [2, 1024],
    "dt_bias": [2, 8], "a_log": [2, 8], "d_skip": [2, 8], "ssm_norm_w": [2, 512],
    "w_out": [2, 1024, 1024], "norm2_w": [2, 1024], "w_mlp_in": [2, 1024, 4096],
    "w_mlp_out": [2, 4096, 1024],
}

SCRATCH = {
    "qkT": ([8, 128, S], BF16), "v_a": ([S, 256], BF16), "v_c": ([S, 256], BF16),
    "sel": ([S, 64], F32), "z": ([S, 512], F32), "xbcT": ([1024, S], F32), "dtr": ([S, 8], F32),
    "Y": ([S, 1024], BF16), "x1": ([S, 1024], F32), "h2T": ([1024, S], BF16), "xcur": ([S, 1024], F32),
}


class Ctx:
    pass


def phase_A(C, l, x_src):
    nc, P, D = C.nc, C.P, C.D
    A = C.arena
    A.off = C.base_off
    bank, rb = C.bank, C.rb
    win = A.alloc(8 * DIN, BF16).rearrange("p (k n) -> p k n", k=8)
    r_win = P.res("win", dma=True)
    for k in range(8):
        P.dma("pool", win[:, k, :], D["w_in"][l, k * 128:(k + 1) * 128, :], writes=[r_win], owner=r_win)
    n1w = A.alloc(1024)
    qkwA = A.alloc(512)
    qwC = A.alloc(256)
    kwC = A.alloc(256)
    r_small = P.res("smallA", dma=True)
    P.dma("sp", n1w, bcast_rows(D["norm1_w"][l]), writes=[r_small], owner=r_small)
    P.dma("sp", qkwA[:, 0:256].rearrange("p (r n) -> p r n", r=4), bcast_rows(D["a_q_norm"][l], 4), writes=[r_small], owner=r_small)
    P.dma("sp", qkwA[:, 256:512].rearrange("p (r n) -> p r n", r=4), bcast_rows(D["a_k_norm"][l], 4), writes=[r_small], owner=r_small)
    P.dma("sp", qwC.rearrange("p (r n) -> p r n", r=4), bcast_rows(D["c_q_norm"][l], 4), writes=[r_small], owner=r_small)
    P.dma("sp", kwC.rearrange("p (r n) -> p r n", r=4), bcast_rows(D["c_k_norm"][l], 4), writes=[r_small], owner=r_small)

    NXB = 3
    xs = [A.alloc(1024) for _ in range(NXB)]
    r_xs = [P.res("xs%d" % i, dma=True) for i in range(NXB)]
    junk = A.alloc(1024)
    r_junk = P.res("junk")
    st = [A.alloc(8) for _ in range(2)]
    r_st = [P.res("st%d" % i) for i in range(2)]
    hb = [A.alloc(1024, BF16) for _ in range(2)]
    r_hb = [P.res("hb%d" % i) for i in range(2)]
    hT = [A.alloc(8 * 512, BF16).rearrange("p (k n) -> p k n", k=8) for _ in range(2)]
    r_hT = [P.res("hT%d" % i) for i in range(2)]
    sq = [A.alloc(512) for _ in range(2)]
    r_sq = [P.res("sq%d" % i) for i in range(2)]
    hst = [A.alloc(48) for _ in range(2)]
    r_hst = [P.res("hst%d" % i) for i in range(2)]
    qn = [A.alloc(512) for _ in range(2)]
    r_qn = [P.res("qn%d" % i) for i in range(2)]
    qkbA = [A.alloc(512, BF16) for _ in range(2)]
    r_qkbA = [P.res("qkbA%d" % i) for i in range(2)]
    qkC = [A.alloc(512) for _ in range(2)]
    r_qkC = [P.res("qkC%d" % i) for i in range(2)]
    stA = [A.alloc(4 * 512, BF16).rearrange("p (j n) -> p j n", j=4) for _ in range(2)]
    r_stA = [P.res("stA%d" % i, dma=True) for i in range(2)]
    stC = [A.alloc(4 * 512, BF16).rearrange("p (j n) -> p j n", j=4) for _ in range(2)]
    r_stC = [P.res("stC%d" % i, dma=True) for i in range(2)]
    stv = [A.alloc(512, BF16) for _ in range(2)]
    r_stv = [P.res("stv%d" % i, dma=True) for i in range(2)]
    stz = [A.alloc(512) for _ in range(2)]
    r_stz = [P.res("stz%d" % i, dma=True) for i in range(2)]
    stdt = [A.alloc(8) for _ in range(2)]
    r_stdt = [P.res("stdt%d" % i, dma=True) for i in range(2)]
    stx = [A.alloc(512) for _ in range(2)]
    r_stx = [P.res("stx%d" % i, dma=True) for i in range(2)]
    qTf = [A.alloc(256).rearrange("p (j n) -> p j n", j=2) for _ in range(2)]
    r_qTf = [P.res("qTf%d" % i) for i in range(2)]
    ksum = [A.alloc(2) for _ in range(2)]
    r_ksum = [P.res("ksum%d" % i) for i in range(2)]
    kmT = A.alloc(32).rearrange("p (j n) -> p j n", j=2)
    r_kmT = P.res("kmT")
    gm = [A.alloc(64) for _ in range(2)]
    r_gm = [P.res("gm%d" % i) for i in range(2)]
    mx8 = [A.alloc(32) for _ in range(2)]
    r_mx8 = [P.res("mx8%d" % i) for i in range(2)]
    selt = [A.alloc(64) for _ in range(2)]
    r_selt = [P.res("selt%d" % i, dma=True) for i in range(2)]

    P.op("dve", lambda e: e.memset(kmT, 0.0), writes=[r_kmT])

    psT = bank[0][:].bitcast(BF16)
    ps6 = bank[6][:].bitcast(BF16)
    x_view = x_src.rearrange("(t p) d -> t p d", p=128)

    def load_x(t):
        b = t % NXB
        P.dma("sp", xs[b], x_view[t], writes=[r_xs[b]], owner=r_xs[b])

    TB = [1, 2, 4, 5]

    def norm_T(t, hTg, r_hTg, i):
        if t + 2 < NT:
            load_x(t + 2)
        b = t % NXB
        p2 = t % 2
        P.op("act", lambda e: e.activation(out=junk, in_=xs[b], func=AF.Square, accum_out=st[p2][:, 0:1]),
             reads=[r_xs[b]], writes=[r_junk, r_st[p2]])
        P.op("act", lambda e: e.activation(out=st[p2][:, 1:2], in_=st[p2][:, 0:1], func=AF.Sqrt, scale=1.0 / DM, bias=C.eps_t[:, 0:1]),
             reads=[r_st[p2]], writes=[r_st[p2]])
        P.op("dve", lambda e: e.reciprocal(out=st[p2][:, 2:3], in_=st[p2][:, 1:2]), reads=[r_st[p2]], writes=[r_st[p2]])
        P.op("dve", lambda e: e.scalar_tensor_tensor(out=hb[p2], in0=xs[b], scalar=st[p2][:, 2:3], in1=n1w, op0=ALU.mult, op1=ALU.mult),
             reads=[r_xs[b], r_st[p2], r_small], writes=[r_hb[p2]])
        for k in range(8):
            P.op("pe", lambda e, k=k: e.transpose(out=psT[:, k * 128:(k + 1) * 128], in_=hb[p2][:, k * 128:(k + 1) * 128], identity=C.ident_b),
                 reads=[r_hb[p2]], writes=[rb[0]])
        P.op("act", lambda e: e.copy(out=hTg[:, :, i * 128:(i + 1) * 128], in_=psT.rearrange("p (k n) -> p k n", k=8)),
             reads=[rb[0]], writes=[r_hTg])

    def head_norm(pb_in, bk, p2, nh, sqs, c_ss, c_s, c_r, qn_out):
        P.op("act", lambda e: e.activation(out=sqs, in_=pb_in, func=AF.Square), reads=[rb[bk]], writes=[r_sq[p2]])
        P.op("dve", lambda e: e.tensor_reduce(out=c_ss, in_=sqs.rearrange("p (h n) -> p h n", h=nh), axis=AX.X, op=ALU.add),
             reads=[r_sq[p2]], writes=[r_hst[p2]])
        P.op("act", lambda e: e.activation(out=c_s, in_=c_ss, func=AF.Sqrt, scale=1.0 / 64, bias=C.eps_t[:, 0:1]),
             reads=[r_hst[p2]], writes=[r_hst[p2]])
        P.op("dve", lambda e: e.reciprocal(out=c_r, in_=c_s), reads=[r_hst[p2]], writes=[r_hst[p2]])
        P.op("dve", lambda e: e.tensor_tensor(out=qn_out.rearrange("p (h n) -> p h n", h=nh), in0=pb_in.rearrange("p (h n) -> p h n", h=nh),
                                              in1=c_r.unsqueeze(2).to_broadcast([128, nh, 64]), op=ALU.mult),
             reads=[rb[bk], r_hst[p2]], writes=[r_qn[p2]])

    def stage1(t, hTg, r_hTg, i):
        p2 = t % 2
        tsl = slice(i * 128, (i + 1) * 128)
        for c in range(4):
            bk = TB[c]
            for k in range(8):
                P.op("pe", lambda e, k=k, c=c, bk=bk: e.matmul(bank[bk][:], lhsT=hTg[:, k, tsl], rhs=win[:, k, c * 512:(c + 1) * 512], start=(k == 0), stop=(k == 7)),
                     reads=[r_hTg, r_win], writes=[rb[bk]])
            pb = bank[bk][:]
            H = hst[p2]
            if c == 0:
                head_norm(pb, bk, p2, 8, sq[p2], H[:, 0:8], H[:, 16:24], H[:, 32:40], qn[p2])
                P.op("pool", lambda e: e.tensor_tensor(out=qkbA[p2], in0=qn[p2], in1=qkwA, op=ALU.mult),
                     reads=[r_qn[p2], r_small], writes=[r_qkbA[p2]])
            elif c == 1:
                P.op("act", lambda e, pb=pb: e.copy(out=stv[p2][:, 0:256], in_=pb[:, 0:256]), reads=[rb[bk]], writes=[r_stv[p2]])
                head_norm(pb[:, 256:512], bk, p2, 4, sq[p2][:, 0:256], H[:, 8:12], H[:, 24:28], H[:, 40:44], qn[p2][:, 0:256])
                P.op("pool", lambda e: e.tensor_tensor(out=qkC[p2][:, 0:256], in0=qn[p2][:, 0:256], in1=qwC, op=ALU.mult),
                     reads=[r_qn[p2], r_small], writes=[r_qkC[p2]])
            elif c == 2:
                P.op("act", lambda e, pb=pb: e.copy(out=stv[p2][:, 256:512], in_=pb[:, 256:512]), reads=[rb[bk]], writes=[r_stv[p2]])
                head_norm(pb[:, 0:256], bk, p2, 4, sq[p2][:, 256:512], H[:, 12:16], H[:, 28:32], H[:, 44:48], qn[p2][:, 256:512])
                P.op("pool", lambda e: e.tensor_tensor(out=qkC[p2][:, 256:512], in0=qn[p2][:, 256:512], in1=kwC, op=ALU.mult),
                     reads=[r_qn[p2], r_small], writes=[r_qkC[p2]])
                P.dma("sp", D["v_a"][t * 128:(t + 1) * 128, :], stv[p2][:, 0:256], reads=[r_stv[p2]], owner=r_stv[p2])
                P.dma("sp", D["v_c"][t * 128:(t + 1) * 128, :], stv[p2][:, 256:512], reads=[r_stv[p2]], owner=r_stv[p2])
            else:
                P.op("act", lambda e, pb=pb: e.copy(out=stz[p2], in_=pb), reads=[rb[bk]], writes=[r_stz[p2]])
                P.dma("sp", D["z"][t * 128:(t + 1) * 128, :], stz[p2], reads=[r_stz[p2]], owner=r_stz[p2])
        for k in range(8):
            P.op("pe", lambda e, k=k: e.matmul(bank[3][:, 0:8], lhsT=hTg[:, k, tsl], rhs=win[:, k, 3072:3080], start=(k == 0), stop=(k == 7)),
                 reads=[r_hTg, r_win], writes=[rb[3]])
        P.op("act", lambda e: e.copy(out=stdt[p2], in_=bank[3][:, 0:8]), reads=[rb[3]], writes=[r_stdt[p2]])
        P.dma("sp", D["dtr"][t * 128:(t + 1) * 128, :], stdt[p2], reads=[r_stdt[p2]], owner=r_stdt[p2])

    def stage2(t):
        p2 = t % 2
        g, i = t // 4, t % 4
        blk = t // 2
        tsl = slice(i * 128, (i + 1) * 128)
        for j in range(4):
            P.op("pe", lambda e, j=j: e.transpose(out=ps6[:, j * 128:(j + 1) * 128], in_=qkbA[p2][:, j * 128:(j + 1) * 128], identity=C.ident_b),
                 reads=[r_qkbA[p2]], writes=[rb[6]])
        P.op("act", lambda e: e.copy(out=stA[g % 2][:, :, tsl], in_=ps6[:, 0:512].rearrange("p (j n) -> p j n", j=4)),
             reads=[rb[6]], writes=[r_stA[g % 2]])
        for j in range(4):
            P.op("pe", lambda e, j=j: e.transpose(out=bank[7][:, j * 128:(j + 1) * 128], in_=qkC[p2][:, j * 128:(j + 1) * 128], identity=C.ident_f),
                 reads=[r_qkC[p2]], writes=[rb[7]])
        P.op("act", lambda e: e.copy(out=stC[g % 2][:, :, tsl], in_=bank[7][:].rearrange("p (j n) -> p j n", j=4)),
             reads=[rb[7]], writes=[r_stC[g % 2]])
        P.op("dve", lambda e: e.tensor_copy(out=qTf[p2], in_=bank[7][:, 0:256].rearrange("p (j n) -> p j n", j=2)),
             reads=[rb[7]], writes=[r_qTf[p2]])
        P.op("dve", lambda e: e.tensor_reduce(out=ksum[p2], in_=bank[7][:, 256:512].rearrange("p (j n) -> p j n", j=2), axis=AX.X, op=ALU.add),
             reads=[rb[7]], writes=[r_ksum[p2]])
        for h in range(4):
            hp, pr = h % 2, h // 2
            P.op("pe", lambda e, h=h, hp=hp, pr=pr: e.matmul(bank[3][:, 32 + h * 16:48 + h * 16], lhsT=qTf[p2][hp * 64:(hp + 1) * 64, pr, :],
                                                           rhs=kmT[hp * 64:(hp + 1) * 64, pr, :], start=True, stop=True),
                 reads=[r_qTf[p2], r_kmT], writes=[rb[3]])
        P.op("dve", lambda e: e.tensor_tensor(out=gm[p2].rearrange("p (h n) -> p h n", h=4), in0=bank[3][:, 32:96].rearrange("p (h n) -> p h n", h=4),
                                              in1=C.pastm[:, 16 - blk:32 - blk].unsqueeze(1).to_broadcast([128, 4, 16]), op=ALU.add),
             reads=[rb[3]], writes=[r_gm[p2]])
        for h in range(4):
            P.op("dve", lambda e, h=h: e.max(out=mx8[p2][:, h * 8:(h + 1) * 8], in_=gm[p2][:, h * 16:(h + 1) * 16]),
                 reads=[r_gm[p2]], writes=[r_mx8[p2]])
        P.op("dve", lambda e: e.tensor_tensor(out=selt[p2].rearrange("p (h n) -> p h n", h=4), in0=gm[p2].rearrange("p (h n) -> p h n", h=4),
                                              in1=mx8[p2].rearrange("p (h n) -> p h n", h=4)[:, :, 2:3].to_broadcast([128, 4, 16]), op=ALU.is_ge),
             reads=[r_gm[p2], r_mx8[p2]], writes=[r_selt[p2]])
        P.op("dve", lambda e: e.memset(selt[p2].rearrange("p (h n) -> p h n", h=4)[:, :, blk:blk + 1], 1.0),
             reads=[], writes=[r_selt[p2]])
        P.dma("sp", D["sel"][t * 128:(t + 1) * 128, :], selt[p2], reads=[r_selt[p2]], owner=r_selt[p2])
        P.op("dve", lambda e: e.scalar_tensor_tensor(out=kmT[:, :, blk], in0=ksum[p2], scalar=1.0 / 256, in1=kmT[:, :, blk], op0=ALU.mult, op1=ALU.add),
             reads=[r_ksum[p2], r_kmT], writes=[r_kmT])
        if i == 3:
            for j in range(4):
                P.dma("sp", D["qkT"][j][:, g * 512:(g + 1) * 512], stA[g % 2][:, j, :], reads=[r_stA[g % 2]], owner=r_stA[g % 2])
                P.dma("sp", D["qkT"][4 + j][:, g * 512:(g + 1) * 512], stC[g % 2][:, j, :], reads=[r_stC[g % 2]], owner=r_stC[g % 2])

    load_x(0)
    load_x(1)
    for i in range(4):
        norm_T(i, hT[0], r_hT[0], i)
    prev = None
    xcnt = 0
    for g in range(8):
        hTg, r_hTg = hT[g % 2], r_hT[g % 2]
        for i in range(4):
            t = 4 * g + i
            stage1(t, hTg, r_hTg, i)
            if prev is not None:
                stage2(prev)
            prev = t
            if g + 1 < 8:
                norm_T(4 * (g + 1) + i, hT[(g + 1) % 2], r_hT[(g + 1) % 2], i)
        for m in range(8):
            bk = TB[xcnt % 4]
            xcnt += 1
            for k in range(8):
                P.op("pe", lambda e, k=k, m=m, bk=bk, hTg=hTg: e.matmul(bank[bk][:], lhsT=win[:, k, 2048 + m * 128:2048 + (m + 1) * 128], rhs=hTg[:, k, :], start=(k == 0), stop=(k == 7)),
                     reads=[r_hTg, r_win], writes=[rb[bk]])
            P.op("act", lambda e, bk=bk, m=m: e.copy(out=stx[m % 2], in_=bank[bk][:]), reads=[rb[bk]], writes=[r_stx[m % 2]])
            P.dma("sp", D["xbcT"][m * 128:(m + 1) * 128, g * 512:(g + 1) * 512], stx[m % 2], reads=[r_stx[m % 2]], owner=r_stx[m % 2])
    stage2(prev)
    P.end_phase()


def phase_attn(C, l, kind):
    nc, P, D = C.nc, C.P, C.D
    A = C.arena
    A.off = C.base_off
    bank, rb = C.bank, C.rb
    isA = kind == "A"
    nof = 17 if isA else 32
    qi0, ki0 = (0, 2) if isA else (4, 6)
    ycol = 0 if isA else 768
    qT = [A.alloc(S, BF16) for _ in range(2)]
    kT = [A.alloc(S, BF16) for _ in range(2)]
    r_qk = P.res("qk", dma=True)
    for p in range(2):
        P.dma("sp", qT[p], D["qkT"][qi0 + p], writes=[r_qk], owner=r_qk)
        P.dma("sp", kT[p], D["qkT"][ki0 + p], writes=[r_qk], owner=r_qk)
    V = A.alloc(NT * 4 * 65, BF16).rearrange("p (t h e) -> p t h e", t=NT, h=4)
    r_V = P.res("V", dma=True)
    vsrc = D["v_a" if isA else "v_c"].rearrange("(t p) (h e) -> p t h e", p=128, h=4)
    for t in range(NT):
        P.dma("sp", V[:, t, :, 0:64], vsrc[:, t], writes=[r_V], owner=r_V)
    P.op("pool", lambda e: e.memset(V[:, :, :, 64:65], 1.0), writes=[r_V])
    mask = A.alloc(4 * nof * 128, BF16).rearrange("p (h n) -> p h n", h=4)
    r_mask = P.res("mask", dma=True)
    msrc = D["c_maskA" if isA else "c_maskC"]
    for h in range(4):
        P.dma("sp", mask[:, h, :], msrc[h], writes=[r_mask], owner=r_mask)
    if not isA:
        selall = A.alloc(NT * 64).rearrange("p (t c) -> p t c", t=NT)
        r_sel = P.res("selall", dma=True)
        ssrc = D["sel"].rearrange("(t p) c -> p t c", p=128)
        for q4 in range(4):
            P.dma("sp", selall[:, q4 * 8:(q4 + 1) * 8, :], ssrc[:, q4 * 8:(q4 + 1) * 8, :], writes=[r_sel], owner=r_sel)
        acc = [A.alloc(4 * 65).rearrange("p (i e) -> p i e", i=4) for _ in range(2)]
        r_acc = [P.res("acc%d" % i) for i in range(2)]
        tmp = [A.alloc(4 * 65).rearrange("p (i e) -> p i e", i=4) for _ in range(3)]
        r_tmp = [P.res("tmp%d" % i) for i in range(3)]
    NSB = 4
    POOL_EVERY = 10 ** 9
    ex = [A.alloc(512, BF16) for _ in range(NSB)]
    r_ex = [P.res("ex%d" % i) for i in range(NSB)]
    pT = [A.alloc(512, BF16) for _ in range(NSB)]
    r_pT = [P.res("pT%d" % i) for i in range(NSB)]
    rc = [A.alloc(4) for _ in range(2)]
    r_rc = [P.res("rc%d" % i) for i in range(2)]
    yst = [A.alloc(4 * 256, BF16).rearrange("p (i c) -> p i c", i=4) for _ in range(2)]
    r_yst = [P.res("yst%d" % i, dma=True) for i in range(2)]

    cnt = {"step": 0, "ob": 0, "tmp": 0}
    slopes = SL_A if isA else SL_C
    maxoff = [min(nof - 1, int(math.floor((125.0 / sl + 127.0) / 128.0))) for sl in slopes]

    def emit_qk(h, j, i0, i1):
        sb = cnt["step"] % NSB
        cnt["step"] += 1
        pr, hp = h // 2, h % 2
        psl = slice(hp * 64, hp * 64 + 64)
        N = (i1 - i0 + 1) * 128
        P.op("pe", lambda e: e.matmul(bank[sb][:, 0:N], lhsT=kT[pr][psl, j * 128:(j + 1) * 128], rhs=qT[pr][psl, i0 * 128:(i1 + 1) * 128], start=True, stop=True),
             reads=[r_qk], writes=[rb[sb]])
        P.op("act", lambda e: e.activation(out=ex[sb][:, 0:N], in_=bank[sb][:, 0:N], func=AF.Exp, scale=0.125), reads=[rb[sb]], writes=[r_ex[sb]])
        meng = "pool" if (cnt["step"] % POOL_EVERY == 0) else "dve"
        P.op(meng, lambda e: e.tensor_tensor(out=pT[sb][:, 0:N], in0=ex[sb][:, 0:N], in1=mask[:, h, (i0 - j) * 128:(i1 - j + 1) * 128], op=ALU.mult),
             reads=[r_ex[sb], r_mask], writes=[r_pT[sb]])
        return sb

    def emit_pv(sb, h, j, i0, i1, g, ob, started):
        for i in range(i0, i1 + 1):
            st = not started[0]
            started[0] = True
            c0 = (i - 4 * g) * 65
            P.op("pe", lambda e, i=i, st=st, c0=c0: e.matmul(bank[ob][:, c0:c0 + 65], lhsT=pT[sb][:, (i - i0) * 128:(i - i0 + 1) * 128], rhs=V[:, j, h, :],
                                                           start=st, stop=(j == i), skip_group_check=True),
                 reads=[r_pT[sb], r_V], writes=[rb[ob]])

    def store_y(g, y2):
        for i in range(4):
            t = 4 * g + i
            P.dma("sp", D["Y"][t * 128:(t + 1) * 128, ycol:ycol + 256], yst[y2][:, i, :], reads=[r_yst[y2]], owner=r_yst[y2])

    pend = []

    def flush(n_keep):
        while len(pend) > n_keep:
            args, post = pend.pop(0)
            emit_pv(*args)
            if post is not None:
                post()

    def run_steps(steps, h, g, ob, post):
        started = [False]
        for si, (j, i0, i1) in enumerate(steps):
            sb = emit_qk(h, j, i0, i1)
            pend.append(((sb, h, j, i0, i1, g, ob, started), post if si == len(steps) - 1 else None))
            flush(NSB - 2)

    for g in range(8):
        y2 = g % 2
        for h in range(4):
            if isA:
                ob = 4 + cnt["ob"] % 2
                cnt["ob"] += 1
                steps = []
                for j in range(max(0, 4 * g - 16), 4 * g + 4):
                    i0, i1 = max(4 * g, j), min(4 * g + 3, j + min(16, maxoff[h]))
                    if i0 <= i1:
                        steps.append((j, i0, i1))
                def postA(ob=ob, h=h, y2=y2, g=g, r2=cnt["ob"] % 2):
                    src = bank[ob][:, 0:260].rearrange("p (i e) -> p i e", i=4)
                    P.op("dve", lambda e: e.reciprocal(out=rc[r2].unsqueeze(2), in_=src[:, :, 64:65]), reads=[rb[ob]], writes=[r_rc[r2]])
                    P.op("dve", lambda e: e.tensor_tensor(out=yst[y2][:, :, h * 64:(h + 1) * 64], in0=src[:, :, 0:64],
                                                          in1=rc[r2].unsqueeze(2).to_broadcast([128, 4, 64]), op=ALU.mult),
                         reads=[rb[ob], r_rc[r2]], writes=[r_yst[y2]])
                    if h == 3:
                        store_y(g, y2)
                run_steps(steps, h, g, ob, postA)
            else:
                a2 = h % 2
                P.op("pool", lambda e, a2=a2: e.memset(acc[a2], 0.0), writes=[r_acc[a2]])
                for n in range(0, 2 * g + 2):
                    ob = 4 + cnt["ob"] % 3
                    cnt["ob"] += 1
                    steps = []
                    for j in (2 * n, 2 * n + 1):
                        i0, i1 = max(4 * g, j), min(4 * g + 3, j + maxoff[h])
                        if i0 <= i1:
                            steps.append((j, i0, i1))
                    if not steps:
                        continue
                    ia_l = min(st_[1] for st_ in steps) - 4 * g
                    ib_l = max(st_[2] for st_ in steps) - 4 * g + 1
                    def postC(ob=ob, h=h, y2=y2, g=g, n=n, a2=a2, last=(n == 2 * g + 1), ia=ia_l, ib=ib_l):
                        ni = ib - ia
                        src = bank[ob][:, 0:260].rearrange("p (i e) -> p i e", i=4)[:, ia:ib, :]
                        selv = selall[:, 4 * g + ia:4 * g + ib, h * 16 + n:h * 16 + n + 1].to_broadcast([128, ni, 65])
                        t3 = cnt["tmp"] % 3
                        cnt["tmp"] += 1
                        P.op("dve", lambda e: e.tensor_tensor(out=tmp[t3][:, ia:ib, :], in0=src, in1=selv, op=ALU.mult),
                             reads=[rb[ob], r_sel], writes=[r_tmp[t3]])
                        P.op("pool", lambda e: e.tensor_tensor(out=acc[a2][:, ia:ib, :], in0=acc[a2][:, ia:ib, :], in1=tmp[t3][:, ia:ib, :], op=ALU.add),
                             reads=[r_tmp[t3], r_acc[a2]], writes=[r_acc[a2]])
                        if last:
                            r2 = h % 2
                            P.op("dve", lambda e: e.reciprocal(out=rc[r2].unsqueeze(2), in_=acc[a2][:, :, 64:65]), reads=[r_acc[a2]], writes=[r_rc[r2]])
                            P.op("dve", lambda e: e.tensor_tensor(out=yst[y2][:, :, h * 64:(h + 1) * 64], in0=acc[a2][:, :, 0:64],
                                                                  in1=rc[r2].unsqueeze(2).to_broadcast([128, 4, 64]), op=ALU.mult),
                                 reads=[r_acc[a2], r_rc[r2]], writes=[r_yst[y2]])
                            if h == 3:
                                store_y(g, y2)
                    run_steps(steps, h, g, ob, postC)
    flush(0)
    P.end_phase()


def phase_D(C, l):
    nc, P, D = C.nc, C.P, C.D
    A = C.arena
    A.off = C.base_off
    bank, rb = C.bank, C.rb
    tri = A.alloc(128)
    upp = A.alloc(128)
    caus = A.alloc(128)
    ones = A.alloc(128)
    r_k = P.res("dconst", dma=True)
    P.dma("sp", tri, D["c_tri"], writes=[r_k], owner=r_k)
    P.dma("sp", upp, D["c_upp"], writes=[r_k], owner=r_k)
    P.dma("sp", caus, D["c_caus"], writes=[r_k], owner=r_k)
    P.op("pool", lambda e: e.memset(ones, 1.0), writes=[r_k])
    cw = A.alloc(32).rearrange("p (m j) -> p m j", m=8)
    cb = A.alloc(8)
    for j in range(4):
        P.dma("sp", cw[:, :, j], D["conv_w"][l, j].rearrange("(m p) -> p m", p=128), writes=[r_k], owner=r_k, allow_slow_non_contiguous=True)
    P.dma("sp", cb, D["conv_b"][l].rearrange("(m p) -> p m", p=128), writes=[r_k], owner=r_k, allow_slow_non_contiguous=True)
    dtb = A.alloc(8)
    alog = A.alloc(8)
    dsk = A.alloc(8)
    nw = A.alloc(512)
    P.dma("sp", dtb, bcast_rows(D["dt_bias"][l]), writes=[r_k], owner=r_k)
    P.dma("sp", alog, bcast_rows(D["a_log"][l]), writes=[r_k], owner=r_k)
    P.dma("sp", dsk, bcast_rows(D["d_skip"][l]), writes=[r_k], owner=r_k)
    P.dma("sp", nw, bcast_rows(D["ssm_norm_w"][l]), writes=[r_k], owner=r_k)
    dtr = A.alloc(256).rearrange("p (t h) -> p t h", t=NT)
    r_dtr = P.res("dtr", dma=True)
    dsrc = D["dtr"].rearrange("(t p) h -> p t h", p=128)
    for q4 in range(4):
        P.dma("sp", dtr[:, q4 * 8:(q4 + 1) * 8, :], dsrc[:, q4 * 8:(q4 + 1) * 8, :], writes=[r_dtr], owner=r_dtr)

    BT = A.alloc(2 * S, BF16).rearrange("p (g n) -> p g n", g=2)
    CT = A.alloc(2 * S, BF16).rearrange("p (g n) -> p g n", g=2)
    r_BT, r_CT = P.res("BT"), P.res("CT")
    xtok = A.alloc(NT * 512).rearrange("p (t c) -> p t c", t=NT)
    r_xtok = P.res("xtok")
    Btok = A.alloc(NT * 256, BF16).rearrange("p (t c) -> p t c", t=NT)
    r_Btok = P.res("Btok")
    mark = A.off
    xin2 = [A.alloc(S + 8) for _ in range(2)]
    r_xin2 = [P.res("xin%d" % i, dma=True) for i in range(2)]
    cacc2 = [A.alloc(S) for _ in range(2)]
    r_cacc2 = [P.res("cacc%d" % i) for i in range(2)]
    for i in range(2):
        P.op("dve", lambda e, i=i: e.memset(xin2[i][:, 0:8], 0.0), writes=[r_xin2[i]])

    tcnt = 0
    for m in range(8):
        xin, r_xin, cacc, r_cacc = xin2[m % 2], r_xin2[m % 2], cacc2[m % 2], r_cacc2[m % 2]
        P.dma("sp", xin[:, 3:3 + S], D["xbcT"][m * 128:(m + 1) * 128, :], writes=[r_xin], owner=r_xin)
        P.op("dve", lambda e, m=m, xin=xin, cacc=cacc: e.tensor_scalar(out=cacc, in0=xin[:, 3:3 + S], scalar1=cw[:, m, 3:4], scalar2=None, op0=ALU.mult),
             reads=[r_xin, r_k], writes=[r_cacc])
        P.op("dve", lambda e, m=m, xin=xin, cacc=cacc: e.scalar_tensor_tensor(out=cacc, in0=xin[:, 2:2 + S], scalar=cw[:, m, 2:3], in1=cacc, op0=ALU.mult, op1=ALU.add),
             reads=[r_xin, r_k, r_cacc], writes=[r_cacc])
        P.op("dve", lambda e, m=m, xin=xin, cacc=cacc: e.scalar_tensor_tensor(out=cacc, in0=xin[:, 1:1 + S], scalar=cw[:, m, 1:2], in1=cacc, op0=ALU.mult, op1=ALU.add),
             reads=[r_xin, r_k, r_cacc], writes=[r_cacc])
        P.op("dve", lambda e, m=m, xin=xin, cacc=cacc: e.scalar_tensor_tensor(out=cacc, in0=xin[:, 0:S], scalar=cw[:, m, 0:1], in1=cacc, op0=ALU.mult, op1=ALU.add),
             reads=[r_xin, r_k, r_cacc], writes=[r_cacc])
        if m < 4:
            P.op("act", lambda e, m=m, xin=xin, cacc=cacc: e.activation(out=cacc, in_=cacc, func=AF.Silu, bias=cb[:, m:m + 1]), reads=[r_cacc, r_k], writes=[r_cacc])
            for c0 in range(0, NT, 4):
                bk = tcnt % 2
                tcnt += 1
                for cc in range(4):
                    c = c0 + cc
                    P.op("pe", lambda e, c=c, cc=cc, bk=bk, cacc=cacc: e.transpose(out=bank[bk][:, cc * 128:(cc + 1) * 128], in_=cacc[:, c * 128:(c + 1) * 128], identity=C.ident_f),
                         reads=[r_cacc], writes=[rb[bk]])
                eng = "act" if (tcnt % 2) else "dve"
                if eng == "act":
                    P.op("act", lambda e, c0=c0, m=m, bk=bk: e.copy(out=xtok[:, c0:c0 + 4, m * 128:(m + 1) * 128], in_=bank[bk][:].rearrange("p (c n) -> p c n", c=4)),
                         reads=[rb[bk]], writes=[r_xtok])
                else:
                    P.op("dve", lambda e, c0=c0, m=m, bk=bk: e.tensor_copy(out=xtok[:, c0:c0 + 4, m * 128:(m + 1) * 128], in_=bank[bk][:].rearrange("p (c n) -> p c n", c=4)),
                         reads=[rb[bk]], writes=[r_xtok])
        elif m < 6:
            gg = m - 4
            P.op("act", lambda e, m=m, gg=gg, cacc=cacc: e.activation(out=BT[:, gg, :], in_=cacc, func=AF.Silu, bias=cb[:, m:m + 1]), reads=[r_cacc, r_k], writes=[r_BT])
            for c0 in range(0, NT, 8):
                bk = tcnt % 2
                tcnt += 1
                pb = bank[bk][:].bitcast(BF16)
                for cc in range(8):
                    c = c0 + cc
                    P.op("pe", lambda e, c=c, cc=cc, pb=pb, gg=gg: e.transpose(out=pb[:, cc * 128:(cc + 1) * 128], in_=BT[:, gg, c * 128:(c + 1) * 128], identity=C.ident_b),
                         reads=[r_BT], writes=[rb[bk]])
                P.op("act", lambda e, c0=c0, gg=gg, pb=pb: e.copy(out=Btok[:, c0:c0 + 8, gg * 128:(gg + 1) * 128], in_=pb.rearrange("p (c n) -> p c n", c=8)),
                     reads=[rb[bk]], writes=[r_Btok])
        else:
            gg = m - 6
            P.op("act", lambda e, m=m, gg=gg, cacc=cacc: e.activation(out=CT[:, gg, :], in_=cacc, func=AF.Silu, bias=cb[:, m:m + 1]), reads=[r_cacc, r_k], writes=[r_CT])
    P.barrier()
    A.off = mark

    W = 256
    dtx = A.alloc(W)
    t_ax = A.alloc(W)
    t_e = A.alloc(W)
    dt_all = A.alloc(W)
    aneg = A.alloc(8)
    da_all = A.alloc(W)
    ea_all = A.alloc(W)
    ds_all = A.alloc(W)
    cd_all = A.alloc(W)
    dtds = A.alloc(W)
    r_t = P.res("dtables")
    v3 = lambda ap: ap.rearrange("p (t h) -> p t h", t=NT)
    P.op("dve", lambda e: e.tensor_tensor(out=v3(dtx), in0=dtr, in1=dtb.unsqueeze(1).to_broadcast([128, NT, 8]), op=ALU.add), reads=[r_dtr, r_k], writes=[r_t])
    P.op("dve", lambda e: e.scalar_tensor_tensor(out=t_ax, in0=dtx, scalar=-1.0, in1=dtx, op0=ALU.mult, op1=ALU.min), reads=[r_t], writes=[r_t])
    P.op("act", lambda e: e.activation(out=t_e, in_=t_ax, func=AF.Exp), reads=[r_t], writes=[r_t])
    P.op("act", lambda e: e.activation(out=t_e, in_=t_e, func=AF.Ln, bias=C.one_t[:, 0:1]), reads=[r_t], writes=[r_t])
    P.op("dve", lambda e: e.tensor_scalar_max(out=t_ax, in0=dtx, scalar1=0.0), reads=[r_t], writes=[r_t])
    P.op("dve", lambda e: e.tensor_tensor(out=dt_all, in0=t_ax, in1=t_e, op=ALU.add), reads=[r_t], writes=[r_t])
    P.op("act", lambda e: e.activation(out=aneg, in_=alog, func=AF.Exp), reads=[r_k], writes=[r_t])
    P.op("dve", lambda e: e.tensor_scalar(out=aneg, in0=aneg, scalar1=-1.0, scalar2=None, op0=ALU.mult), reads=[r_t], writes=[r_t])
    P.op("dve", lambda e: e.tensor_tensor(out=v3(da_all), in0=v3(dt_all), in1=aneg.unsqueeze(1).to_broadcast([128, NT, 8]), op=ALU.mult), reads=[r_t], writes=[r_t])
    for (lhs, dst) in ((tri, ea_all), (upp, ds_all), (ones, cd_all)):
        P.op("pe", lambda e, lhs=lhs: e.matmul(bank[2][:, 0:W], lhsT=lhs, rhs=da_all, start=True, stop=True), reads=[r_t, r_k], writes=[rb[2]])
        P.op("act", lambda e, dst=dst: e.activation(out=dst, in_=bank[2][:, 0:W], func=AF.Exp), reads=[rb[2]], writes=[r_t])
    P.op("dve", lambda e: e.tensor_tensor(out=dtds, in0=dt_all, in1=ds_all, op=ALU.mult), reads=[r_t], writes=[r_t])

    NB = 4
    mk3 = lambda n, dt=F32: [A.alloc(n, dt).rearrange("p (h n) -> p h n", h=4) for _ in range(NB)]
    rT, r_rT = mk3(512), [P.res("rT%d" % i) for i in range(NB)]
    Lt, r_Lt = mk3(512), [P.res("Lt%d" % i) for i in range(NB)]
    Gm, r_Gm = [A.alloc(128) for _ in range(NB)], [P.res("Gm%d" % i) for i in range(NB)]
    sc, r_sc = mk3(512, BF16), [P.res("sc%d" % i) for i in range(NB)]
    xd, r_xd = mk3(256, BF16), [P.res("xd%d" % i) for i in range(NB)]
    xdd, r_xdd = mk3(256, BF16), [P.res("xdd%d" % i) for i in range(NB)]
    t1, r_t1 = mk3(256), [P.res("t1%d" % i) for i in range(NB)]
    t3, r_t3 = mk3(256), [P.res("t3%d" % i) for i in range(NB)]
    St = [A.alloc(256).rearrange("p (h n) -> p h n", h=4) for _ in range(2)]
    r_St = [P.res("St%d" % i) for i in range(2)]
    tS = [A.alloc(256).rearrange("p (h n) -> p h n", h=4) for _ in range(2)]
    r_tS = [P.res("tS%d" % i) for i in range(2)]
    Sbf = [A.alloc(256, BF16) for _ in range(2)]
    r_Sbf = [P.res("Sbf%d" % i) for i in range(2)]
    yg = [A.alloc(512) for _ in range(2)]
    r_yg = [P.res("yg%d" % i) for i in range(2)]
    zt4 = [A.alloc(4 * 512).rearrange("p (j n) -> p j n", j=4) for _ in range(2)]
    r_zt4 = [P.res("zt%d" % i, dma=True) for i in range(2)]
    zsrc = D["z"].rearrange("(q j p) n -> q p j n", j=4, p=128)
    junk = A.alloc(256)
    r_junk = P.res("junkD")
    nst = [A.alloc(8) for _ in range(2)]
    r_nst = [P.res("nst%d" % i) for i in range(2)]
    yo = [A.alloc(512, BF16) for _ in range(2)]
    r_yo = [P.res("yo%d" % i, dma=True) for i in range(2)]

    def b4(ap2d, c, g):
        return v3(ap2d)[:, c, g * 4:g * 4 + 4].unsqueeze(2).to_broadcast([128, 4, 64])

    def xg_of(c, g):
        return xtok[:, c, g * 256:(g + 1) * 256].rearrange("p (h n) -> p h n", h=4)

    def S0(it):
        c, g = it // 2, it % 2
        b = it % NB
        bX, bY = it % 3, 6 + it % 2
        csl = slice(c * 128, (c + 1) * 128)
        xg = xg_of(c, g)
        P.op("dve", lambda e: e.tensor_tensor(out=rT[b], in0=tri.unsqueeze(1).to_broadcast([128, 4, 128]),
                                              in1=v3(da_all)[:, c, g * 4:g * 4 + 4].unsqueeze(2).to_broadcast([128, 4, 128]), op=ALU.mult),
             reads=[r_k, r_t], writes=[r_rT[b]])
        P.op("pe", lambda e: e.matmul(bank[bX][:], lhsT=upp, rhs=rT[b].rearrange("p h n -> p (h n)"), start=True, stop=True),
             reads=[r_k, r_rT[b]], writes=[rb[bX]])
        P.op("pe", lambda e: e.matmul(bank[bY][:, 0:128], lhsT=BT[:, g, csl], rhs=CT[:, g, csl], start=True, stop=True),
             reads=[r_BT, r_CT], writes=[rb[bY]])
        P.op("pool", lambda e: e.tensor_tensor(out=xd[b], in0=xg, in1=b4(dt_all, c, g), op=ALU.mult), reads=[r_xtok, r_t], writes=[r_xd[b]])
        P.op("pool", lambda e: e.tensor_tensor(out=xdd[b], in0=xg, in1=b4(dtds, c, g), op=ALU.mult), reads=[r_xtok, r_t], writes=[r_xdd[b]])
        P.op("pool", lambda e: e.tensor_tensor(out=t3[b], in0=xg, in1=dsk[:, g * 4:g * 4 + 4].unsqueeze(2).to_broadcast([128, 4, 64]), op=ALU.mult),
             reads=[r_xtok, r_k], writes=[r_t3[b]])

    def S1(it):
        b = it % NB
        bX, bY = it % 3, 6 + it % 2
        P.op("act", lambda e: e.activation(out=Lt[b].rearrange("p h n -> p (h n)"), in_=bank[bX][:], func=AF.Exp), reads=[rb[bX]], writes=[r_Lt[b]])
        P.op("dve", lambda e: e.tensor_tensor(out=Gm[b], in0=bank[bY][:, 0:128], in1=caus, op=ALU.mult), reads=[rb[bY], r_k], writes=[r_Gm[b]])
        P.op("dve", lambda e: e.tensor_tensor(out=sc[b], in0=Lt[b], in1=Gm[b].unsqueeze(1).to_broadcast([128, 4, 128]), op=ALU.mult),
             reads=[r_Lt[b], r_Gm[b]], writes=[r_sc[b]])

    def S2(it):
        c, g = it // 2, it % 2
        b = it % NB
        bZ, bW = 3 + it % 3, 6 + it % 2
        csl = slice(c * 128, (c + 1) * 128)
        for hh in range(4):
            P.op("pe", lambda e, hh=hh: e.matmul(bank[bZ][:, hh * 64:(hh + 1) * 64], lhsT=sc[b][:, hh, :], rhs=xd[b][:, hh, :], start=True, stop=True),
                 reads=[r_sc[b], r_xd[b]], writes=[rb[bZ]])
        if c > 0:
            P.op("pe", lambda e: e.matmul(bank[bZ][:, 256:512], lhsT=CT[:, g, csl], rhs=Sbf[g], start=True, stop=True),
                 reads=[r_CT, r_Sbf[g]], writes=[rb[bZ]])
        if c + 1 < NT:
            P.op("pe", lambda e: e.matmul(bank[bW][:, 256:512], lhsT=Btok[:, c, g * 128:(g + 1) * 128], rhs=xdd[b].rearrange("p h n -> p (h n)"), start=True, stop=True),
                 reads=[r_Btok, r_xdd[b]], writes=[rb[bW]])

    def S3(it):
        c, g = it // 2, it % 2
        b = it % NB
        c2 = c % 2
        bZ, bW = 3 + it % 3, 6 + it % 2
        if c + 1 < NT:
            pw = bank[bW][:, 256:512].rearrange("p (h n) -> p h n", h=4)
            if c == 0:
                P.op("dve", lambda e: e.tensor_copy(out=St[g], in_=pw), reads=[rb[bW]], writes=[r_St[g]])
            else:
                P.op("dve", lambda e: e.tensor_tensor(out=tS[g], in0=St[g], in1=b4(cd_all, c, g), op=ALU.mult), reads=[r_St[g], r_t], writes=[r_tS[g]])
                P.op("dve", lambda e: e.tensor_tensor(out=St[g], in0=tS[g], in1=pw, op=ALU.add), reads=[r_tS[g], rb[bW]], writes=[r_St[g]])
            P.op("act", lambda e: e.copy(out=Sbf[g], in_=St[g].rearrange("p h n -> p (h n)")), reads=[r_St[g]], writes=[r_Sbf[g]])
        pz = bank[bZ][:]
        if c > 0:
            P.op("dve", lambda e: e.tensor_tensor(out=t1[b], in0=pz[:, 256:512].rearrange("p (h n) -> p h n", h=4), in1=b4(ea_all, c, g), op=ALU.mult),
                 reads=[rb[bZ], r_t], writes=[r_t1[b]])
            P.op("dve", lambda e: e.tensor_tensor(out=t1[b], in0=t1[b], in1=pz[:, 0:256].rearrange("p (h n) -> p h n", h=4), op=ALU.add),
                 reads=[rb[bZ], r_t1[b]], writes=[r_t1[b]])
        else:
            P.op("dve", lambda e: e.tensor_copy(out=t1[b], in_=pz[:, 0:256].rearrange("p (h n) -> p h n", h=4)), reads=[rb[bZ]], writes=[r_t1[b]])
        P.op("pool", lambda e: e.tensor_tensor(out=yg[c2][:, g * 256:(g + 1) * 256].rearrange("p (h n) -> p h n", h=4), in0=t1[b], in1=t3[b], op=ALU.add),
             reads=[r_t1[b], r_t3[b]], writes=[r_yg[c2]])

    def S4(c):
        c2 = c % 2
        q, j = c // 4, c % 4
        z4, r_z4 = zt4[q % 2], r_zt4[q % 2]
        if j == 0:
            P.op("act", lambda e: e.activation(out=z4, in_=z4, func=AF.Silu), reads=[r_z4], writes=[r_z4])
        P.op("dve", lambda e: e.tensor_tensor(out=yg[c2], in0=yg[c2], in1=z4[:, j, :], op=ALU.mult), reads=[r_yg[c2], r_z4], writes=[r_yg[c2]])
        for g in range(2):
            P.op("act", lambda e, g=g: e.activation(out=junk, in_=yg[c2][:, g * 256:(g + 1) * 256], func=AF.Square, accum_out=nst[c2][:, g:g + 1]),
                 reads=[r_yg[c2]], writes=[r_junk, r_nst[c2]])
        P.op("act", lambda e: e.activation(out=nst[c2][:, 2:4], in_=nst[c2][:, 0:2], func=AF.Ln, scale=1.0 / 256, bias=C.eps_t[:, 0:1]), reads=[r_nst[c2]], writes=[r_nst[c2]])
        P.op("act", lambda e: e.activation(out=nst[c2][:, 4:6], in_=nst[c2][:, 2:4], func=AF.Exp, scale=-0.5), reads=[r_nst[c2]], writes=[r_nst[c2]])
        P.op("dve", lambda e: e.tensor_tensor(out=yg[c2].rearrange("p (g n) -> p g n", g=2), in0=yg[c2].rearrange("p (g n) -> p g n", g=2),
                                              in1=nst[c2][:, 4:6].unsqueeze(2).to_broadcast([128, 2, 256]), op=ALU.mult),
             reads=[r_yg[c2], r_nst[c2]], writes=[r_yg[c2]])
        P.op("pool", lambda e: e.tensor_tensor(out=yo[c2], in0=yg[c2], in1=nw, op=ALU.mult), reads=[r_yg[c2], r_k], writes=[r_yo[c2]])
        P.dma("sp", D["Y"][c * 128:(c + 1) * 128, 256:768], yo[c2], reads=[r_yo[c2]], owner=r_yo[c2])
        if j == 3 and q + 2 < NT // 4:
            P.dma("sp", z4, zsrc[q + 2], writes=[r_z4], owner=r_z4)

    P.dma("sp", zt4[0], zsrc[0], writes=[r_zt4[0]], owner=r_zt4[0])
    P.dma("sp", zt4[1], zsrc[1], writes=[r_zt4[1]], owner=r_zt4[1])
    NI = 2 * NT
    for s_ in range(NI + 3):
        if s_ < NI:
            S0(s_)
        if 0 <= s_ - 1 < NI:
            S1(s_ - 1)
        if 0 <= s_ - 2 < NI:
            S2(s_ - 2)
        if 0 <= s_ - 3 < NI:
            S3(s_ - 3)
            if (s_ - 3) % 2 == 1:
                S4((s_ - 3) // 2)
    P.end_phase()


def phase_E(C, l, x_src, x_dst):
    nc, P, D = C.nc, C.P, C.D
    A = C.arena
    A.off = C.base_off
    bank, rb = C.bank, C.rb
    w1 = A.alloc(8 * DFF, BF16).rearrange("p (k n) -> p k n", k=8)
    w2 = A.alloc(32 * DM, BF16).rearrange("p (f n) -> p f n", f=32)
    r_w1 = P.res("w1", dma=True)
    r_w2 = P.res("w2", dma=True)
    mark = A.off
    wout = A.alloc(8 * DM, BF16).rearrange("p (k n) -> p k n", k=8)
    r_wout = P.res("wout", dma=True)
    for k in range(8):
        P.dma("pool", wout[:, k, :], D["w_out"][l, k * 128:(k + 1) * 128, :], writes=[r_wout], owner=r_wout)
    for k in range(8):
        P.dma("pool", w1[:, k, :], D["w_mlp_in"][l, k * 128:(k + 1) * 128, :], writes=[r_w1], owner=r_w1)
    w2src = D["w_mlp_out"][l].rearrange("(f p) n -> p f n", p=128)
    for q in range(8):
        P.dma("pool", w2[:, q * 4:(q + 1) * 4, :], w2src[:, q * 4:(q + 1) * 4, :], writes=[r_w2], owner=r_w2)
    n2w = A.alloc(DM)
    r_n2w = P.res("n2w", dma=True)
    P.dma("sp", n2w, bcast_rows(D["norm2_w"][l]), writes=[r_n2w], owner=r_n2w)
    yt = [A.alloc(DM, BF16) for _ in range(2)]
    r_yt = [P.res("yt%d" % i, dma=True) for i in range(2)]
    xs = [A.alloc(DM) for _ in range(2)]
    r_xs = [P.res("xsE%d" % i, dma=True) for i in range(2)]
    yT = [A.alloc(DM, BF16).rearrange("p (k n) -> p k n", k=8) for _ in range(2)]
    r_yT = [P.res("yT%d" % i) for i in range(2)]
    x1t = [A.alloc(DM) for _ in range(2)]
    r_x1t = [P.res("x1t%d" % i, dma=True) for i in range(2)]
    junk = A.alloc(DM)
    r_junk = P.res("junkE")
    st = [A.alloc(8) for _ in range(2)]
    r_st = [P.res("stE%d" % i) for i in range(2)]
    h2b = [A.alloc(DM, BF16) for _ in range(2)]
    r_h2b = [P.res("h2b%d" % i) for i in range(2)]
    h2s = [A.alloc(DM, BF16).rearrange("p (k n) -> p k n", k=8) for _ in range(2)]
    r_h2s = [P.res("h2s%d" % i, dma=True) for i in range(2)]
    Yv = D["Y"].rearrange("(t p) c -> t p c", p=128)
    xv = x_src.rearrange("(t p) c -> t p c", p=128)
    x1v = D["x1"].rearrange("(t p) c -> t p c", p=128)
    h2Tv = D["h2T"].rearrange("(k p) s -> p k s", p=128)
    psA = bank[0][:].bitcast(BF16)
    psB = bank[3][:].bitcast(BF16)

    def loadE1(t):
        P.dma("sp", yt[t % 2], Yv[t], writes=[r_yt[t % 2]], owner=r_yt[t % 2])
        P.dma("sp", xs[t % 2], xv[t], writes=[r_xs[t % 2]], owner=r_xs[t % 2])

    def stageY(t):
        p2 = t % 2
        if t + 1 < NT:
            loadE1(t + 1)
        for k in range(8):
            P.op("pe", lambda e, k=k: e.transpose(out=psA[:, k * 128:(k + 1) * 128], in_=yt[p2][:, k * 128:(k + 1) * 128], identity=C.ident_b),
                 reads=[r_yt[p2]], writes=[rb[0]])
        P.op("act", lambda e: e.copy(out=yT[p2], in_=psA.rearrange("p (k n) -> p k n", k=8)), reads=[rb[0]], writes=[r_yT[p2]])

    def stageP(t):
        p2 = t % 2
        for cg in range(2):
            bk = 1 + cg
            for k in range(8):
                P.op("pe", lambda e, k=k, cg=cg, bk=bk: e.matmul(bank[bk][:], lhsT=yT[p2][:, k, :], rhs=wout[:, k, cg * 512:(cg + 1) * 512], start=(k == 0), stop=(k == 7)),
                     reads=[r_yT[p2], r_wout], writes=[rb[bk]])
            P.op("dve", lambda e, cg=cg, bk=bk: e.tensor_tensor(out=x1t[p2][:, cg * 512:(cg + 1) * 512], in0=xs[p2][:, cg * 512:(cg + 1) * 512], in1=bank[bk][:], op=ALU.add),
                 reads=[rb[bk], r_xs[p2]], writes=[r_x1t[p2]])
        P.dma("sp", x1v[t], x1t[p2], reads=[r_x1t[p2]], owner=r_x1t[p2])
        P.op("act", lambda e: e.activation(out=junk, in_=x1t[p2], func=AF.Square, accum_out=st[p2][:, 0:1]), reads=[r_x1t[p2]], writes=[r_junk, r_st[p2]])
        P.op("act", lambda e: e.activation(out=st[p2][:, 1:2], in_=st[p2][:, 0:1], func=AF.Sqrt, scale=1.0 / DM, bias=C.eps_t[:, 0:1]), reads=[r_st[p2]], writes=[r_st[p2]])
        P.op("dve", lambda e: e.reciprocal(out=st[p2][:, 2:3], in_=st[p2][:, 1:2]), reads=[r_st[p2]], writes=[r_st[p2]])
        P.op("dve", lambda e: e.scalar_tensor_tensor(out=h2b[p2], in0=x1t[p2], scalar=st[p2][:, 2:3], in1=n2w, op0=ALU.mult, op1=ALU.mult),
             reads=[r_x1t[p2], r_st[p2], r_n2w], writes=[r_h2b[p2]])

    def stageH(t):
        p2 = t % 2
        for k in range(8):
            P.op("pe", lambda e, k=k: e.transpose(out=psB[:, k * 128:(k + 1) * 128], in_=h2b[p2][:, k * 128:(k + 1) * 128], identity=C.ident_b),
                 reads=[r_h2b[p2]], writes=[rb[3]])
        P.op("act", lambda e: e.copy(out=h2s[p2], in_=psB.rearrange("p (k n) -> p k n", k=8)), reads=[rb[3]], writes=[r_h2s[p2]])
        P.dma("sp", h2Tv[:, :, t * 128:(t + 1) * 128], h2s[p2], reads=[r_h2s[p2]], owner=r_h2s[p2])

    loadE1(0)
    stageY(0)
    for t in range(NT):
        stageP(t)
        if t + 1 < NT:
            stageY(t + 1)
        if t > 0:
            stageH(t - 1)
    stageH(NT - 1)
    P.barrier()

    A.off = mark
    hid = A.alloc(32 * 512, BF16).rearrange("p (f n) -> p f n", f=32)
    r_hid = [P.res("hid%d" % f) for f in range(32)]
    h2g = [A.alloc(8 * 512, BF16).rearrange("p (k n) -> p k n", k=8) for _ in range(2)]
    r_h2g = [P.res("h2g%d" % i, dma=True) for i in range(2)]
    rl = [A.alloc(512) for _ in range(2)]
    r_rl = [P.res("rl%d" % i) for i in range(2)]
    x1b = [A.alloc(DM) for _ in range(2)]
    r_x1b = [P.res("x1b%d" % i, dma=True) for i in range(2)]
    ot = [A.alloc(DM) for _ in range(2)]
    r_ot = [P.res("ot%d" % i, dma=True) for i in range(2)]
    ov = x_dst.rearrange("(t p) c -> t p c", p=128)
    P.dma("sp", h2g[0], h2Tv[:, :, 0:512], writes=[r_h2g[0]], owner=r_h2g[0])
    fcnt = 0
    for g in range(8):
        g2 = g % 2
        if g + 1 < 8:
            P.dma("sp", h2g[(g + 1) % 2], h2Tv[:, :, (g + 1) * 512:(g + 2) * 512], writes=[r_h2g[(g + 1) % 2]], owner=r_h2g[(g + 1) % 2])
        for f in range(32):
            bk = fcnt % 4
            r2 = fcnt % 2
            fcnt += 1
            for k in range(8):
                P.op("pe", lambda e, k=k, f=f, bk=bk, g2=g2: e.matmul(bank[bk][:], lhsT=w1[:, k, f * 128:(f + 1) * 128], rhs=h2g[g2][:, k, :], start=(k == 0), stop=(k == 7)),
                     reads=[r_w1, r_h2g[g2]], writes=[rb[bk]])
            P.op("act", lambda e, bk=bk, r2=r2: e.activation(out=rl[r2], in_=bank[bk][:], func=AF.Relu), reads=[rb[bk]], writes=[r_rl[r2]])
            P.op("dve", lambda e, bk=bk, r2=r2, f=f: e.tensor_tensor(out=hid[:, f, :], in0=rl[r2], in1=bank[bk][:], op=ALU.mult), reads=[rb[bk], r_rl[r2]], writes=[r_hid[f]])
        for i in range(4):
            t = 4 * g + i
            p2 = t % 2
            P.dma("sp", x1b[p2], x1v[t], writes=[r_x1b[p2]], owner=r_x1b[p2])
            for cg in range(2):
                bk = 4 + (2 * i + cg) % 4
                for f in range(32):
                    P.op("pe", lambda e, f=f, cg=cg, bk=bk, i=i: e.matmul(bank[bk][:], lhsT=hid[:, f, i * 128:(i + 1) * 128], rhs=w2[:, f, cg * 512:(cg + 1) * 512], start=(f == 0), stop=(f == 31)),
                         reads=[r_hid[f], r_w2], writes=[rb[bk]])
                P.op("dve", lambda e, cg=cg, bk=bk, p2=p2: e.tensor_tensor(out=ot[p2][:, cg * 512:(cg + 1) * 512], in0=x1b[p2][:, cg * 512:(cg + 1) * 512], in1=bank[bk][:], op=ALU.add),
                     reads=[rb[bk], r_x1b[p2]], writes=[r_ot[p2]])
            P.dma("sp", ov[t], ot[p2], reads=[r_ot[p2]], owner=r_ot[p2])
    P.end_phase()


def build(layers=(0, 1), phases=("A", "B", "C", "D", "E"), feed=(), expose=()):
    nc = bass.Bass("TRN2", target_bir_lowering=False)
    D = {}
    D["x"] = nc.dram_tensor("x", [S, DM], F32, kind="ExternalInput").ap()
    for n, shp in PARAM_SHAPES.items():
        D[n] = nc.dram_tensor(n, shp, F32, kind="ExternalInput").ap()
    for n, shp in CONST_SHAPES.items():
        D["c_" + n] = nc.dram_tensor("c_" + n, shp, BF16 if n.startswith("mask") else F32, kind="ExternalInput").ap()
    for n, (shp, dt) in SCRATCH.items():
        kind = "ExternalInput" if n in feed else ("ExternalOutput" if n in expose else "Internal")
        D[n] = nc.dram_tensor(n, shp, dt, kind=kind).ap()
    D["out"] = nc.dram_tensor("out", [S, DM], F32, kind="ExternalOutput").ap()

    with ExitStack() as es:
        C = Ctx()
        C.nc, C.D = nc, D
        C.P = P = Prog(nc, es)
        C.arena = A = Arena(nc)
        C.bank = [nc.alloc_psum_tensor("bank%d" % i, [128, 512], F32) for i in range(8)]
        C.rb = [P.res("bank%d" % i) for i in range(8)]
        for r in C.rb:
            r.excl = True
        idf = A.alloc(128)
        C.ident_f = idf
        C.ident_b = A.alloc(128, BF16)
        C.eps_t = A.alloc(8)
        C.pastm = A.alloc(32)
        r_c = P.res("consts", dma=True)
        P.dma("sp", idf, D["c_ident"], writes=[r_c], owner=r_c)
        P.dma("sp", C.pastm, D["c_pastm"], writes=[r_c], owner=r_c)
        P.op("dve", lambda e: e.tensor_copy(out=C.ident_b, in_=idf), reads=[r_c], writes=[r_c])
        P.op("dve", lambda e: e.memset(C.eps_t, EPS), writes=[r_c])
        C.one_t = A.alloc(8)
        P.op("dve", lambda e: e.memset(C.one_t, 1.0), writes=[r_c])
        P.barrier()
        C.base_off = A.off

        for l in layers:
            x_src = D["x"] if l == 0 else D["xcur"]
            x_dst = D["xcur"] if l == 0 else D["out"]
            if "A" in phases:
                phase_A(C, l, x_src)
            if "B" in phases:
                phase_attn(C, l, "A")
            if "C" in phases:
                phase_attn(C, l, "C")
            if "D" in phases:
                phase_D(C, l)
            if "E" in phases:
                phase_E(C, l, x_src, x_dst)
        stats = P.emit()
    return nc, stats


def make_in_map(inputs, b, consts):
    m = {"x": np.ascontiguousarray(inputs["x"][b])}
    for n in PARAM_SHAPES:
        m[n] = np.ascontiguousarray(inputs[n])
    for n, v in consts.items():
        m["c_" + n] = v
    return m


def kernel(**inputs):
    inputs = {k: np.asarray(v) for k, v in inputs.items()}
    nc, _ = build()
    consts = host_consts()
    in_maps = [make_in_map(inputs, c % 4, consts) for c in range(8)]
    res = run_bass_kernel_spmd(nc, in_maps, core_ids=list(range(8)))
    out = np.stack([res.results[b]["out"] for b in range(4)], axis=0)
    return out.astype(np.float32)
```
